# Optimizing a Trainium2 kernel written in Bass

```python
import jax, jax.numpy as jnp
from jax import lax
import numpy as np

D_MODEL = 1024
BATCH = 16
SEQ = 2048
DEPTH = 1
DEC_BATCH = 16
DEC_SEQ = 64
PAST_LEN = 4096

CHUNK = 64
D_MIX = D_MODEL
N_HEADS = 8
HEAD_DIM = 64
ATTN_DIM = N_HEADS * HEAD_DIM
IDX_HEADS = 8
IDX_DIM = 64
TOPK_MAX = 256
CONV_DIM = D_MIX - ATTN_DIM
CONV_WIDTH = 31
D_FF = 4 * D_MODEL
Q_BLOCK = CHUNK
EPS = 1e-6
NEG_INF = -1e30

PROJ_SIZES = (ATTN_DIM, ATTN_DIM, ATTN_DIM, IDX_HEADS * IDX_DIM, IDX_DIM, IDX_HEADS, CONV_DIM, CONV_DIM)
PROJ_DIM = int(sum(PROJ_SIZES))
PROJ_SPLITS = tuple(int(v) for v in np.cumsum(PROJ_SIZES)[:-1])

kernel_name = "hybrid_dsa_conformer_stream_step"


def alibi_slopes():
    return jnp.asarray([2.0 ** (-8.0 * (h + 1) / N_HEADS) for h in range(N_HEADS)], jnp.float32)


def rms_norm(x, g):
    xf = x.astype(jnp.float32)
    y = xf * lax.rsqrt(jnp.mean(xf * xf, axis=-1, keepdims=True) + EPS)
    return (y * g.astype(jnp.float32)).astype(x.dtype)


def layer_norm(x, g, b):
    xf = x.astype(jnp.float32)
    mu = jnp.mean(xf, axis=-1, keepdims=True)
    xc = xf - mu
    var = jnp.mean(xc * xc, axis=-1, keepdims=True)
    return (xc * lax.rsqrt(var + EPS) * g.astype(jnp.float32) + b.astype(jnp.float32)).astype(x.dtype)


def ada_modulation(c, w_ada, b_ada):
    mod = jax.nn.silu(c) @ w_ada + b_ada
    return jnp.split(mod[:, None, :], 6, axis=-1)


def split_projection(h, w_in):
    b, t, _ = h.shape
    q, k, v, qi, ki, wi, ga, gb = jnp.split(h @ w_in, PROJ_SPLITS, axis=-1)
    q = q.reshape(b, t, N_HEADS, HEAD_DIM)
    k = k.reshape(b, t, N_HEADS, HEAD_DIM)
    v = v.reshape(b, t, N_HEADS, HEAD_DIM)
    qi = qi.reshape(b, t, IDX_HEADS, IDX_DIM)
    u = ga * jax.nn.sigmoid(gb)
    return q, k, v, qi, ki, wi, u


def indexer_scores(qi, wi, ki):
    dots = jnp.einsum('bthd,bsd->bths', qi, ki, preferred_element_type=jnp.float32) * (IDX_DIM ** -0.5)
    return jnp.einsum('bths,bth->bts', jax.nn.relu(dots), wi.astype(jnp.float32) * (IDX_HEADS ** -0.5))


def sparse_attention(q, k_all, v_all, scores, q_pos, topk, slopes):
    key_pos = jnp.arange(k_all.shape[1])
    admissible = (key_pos[None, :] // CHUNK) <= (q_pos[:, None] // CHUNK)
    _, idx = lax.top_k(jnp.where(admissible[None], scores, NEG_INF), topk)
    valid = (idx // CHUNK) <= (q_pos[None, :, None] // CHUNK)
    gather_rows = jax.vmap(lambda rows, ids: rows[ids])
    k_sel = gather_rows(k_all, idx)
    v_sel = gather_rows(v_all, idx)
    logits = jnp.einsum('bthd,btkhd->bhtk', q, k_sel, preferred_element_type=jnp.float32) * (HEAD_DIM ** -0.5)
    dist = jnp.abs(q_pos[None, :, None] - idx).astype(jnp.float32)
    logits = logits - slopes[None, :, None, None] * dist[:, None]
    logits = jnp.where(valid[:, None], logits, NEG_INF)
    p = jax.nn.softmax(logits, axis=-1)
    out = jnp.einsum('bhtk,btkhd->bthd', p.astype(v_sel.dtype), v_sel, preferred_element_type=jnp.float32)
    return out.astype(q.dtype)


def conv_module(u_ext, conv_w, conv_b, ln_g, ln_b):
    y = lax.conv_general_dilated(u_ext, conv_w[:, None, :].astype(u_ext.dtype), window_strides=(1,),
                                 padding='VALID', dimension_numbers=('NWC', 'WIO', 'NWC'),
                                 feature_group_count=CONV_DIM) + conv_b
    return jax.nn.silu(layer_norm(y, ln_g, ln_b))


def prompt_mixer(h, w_in, conv_w, conv_b, ln_g, ln_b, slopes):
    b, s, _ = h.shape
    q, k, v, qi, ki, wi, u = split_projection(h, w_in)
    topk = min(TOPK_MAX, s // 4)

    def query_block(j):
        start = j * Q_BLOCK
        sl = lambda a: lax.dynamic_slice_in_dim(a, start, Q_BLOCK, axis=1)
        q_pos = start + jnp.arange(Q_BLOCK)
        sc = indexer_scores(sl(qi), sl(wi), ki)
        return sparse_attention(sl(q), k, v, sc, q_pos, topk, slopes)

    attn = lax.map(query_block, jnp.arange(s // Q_BLOCK))
    attn = jnp.moveaxis(attn, 0, 1).reshape(b, s, ATTN_DIM)
    u_ext = jnp.pad(u, ((0, 0), (CONV_WIDTH - 1, 0), (0, 0)))
    conv = conv_module(u_ext, conv_w, conv_b, ln_g, ln_b)
    mix = jnp.concatenate([attn, conv], axis=-1)
    return mix, (k, v, ki, u_ext[:, -(CONV_WIDTH - 1):])


def sample_mixer(h, cache_k, cache_v, cache_kidx, state_conv, w_in, conv_w, conv_b, ln_g, ln_b, slopes):
    b, t, _ = h.shape
    past = cache_k.shape[1]
    q, k, v, qi, ki, wi, u = split_projection(h, w_in)
    k_all = jnp.concatenate([cache_k, k], axis=1)
    v_all = jnp.concatenate([cache_v, v], axis=1)
    ki_all = jnp.concatenate([cache_kidx, ki], axis=1)
    topk = min(TOPK_MAX, (past + t) // 4)
    q_pos = past + jnp.arange(t)
    sc = indexer_scores(qi, wi, ki_all)
    attn = sparse_attention(q, k_all, v_all, sc, q_pos, topk, slopes).reshape(b, t, ATTN_DIM)
    u_ext = jnp.concatenate([state_conv.astype(u.dtype), u], axis=1)
    conv = conv_module(u_ext, conv_w, conv_b, ln_g, ln_b)
    mix = jnp.concatenate([attn, conv], axis=-1)
    return mix, (k, v, ki, u_ext[:, -(CONV_WIDTH - 1):])


def residual_block(x, c, mixer, w_ada, b_ada, norm1_g, w_out, norm2_g, w_ff1, w_ff2):
    sh1, sc1, g1, sh2, sc2, g2 = ada_modulation(c, w_ada, b_ada)
    h = rms_norm(x, norm1_g) * (1 + sc1) + sh1
    mix, state = mixer(h)
    x = x + g1 * (mix @ w_out)
    h = rms_norm(x, norm2_g) * (1 + sc2) + sh2
    x = x + g2 * (jnp.square(jax.nn.relu(h @ w_ff1)) @ w_ff2)
    return x, state


def setup_inputs(seed: int = 0) -> dict:
    key = jax.random.key(seed)
    ks = jax.random.split(key, 24)
    nrm = lambda k, shape, scale: jax.random.normal(k, shape, jnp.float32) * scale
    return {
        "x_prompt": nrm(ks[0], (BATCH, SEQ, D_MODEL), 1.0),
        "x_sample": nrm(ks[1], (DEC_BATCH, DEC_SEQ, D_MODEL), 1.0),
        "c_prompt": nrm(ks[2], (BATCH, D_MODEL), 1.0),
        "c_sample": nrm(ks[3], (DEC_BATCH, D_MODEL), 1.0),
        "cache_k": nrm(ks[4], (DEPTH, DEC_BATCH, PAST_LEN, N_HEADS, HEAD_DIM), 1.0),
        "cache_v": nrm(ks[5], (DEPTH, DEC_BATCH, PAST_LEN, N_HEADS, HEAD_DIM), 1.0),
        "cache_kidx": nrm(ks[6], (DEPTH, DEC_BATCH, PAST_LEN, IDX_DIM), 1.0),
        "state_conv": nrm(ks[7], (DEPTH, DEC_BATCH, CONV_WIDTH - 1, CONV_DIM), 0.5),
        "w_ada": nrm(ks[8], (DEPTH, D_MODEL, 6 * D_MODEL), 0.5 * D_MODEL ** -0.5),
        "b_ada": nrm(ks[9], (DEPTH, 6 * D_MODEL), 0.02),
        "norm1_g": 1.0 + nrm(ks[10], (DEPTH, D_MODEL), 0.02),
        "w_in": nrm(ks[11], (DEPTH, D_MODEL, PROJ_DIM), D_MODEL ** -0.5),
        "conv_w": nrm(ks[12], (DEPTH, CONV_WIDTH, CONV_DIM), CONV_WIDTH ** -0.5),
        "conv_b": nrm(ks[13], (DEPTH, CONV_DIM), 0.02),
        "conv_ln_g": 1.0 + nrm(ks[14], (DEPTH, CONV_DIM), 0.02),
        "conv_ln_b": nrm(ks[15], (DEPTH, CONV_DIM), 0.02),
        "w_out": nrm(ks[16], (DEPTH, D_MIX, D_MODEL), D_MIX ** -0.5),
        "norm2_g": 1.0 + nrm(ks[17], (DEPTH, D_MODEL), 0.02),
        "w_ff1": nrm(ks[18], (DEPTH, D_MODEL, D_FF), D_MODEL ** -0.5),
        "w_ff2": nrm(ks[19], (DEPTH, D_FF, D_MODEL), D_FF ** -0.5),
        "final_g": 1.0 + nrm(ks[20], (D_MODEL,), 0.02),
    }


def reference(x_prompt, x_sample, c_prompt, c_sample, cache_k, cache_v, cache_kidx, state_conv,
              w_ada, b_ada, norm1_g, w_in, conv_w, conv_b, conv_ln_g, conv_ln_b, w_out, norm2_g,
              w_ff1, w_ff2, final_g):
    slopes = alibi_slopes()
    yp, ys = x_prompt, x_sample
    kp_l, vp_l, kip_l, cp_l, ks_l, vs_l, kis_l, cs_l = [], [], [], [], [], [], [], []
    for l in range(DEPTH):
        pmix = lambda h: prompt_mixer(h, w_in[l], conv_w[l], conv_b[l], conv_ln_g[l], conv_ln_b[l], slopes)
        yp, (kp, vp, kip, cp) = residual_block(yp, c_prompt, pmix, w_ada[l], b_ada[l], norm1_g[l],
                                               w_out[l], norm2_g[l], w_ff1[l], w_ff2[l])
        smix = lambda h: sample_mixer(h, cache_k[l], cache_v[l], cache_kidx[l], state_conv[l], w_in[l],
                                      conv_w[l], conv_b[l], conv_ln_g[l], conv_ln_b[l], slopes)
        ys, (kss, vss, kis, css) = residual_block(ys, c_sample, smix, w_ada[l], b_ada[l], norm1_g[l],
                                                  w_out[l], norm2_g[l], w_ff1[l], w_ff2[l])
        kp_l.append(kp); vp_l.append(vp); kip_l.append(kip); cp_l.append(cp)
        ks_l.append(kss); vs_l.append(vss); kis_l.append(kis); cs_l.append(css)
    y_prompt = rms_norm(yp, final_g)
    y_sample = rms_norm(ys, final_g)
    return (y_prompt, y_sample,
            jnp.stack(kp_l), jnp.stack(vp_l), jnp.stack(kip_l), jnp.stack(cp_l),
            jnp.stack(ks_l), jnp.stack(vs_l), jnp.stack(kis_l), jnp.stack(cs_l))
```

```python
import numpy as np
import ml_dtypes
import concourse.bass as bass
import concourse.mybir as mybir
from concourse.bass_utils import run_bass_kernel_spmd
from contextlib import ExitStack

F32 = mybir.dt.float32
BF16 = mybir.dt.bfloat16
AF = mybir.ActivationFunctionType
ALU = mybir.AluOpType

ENGS = ['pe', 'act', 'dve', 'pool', 'sp']
EPOCH = 12000
NDMA_SEM = 22
BLK = 256

D = 1024
SEQ = 2048
DSEQ = 64
PAST = 4096
NKS = PAST + DSEQ
PROJ = 3144
EPS = 1e-6
NITER = 18
PIPE = True
BIS_YIELDS = 3
BIS_B = 8.0


class Buf:
    def __init__(self, ap, off, nbytes, esz):
        self.ap, self.off, self.nbytes, self.esz = ap, off, nbytes, esz

    def keys(self, lo=None, hi=None):
        if lo is None:
            a, b = self.off, self.off + self.nbytes
        else:
            a, b = self.off + lo * self.esz, self.off + hi * self.esz
        return [('sb', i) for i in range(a // BLK, (b - 1) // BLK + 1)]


def _expand(items):
    out = []
    for it in items:
        if isinstance(it, Buf):
            out.extend(it.keys())
        elif isinstance(it, tuple) and len(it) == 3 and isinstance(it[0], Buf):
            out.extend(it[0].keys(it[1], it[2]))
        else:
            out.append(it)
    return out


class Prog:
    def __init__(self, nc, stack):
        self.nc = nc
        self.stack = stack
        self.ops = {e: [] for e in ENGS}
        self.cnt = {e: 0 for e in ENGS}
        self.sems = {e: [] for e in ENGS}
        self.dma_sems = [stack.enter_context(nc.semaphore(f"dq{i}")) for i in range(NDMA_SEM)]
        self.dma_val = [0] * NDMA_SEM
        self.dma_rr = 0
        self.dma_rr_pool = 0
        self.waited = {e: {} for e in ENGS}
        self.last_w = {}
        self.readers = {}
        self.nins = 0

    def _sem(self, e, ep):
        while len(self.sems[e]) <= ep:
            self.sems[e].append(self.stack.enter_context(self.nc.semaphore(f"s_{e}{len(self.sems[e])}")))
        return self.sems[e][ep]

    def _deps(self, eng, reads, writes, is_dma):
        deps = set()
        for r in reads:
            t = self.last_w.get(r)
            if t is not None:
                deps.add(t)
        skip_same = (eng == 'pe') and not is_dma
        for w in writes:
            t = self.last_w.get(w)
            if t is not None and not (skip_same and t[0] == 'e' and t[1] == eng):
                deps.add(t)
            for t in self.readers.get(w, ()):
                if not (skip_same and t[0] == 'e' and t[1] == eng):
                    deps.add(t)
        return deps

    def _emit_waits(self, eng, deps):
        best = {}
        for t in deps:
            key = (t[0], t[1], t[2])
            if best.get(key, 0) < t[3]:
                best[key] = t[3]
        for key, val in best.items():
            if self.waited[eng].get(key, 0) >= val:
                continue
            self.waited[eng][key] = val
            sem = self._sem(key[1], key[2]) if key[0] == 'e' else self.dma_sems[key[1]]
            self.ops[eng].append(lambda E, sem=sem, val=val: E.wait_ge(sem, val))

    def _commit(self, tok, reads, writes):
        for r in reads:
            self.readers.setdefault(r, []).append(tok)
        for w in writes:
            self.last_w[w] = tok
            self.readers[w] = []

    def op(self, eng, fn, reads=(), writes=()):
        reads, writes = _expand(reads), _expand(writes)
        self._emit_waits(eng, self._deps(eng, reads, writes, False))
        n = self.cnt[eng]
        ep, idx = divmod(n, EPOCH)
        self.cnt[eng] = n + 1
        sem = self._sem(eng, ep)
        self.ops[eng].append(lambda E, fn=fn, sem=sem: fn(E).then_inc(sem, 1))
        tok = ('e', eng, ep, idx + 1)
        self._commit(tok, reads, writes)
        self.nins += 1
        return tok

    def dma(self, q, out, in_, reads=(), writes=(), **kw):
        reads, writes = _expand(reads), _expand(writes)
        half = NDMA_SEM // 2
        if q == 'pool':
            k = half + self.dma_rr_pool
            self.dma_rr_pool = (self.dma_rr_pool + 1) % (NDMA_SEM - half)
        else:
            k = self.dma_rr
            self.dma_rr = (k + 1) % half
        deps = self._deps(q, reads, writes, True)
        if self.dma_val[k] > 0:
            deps.add(('d', k, 0, self.dma_val[k]))
        self._emit_waits(q, deps)
        self.dma_val[k] += 16
        sem = self.dma_sems[k]
        self.ops[q].append(lambda E, sem=sem, out=out, in_=in_, kw=kw: E.dma_start(out=out, in_=in_, **kw).then_inc(sem, 16))
        tok = ('d', k, 0, self.dma_val[k])
        self._commit(tok, reads, writes)
        self.nins += 1
        return tok

    def finish(self):
        deps = set()
        for k in range(NDMA_SEM):
            if self.dma_val[k] > 0:
                deps.add(('d', k, 0, self.dma_val[k]))
        for e in ['pe', 'act', 'dve', 'pool']:
            n = self.cnt[e]
            if n > 0:
                ep, idx = divmod(n - 1, EPOCH)
                deps.add(('e', e, ep, idx + 1))
        self._emit_waits('sp', deps)

    def run_block(self):
        nc = self.nc
        ops = self.ops
        with nc.Block() as block:
            @block.sync
            def _(E):
                for f in ops['sp']:
                    f(E)

            @block.tensor
            def _(E):
                for f in ops['pe']:
                    f(E)

            @block.scalar
            def _(E):
                for f in ops['act']:
                    f(E)

            @block.vector
            def _(E):
                for f in ops['dve']:
                    f(E)

            @block.gpsimd
            def _(E):
                for f in ops['pool']:
                    f(E)


class Region:
    def __init__(self, base, size):
        self.base, self.size, self.off = base, size, 0

    def reset(self):
        self.off = 0

    def alloc(self, nbytes):
        al = BLK if nbytes >= 1024 else 32
        start = (self.base + self.off + al - 1) // al * al
        self.off = start - self.base + (nbytes + al - 1) // al * al
        assert self.off <= self.size, (self.off, self.size)
        return start


FM_COLS = ([(c * 128, 128) for c in range(4)] + [(512 + c * 128, 128) for c in range(4)] +
           [(1536 + c * 128, 128) for c in range(4)] + [(2048, 64)] +
           [(2120 + c * 128, 128) for c in range(4)] + [(2632 + c * 128, 128) for c in range(4)])

R_P_SZ = 10 * 1024
R_U_SZ = 90 * 1024
R_UW = (90 + 52) * 1024
R_U_K = [74 * 1024, 90 * 1024] if PIPE else [90 * 1024, 90 * 1024]
R_M_SZ = 32 * 1024
R_W_SZ = 70 * 1024
ARENA = R_P_SZ + R_U_SZ + R_M_SZ + R_W_SZ


def build(phases=(1, 2, 3)):
    nc = bass.Bass("TRN2", target_bir_lowering=False)
    din = lambda name, shape, dt=F32: nc.dram_tensor(name, list(shape), dt, kind="ExternalInput").ap()
    dout = lambda name, shape: nc.dram_tensor(name, list(shape), F32, kind="ExternalOutput").ap()
    dint = lambda name, shape, dt=BF16: nc.dram_tensor(name, list(shape), dt, kind="Internal").ap()

    xp = din("xp", [2, SEQ, D]); xs = din("xs", [2, DSEQ, D])
    cT_d = din("cT", [128, 8, 4])
    ckT = din("ckT", [2, 8, 64, PAST]); cv = din("cv", [2, PAST, 512]); ckiT = din("ckiT", [2, 64, PAST])
    scT_d = din("scT", [2, 512, 30])
    w_ada = din("w_ada", [D, 6 * D]); b_adaT = din("b_adaT", [128, 48])
    n1gT_d = din("n1gT", [128, 8]); n2gT_d = din("n2gT", [128, 8]); fgbc_d = din("fgbc", [128, D])
    w_in = din("w_in", [D, PROJ]); w_out = din("w_out", [D, D]); w_ff1 = din("w_ff1", [D, 4 * D]); w_ff2 = din("w_ff2", [4 * D, D])
    convwT_d = din("convwT", [128, 4, 31]); convbT_d = din("convbT", [128, 4]); lngT_d = din("lngT", [128, 4]); lnbT_d = din("lnbT", [128, 4])
    c_cdiag = din("c_cdiag", [128, 8, 128], BF16)
    c_btab = din("c_btab", [128, 8, 36])

    o_yp = dout("yp", [2, SEQ, D]); o_ys = dout("ys", [2, DSEQ, D])
    o_kp = dout("kp", [2, SEQ, 512]); o_vp = dout("vp", [2, SEQ, 512]); o_kip = dout("kip", [2, SEQ, 64]); o_cp = dout("cp", [2, 30, 512])
    o_ks = dout("ks", [2, DSEQ, 512]); o_vs = dout("vs", [2, DSEQ, 512]); o_kis = dout("kis", [2, DSEQ, 64]); o_cs = dout("cs", [2, 30, 512])

    wsc_fm = dint("wsc_fm", [21, 128, 8, 128])
    wsc_tm = dint("wsc_tm", [D, 1096])
    wsc_out = dint("wsc_out", [D, D])
    wsc_ff1 = dint("wsc_ff1", [32, 128, 8, 128])
    wsc_ff2 = dint("wsc_ff2", [4 * D, D])

    with ExitStack() as st:
        P = Prog(nc, st)
        arena = st.enter_context(nc.sbuf_tensor("arena", [128, ARENA // 2], BF16))
        banks = [st.enter_context(nc.psum_tensor(f"bank{i}", [128, 512], F32)) for i in range(8)]
        BK = lambda i: ('ps', i)
        bkb = lambda i: banks[i][:, :].bitcast(BF16)

        if True:
            RP = Region(0, R_P_SZ); RU = Region(R_P_SZ, R_U_SZ); RM = Region(R_P_SZ + R_U_SZ, R_M_SZ)
            RW = Region(R_P_SZ + R_U_SZ + R_M_SZ, R_W_SZ)

        def mk(reg, shape, dt):
            esz = 4 if dt == F32 else 2
            n = int(np.prod(shape))
            off = reg.alloc(n * esz)
            ap = arena[:, off // 2: off // 2 + n * esz // 2]
            if dt == F32:
                ap = ap.bitcast(F32)
            if len(shape) == 2:
                ap = ap.rearrange("p (a b) -> p a b", b=shape[1])
            elif len(shape) == 3:
                ap = ap.rearrange("p (a b c) -> p a b c", b=shape[1], c=shape[2])
            return Buf(ap, off, n * esz, esz)

        identf = mk(RP, [128], F32); identb = mk(RP, [128], BF16)
        ones64b = mk(RP, [64], BF16); onesb = mk(RP, [128], BF16); onesf = mk(RP, [128], F32)
        cdiag = mk(RP, [8, 128], BF16); btab = mk(RP, [8, 36], F32)
        modT = mk(RP, [48, 4], F32)
        cTs = mk(RP, [8, 4], F32); scs = mk(RP, [8, 4], F32)
        badaT = mk(RP, [48], F32); n1gT = mk(RP, [8], F32); n2gT = mk(RP, [8], F32)
        convwT = mk(RP, [4, 31], F32); convbT = mk(RP, [4], F32); lngT = mk(RP, [4], F32); lnbT = mk(RP, [4], F32)
        G1T = mk(RP, [8], F32); S1T = mk(RP, [8], F32); G2T = mk(RP, [8], F32); S2T = mk(RP, [8], F32)
        g1T = mk(RP, [8], F32); g2T = mk(RP, [8], F32)
        ss = mk(RP, [16], F32); rs = mk(RP, [16], F32)
        sacc = mk(RP, [2], F32); dd = mk(RP, [2], F32); negmid = mk(RP, [2], F32); thr = mk(RP, [2], F32)
        dgt = mk(RP, [128], F32)

        ident_tok = None

        for ci, (c0, cw) in enumerate(FM_COLS):
            if cw == 128:
                P.dma('pool', wsc_fm[ci].rearrange("p kc n -> kc p n"),
                      w_in[:, c0:c0 + 128].rearrange("(kc p) n -> kc p n", p=128), writes=[('wfm', ci)])
            else:
                for hh in range(2):
                    P.dma('pool', wsc_fm[ci][:, :, hh * 64:(hh + 1) * 64].rearrange("p kc n -> kc p n"),
                          w_in[:, c0:c0 + 64].rearrange("(kc p) n -> kc p n", p=128), writes=[('wfm', ci, hh)])
        P.dma('pool', wsc_tm[:, 0:1024], w_in[:, 512:1536], writes=['wtm_a'])
        P.dma('pool', wsc_tm[:, 1024:1096], w_in[:, 2048:2120], writes=['wtm_b'])
        for i in range(4):
            P.dma('pool', wsc_out[i * 256:(i + 1) * 256, :], w_out[i * 256:(i + 1) * 256, :], writes=[('wout', i)])
        for j in range(32):
            P.dma('pool', wsc_ff1[j].rearrange("p kc n -> kc p n"),
                  w_ff1[:, j * 128:(j + 1) * 128].rearrange("(kc p) n -> kc p n", p=128), writes=[('wff1', j)])
        for i in range(16):
            P.dma('pool', wsc_ff2[i * 256:(i + 1) * 256, :], w_ff2[i * 256:(i + 1) * 256, :], writes=[('wff2', i)])
        WFM = [('wfm', ci) for ci in range(21)] + [('wfm', 12, 0), ('wfm', 12, 1)]
        WOUT = [('wout', i) for i in range(4)]
        WFF1 = [('wff1', j) for j in range(32)]
        WFF2 = [('wff2', i) for i in range(16)]

        P.op('dve', lambda E: E.memset(identf.ap, 0.0), writes=[identf])
        P.op('pool', lambda E: E.affine_select(out=identf.ap, in_=identf.ap, pattern=[[-1, 128]], compare_op=ALU.not_equal,
                                                 fill=1.0, base=0, channel_multiplier=1), reads=[identf], writes=[identf])
        P.op('dve', lambda E: E.tensor_copy(out=identb.ap, in_=identf.ap), reads=[identf], writes=[identb])
        P.op('dve', lambda E: E.memset(onesb.ap, 1.0), writes=[onesb])
        P.op('dve', lambda E: E.memset(onesf.ap, 1.0), writes=[onesf])
        for (b, d_) in [(cdiag, c_cdiag), (btab, c_btab), (cTs, cT_d), (badaT, b_adaT), (n1gT, n1gT_d), (n2gT, n2gT_d), (convwT, convwT_d),
                        (convbT, convbT_d), (lngT, lngT_d), (lnbT, lnbT_d)]:
            P.dma('sp', b.ap, d_, writes=[b])

        P.op('act', lambda E: E.activation(out=scs.ap, in_=cTs.ap, func=AF.Silu), reads=[cTs], writes=[scs])
        RU.reset()
        wada_b = [mk(RU, [8, 1024], F32), mk(RU, [8, 1024], F32)]
        modps = banks[0][:, 0:192].rearrange("p (a b) -> p a b", b=4)
        for i in range(6):
            wb_ = wada_b[i % 2]
            P.dma('sp', wb_.ap, w_ada[:, i * 1024:(i + 1) * 1024].rearrange("(kc p) n -> p kc n", p=128), writes=[wb_])
            for cl in range(8):
                ch = i * 8 + cl
                for kc in range(8):
                    P.op('pe', lambda E, wb_=wb_, cl=cl, kc=kc, ch=ch: E.matmul(modps[:, ch, :], lhsT=wb_.ap[:, kc, cl * 128:(cl + 1) * 128],
                                                                             rhs=scs.ap[:, kc, :], start=(kc == 0), stop=(kc == 7)),
                         reads=[wb_, scs], writes=[BK(0)])
        P.op('dve', lambda E: E.tensor_tensor(out=modT.ap, in0=modps, in1=badaT.ap.unsqueeze(2).to_broadcast([128, 48, 4]), op=ALU.add),
             reads=[BK(0), badaT], writes=[modT])

        def bcast_vec(srcT, dst):
            for half in range(2):
                for q in range(4):
                    kc = half * 4 + q
                    P.op('dve', lambda E, kc=kc: E.tensor_scalar(out=dgt.ap, in0=identf.ap, scalar1=srcT.ap[:, kc:kc + 1], scalar2=None, op0=ALU.mult),
                         reads=[identf, srcT], writes=[dgt])
                    P.op('pe', lambda E, q=q: E.matmul(banks[1][:, q * 128:(q + 1) * 128], lhsT=onesf.ap, rhs=dgt.ap, start=True, stop=True),
                         reads=[onesf, dgt], writes=[BK(1)])
                P.op('act', lambda E, half=half: E.activation(out=dst.ap[:, half * 512:(half + 1) * 512], in_=banks[1][:, :], func=AF.Copy),
                     reads=[BK(1)], writes=[dst])

        units = [dict(kind=0, j=0, sq=0), dict(kind=0, j=1, sq=1), dict(kind=1, j=2, sq=0), dict(kind=1, j=3, sq=1)]
        def do_unit(U):
            kind, j, sq = U['kind'], U['j'], U['sq']
            T = SEQ if kind == 0 else DSEQ
            TP = 128 if kind == 0 else 64
            NT = T // TP
            GT = 512 if kind == 0 else 64
            NG = T // GT
            TPG = GT // TP
            NK = SEQ if kind == 0 else NKS
            NKB = (NK + 127) // 128
            KOFF = 0 if kind == 0 else PAST
            x_d = xp[sq] if kind == 0 else xs[sq]
            o_y = o_yp[sq] if kind == 0 else o_ys[sq]
            o_k = o_kp[sq] if kind == 0 else o_ks[sq]
            o_v = o_vp[sq] if kind == 0 else o_vs[sq]
            o_ki = o_kip[sq] if kind == 0 else o_kis[sq]
            o_c = o_cp[sq] if kind == 0 else o_cs[sq]
            UE = 30 + T

            for (dst, gsrc, off) in [(G1T, n1gT, 8), (G2T, n2gT, 32)]:
                P.op('dve', lambda E, dst=dst, gsrc=gsrc, off=off: E.scalar_tensor_tensor(out=dst.ap, in0=modT.ap[:, off:off + 8, j], scalar=1.0, in1=gsrc.ap,
                                                                                      op0=ALU.add, op1=ALU.mult), reads=[modT, gsrc], writes=[dst])
            for (dst, off) in [(S1T, 0), (S2T, 24), (g1T, 16), (g2T, 40)]:
                P.op('dve', lambda E, dst=dst, off=off: E.tensor_copy(out=dst.ap, in_=modT.ap[:, off:off + 8, j]), reads=[modT], writes=[dst])

            RU.reset(); RM.reset(); RW.reset()
            QA = mk(RU, [8, T], BF16); KA = mk(RU, [4, NK], BF16); Vb = mk(RU, [NKB, 8, 65], BF16)
            KIz = [mk(RU, [NK], BF16) for _ in range(2)]; QIT = mk(RU, [4, T], BF16)
            Wt = mk(RU, [16, 8], F32)
            mixT = mk(RM, [8, T], BF16)
            uT = mk(RW, [4, UE], BF16)

            P.op('pool', lambda E: E.memset(QA.ap, 0.0), writes=[QA])
            P.op('pool', lambda E: E.memset(Vb.ap, 1.0), writes=[Vb])
            P.op('pool', lambda E: E.memset(KIz[0].ap[64:128, :], 0.0), writes=[KIz[0]])
            P.op('pool', lambda E: E.memset(KIz[1].ap[0:64, :], 0.0), writes=[KIz[1]])

            RM.reset()
            wtm = mk(RM, [8, 1096], BF16); hT0 = mk(RM, [8, GT], BF16)
            fmr = [mk(RM, [8, 128], BF16) for _ in range(3)]
            xt = [mk(RW, [D], F32) for _ in range(2)]
            kout = mk(RW, [512], F32); vout = mk(RW, [512], F32); kiw = mk(RW, [72], F32)
            sig = mk(RW, [GT], F32); junk = mk(RW, [D], BF16)
            ulast = mk(RW, [4, 32], F32); cst = mk(RW, [512], F32)
            hTs = [hT0, mk(RW, [8, GT], BF16)]

            P.dma('sp', wtm.ap[:, :, 0:1024], wsc_tm[:, 0:1024].rearrange("(kc p) n -> p kc n", p=128), reads=['wtm_a'], writes=[wtm])
            P.dma('sp', wtm.ap[:, :, 1024:1096], wsc_tm[:, 1024:1096].rearrange("(kc p) n -> p kc n", p=128), reads=['wtm_b'], writes=[wtm])
            if kind == 0:
                P.op('pool', lambda E: E.memset(uT.ap[:, :, 0:30], 0.0), writes=[uT])
            else:
                stg_off = [RW.base + RW.size - 2 * PAST * 4, RW.base + RW.size - PAST * 4]
                stg = [Buf(arena[:, o_ // 2:o_ // 2 + PAST * 2].bitcast(F32), o_, PAST * 4, 4) for o_ in stg_off]
                P.dma('sp', stg[0].ap[0:64, :], ckiT[sq], writes=[stg[0]])
                P.dma('sp', stg[0].ap[64:128, :], ckiT[sq], writes=[stg[0]])
                P.dma('sp', stg[1].ap, ckT[sq, 0:2].rearrange("a d n -> (a d) n"), writes=[stg[1]])

                def load_kidx():
                    P.op('act', lambda E: E.activation(out=KIz[0].ap[0:64, 0:PAST], in_=stg[0].ap[0:64, :], func=AF.Copy), reads=[stg[0]], writes=[(KIz[0], 0, PAST)])
                    P.op('act', lambda E: E.activation(out=KIz[1].ap[64:128, 0:PAST], in_=stg[0].ap[64:128, :], func=AF.Copy), reads=[stg[0]], writes=[(KIz[1], 0, PAST)])

                def gen_load():
                    items = [('k', 1), ('k', 2), ('k', 3), ('v', 0), ('v', 1), ('v', 2), ('v', 3)]
                    loaded = [('k', 0, stg[1])]
                    free = [stg[0]]
                    ci = 0
                    while loaded or items:
                        if items and free:
                            typ, idx = items.pop(0)
                            sb2 = free.pop(0)
                            if typ == 'k':
                                P.dma('sp', sb2.ap, ckT[sq, 2 * idx:2 * idx + 2].rearrange("a d n -> (a d) n"), writes=[sb2])
                            else:
                                P.dma('sp', sb2.ap.rearrange("p (a f) -> p a f", f=512), cv[sq][idx * 1024:(idx + 1) * 1024, :].rearrange("(kb p) f -> p kb f", p=128), writes=[sb2])
                            loaded.append((typ, idx, sb2))
                        typ, idx, sb2 = loaded.pop(0)
                        eng_ = 'dve' if ci % 2 == 0 else 'act'
                        ci += 1
                        if typ == 'k':
                            if eng_ == 'dve':
                                P.op('dve', lambda E, sb2=sb2, idx=idx: E.tensor_copy(out=KA.ap[:, idx, 0:PAST], in_=sb2.ap), reads=[sb2], writes=[(KA, idx * NK, idx * NK + PAST)])
                            else:
                                P.op('act', lambda E, sb2=sb2, idx=idx: E.activation(out=KA.ap[:, idx, 0:PAST], in_=sb2.ap, func=AF.Copy), reads=[sb2], writes=[(KA, idx * NK, idx * NK + PAST)])
                        else:
                            outv = Vb.ap[:, idx * 8:(idx + 1) * 8, :, 0:64].rearrange("p a h d -> p (a h) d")
                            inv = sb2.ap.rearrange("p (ah d) -> p ah d", d=64)
                            if eng_ == 'dve':
                                P.op('dve', lambda E, outv=outv, inv=inv: E.tensor_copy(out=outv, in_=inv), reads=[sb2], writes=[(Vb, idx * 8 * 520, (idx + 1) * 8 * 520)])
                            else:
                                P.op('act', lambda E, outv=outv, inv=inv: E.activation(out=outv, in_=inv, func=AF.Copy), reads=[sb2], writes=[(Vb, idx * 8 * 520, (idx + 1) * 8 * 520)])
                        free.append(sb2)
                        yield
                P.dma('pool', uT.ap[:, :, 0:30], scT_d[sq].rearrange("(c p) t -> p c t", p=128), writes=[uT])
            P.op('pool', lambda E: E.memset(ulast.ap, 0.0), writes=[ulast])

            fm_rr = 0
            fmst = dict(rr=0)
            def p1_pro(g):
                hT = hTs[g % 2]
                for tl in range(TPG):
                    tt = g * TPG + tl
                    xb = xt[tt % 2]
                    P.dma('sp', xb.ap[0:TP, :], x_d[tt * TP:(tt + 1) * TP, :], writes=[xb])
                    P.op('act', lambda E, xb=xb, tt=tt: E.activation(out=junk.ap[0:TP, :], in_=xb.ap[0:TP, :], func=AF.Square, accum_out=ss.ap[0:TP, tt:tt + 1]),
                         reads=[xb], writes=[junk, (ss, tt, tt + 1)])
                    P.op('dve', lambda E, tt=tt: E.tensor_scalar(out=rs.ap[0:TP, tt:tt + 1], in0=ss.ap[0:TP, tt:tt + 1], scalar1=1.0 / D, scalar2=EPS, op0=ALU.mult, op1=ALU.add),
                         reads=[(ss, tt, tt + 1)], writes=[(rs, tt, tt + 1)])
                    P.op('act', lambda E, tt=tt: E.activation(out=rs.ap[0:TP, tt:tt + 1], in_=rs.ap[0:TP, tt:tt + 1], func=AF.Sqrt),
                         reads=[(rs, tt, tt + 1)], writes=[(rs, tt, tt + 1)])
                    P.op('dve', lambda E, tt=tt: E.reciprocal(out=rs.ap[0:TP, tt:tt + 1], in_=rs.ap[0:TP, tt:tt + 1]),
                         reads=[(rs, tt, tt + 1)], writes=[(rs, tt, tt + 1)])
                    P.op('dve', lambda E, xb=xb, tt=tt: E.tensor_scalar(out=xb.ap[0:TP, :], in0=xb.ap[0:TP, :], scalar1=rs.ap[0:TP, tt:tt + 1], scalar2=None, op0=ALU.mult),
                         reads=[xb, (rs, tt, tt + 1)], writes=[xb])
                    for kc in range(8):
                        bk = kc // 4
                        P.op('pe', lambda E, xb=xb, kc=kc, bk=bk: E.transpose(banks[bk][:, (kc % 4) * 128:(kc % 4) * 128 + TP], xb.ap[0:TP, kc * 128:(kc + 1) * 128], identf.ap[0:TP, 0:TP]),
                             reads=[xb, identf], writes=[BK(bk)])
                    for kc in range(8):
                        bk = kc // 4
                        P.op('act', lambda E, kc=kc, bk=bk, tl=tl: E.activation(out=hT.ap[:, kc, tl * TP:(tl + 1) * TP], in_=banks[bk][:, (kc % 4) * 128:(kc % 4) * 128 + TP],
                                                                        func=AF.Identity, scale=G1T.ap[:, kc:kc + 1], bias=S1T.ap[:, kc:kc + 1]),
                             reads=[BK(bk), G1T, S1T], writes=[(hT, kc * GT + tl * TP, kc * GT + (tl + 1) * TP)])
            def p1_fm(g):
                hT = hTs[g % 2]
                order = [0, 1, 2, 3, 4, 5, 6, 7, 8, 9, 10, 11, 12, 17, 13, 18, 14, 19, 15, 20, 16]
                t0 = g * GT
                for ci in order:
                    wb_ = fmr[fmst['rr'] % 3]
                    pb = 2 + (fmst['rr'] % 3)
                    fmst['rr'] += 1
                    P.dma('sp', wb_.ap, wsc_fm[ci], reads=WFM, writes=[wb_])
                    for kc in range(8):
                        P.op('pe', lambda E, wb_=wb_, kc=kc, pb=pb: E.matmul(banks[pb][:, 0:GT], lhsT=wb_.ap[:, kc, :], rhs=hT.ap[:, kc, :], start=(kc == 0), stop=(kc == 7)),
                             reads=[wb_, hT], writes=[BK(pb)])
                    src = banks[pb][:, 0:GT]
                    if ci < 4:
                        c = ci
                        P.op('act', lambda E, src=src, c=c, t0=t0: E.activation(out=QA.ap[0:64, 2 * c, t0:t0 + GT], in_=src[0:64, :], func=AF.Copy, scale=0.125),
                             reads=[BK(pb)], writes=[(QA, 2 * c * T + t0, 2 * c * T + t0 + GT)])
                        P.op('act', lambda E, src=src, c=c, t0=t0: E.activation(out=QA.ap[64:128, 2 * c + 1, t0:t0 + GT], in_=src[64:128, :], func=AF.Copy, scale=0.125),
                             reads=[BK(pb)], writes=[((QA, (2 * c + 1) * T + t0, (2 * c + 1) * T + t0 + GT))])
                    elif ci < 8:
                        c = ci - 4
                        P.op('dve', lambda E, src=src, c=c, t0=t0: E.tensor_copy(out=KA.ap[:, c, KOFF + t0:KOFF + t0 + GT], in_=src),
                             reads=[BK(pb)], writes=[(KA, c * NK + KOFF + t0, c * NK + KOFF + t0 + GT)])
                    elif ci < 12:
                        c = ci - 8
                        P.op('act', lambda E, src=src, c=c, t0=t0: E.activation(out=QIT.ap[:, c, t0:t0 + GT], in_=src, func=AF.Copy),
                             reads=[BK(pb)], writes=[(QIT, c * T + t0, c * T + t0 + GT)])
                    elif ci == 12:
                        P.op('dve', lambda E, src=src, t0=t0: E.tensor_copy(out=KIz[0].ap[0:64, KOFF + t0:KOFF + t0 + GT], in_=src[0:64, :]),
                             reads=[BK(pb)], writes=[(KIz[0], KOFF + t0, KOFF + t0 + GT)])
                        P.op('dve', lambda E, src=src, t0=t0: E.tensor_copy(out=KIz[1].ap[64:128, KOFF + t0:KOFF + t0 + GT], in_=src[64:128, :]),
                             reads=[BK(pb)], writes=[(KIz[1], KOFF + t0, KOFF + t0 + GT)])
                    elif ci >= 17:
                        P.op('act', lambda E, src=src: E.activation(out=sig.ap, in_=src, func=AF.Sigmoid), reads=[BK(pb)], writes=[sig])
                    else:
                        c = ci - 13
                        P.op('dve', lambda E, src=src, c=c, t0=t0: E.tensor_tensor(out=uT.ap[:, c, 30 + t0:30 + t0 + GT], in0=src, in1=sig.ap, op=ALU.mult),
                             reads=[BK(pb), sig], writes=[(uT, c * UE + 30 + t0, c * UE + 30 + t0 + GT)])
                        if g == NG - 1:
                            P.op('dve', lambda E, src=src, c=c: E.tensor_tensor(out=ulast.ap[:, c, 0:30], in0=src[:, GT - 30:GT], in1=sig.ap[:, GT - 30:GT], op=ALU.mult),
                                 reads=[BK(pb), sig], writes=[ulast])
            def p1_tm(g):
                hT = hTs[g % 2]
                for tl in range(TPG):
                    tt = g * TPG + tl
                    for (pb, c0_, cw_) in [(5, 0, 512), (6, 512, 512), (7, 1024, 72)]:
                        for kc in range(8):
                            P.op('pe', lambda E, kc=kc, pb=pb, c0_=c0_, cw_=cw_, tl=tl: E.matmul(banks[pb][0:TP, 0:cw_], lhsT=hT.ap[:, kc, tl * TP:(tl + 1) * TP],
                                                                                           rhs=wtm.ap[:, kc, c0_:c0_ + cw_], start=(kc == 0), stop=(kc == 7)),
                                 reads=[hT, wtm], writes=[BK(pb)])
                    P.op('act', lambda E: E.activation(out=kout.ap[0:TP, :], in_=banks[5][0:TP, :], func=AF.Copy), reads=[BK(5)], writes=[kout])
                    P.dma('pool', o_k[tt * TP:(tt + 1) * TP, :], kout.ap[0:TP, :], reads=[kout], writes=[('o_k', j, tt)])
                    P.op('dve', lambda E: E.tensor_copy(out=vout.ap[0:TP, :], in_=banks[6][0:TP, :]), reads=[BK(6)], writes=[vout])
                    P.dma('pool', o_v[tt * TP:(tt + 1) * TP, :], vout.ap[0:TP, :], reads=[vout], writes=[('o_v', j, tt)])
                    kbv = tt if kind == 0 else 32
                    P.op('pool', lambda E, kbv=kbv: E.tensor_copy(out=Vb.ap[0:TP, kbv, :, 0:64], in_=vout.ap[0:TP, :].rearrange("p (h d) -> p h d", d=64)),
                         reads=[vout], writes=[(Vb, kbv * 520, kbv * 520 + 520)])
                    P.op('dve', lambda E: E.tensor_copy(out=kiw.ap[0:TP, :], in_=banks[7][0:TP, 0:72]), reads=[BK(7)], writes=[kiw])
                    P.dma('pool', o_ki[tt * TP:(tt + 1) * TP, :], kiw.ap[0:TP, 0:64], reads=[kiw], writes=[('o_ki', j, tt)])
                    P.op('dve', lambda E, tt=tt: E.tensor_scalar(out=Wt.ap[0:TP, tt, :], in0=kiw.ap[0:TP, 64:72], scalar1=float(1.0 / (8.0 * np.sqrt(8.0))), scalar2=None, op0=ALU.mult),
                         reads=[kiw], writes=[(Wt, tt * 8, tt * 8 + 8)])
            p1_pro(0)
            for g in range(NG):
                p1_fm(g)
                if g + 1 < NG:
                    p1_pro(g + 1)
                p1_tm(g)
            for c in range(4):
                P.op('pe', lambda E, c=c: E.transpose(banks[0][0:32, c * 128:(c + 1) * 128], ulast.ap[:, c, :], identf.ap), reads=[ulast, identf], writes=[BK(0)])
            P.op('act', lambda E: E.activation(out=cst.ap[0:32, :], in_=banks[0][0:32, :], func=AF.Copy), reads=[BK(0)], writes=[cst])
            P.dma('pool', o_c, cst.ap[0:30, :], reads=[cst], writes=[('o_c', j)])

            if 2 not in phases:
                return
            RM.reset()
            mixT = mk(RM, [8, T], BF16)
            RW.reset()
            uT = mk(RW, [4, UE], BF16)
            ysq = mk(RW, [GT], F32); mean_sb = mk(RW, [GT], F32); rstd_sb = mk(RW, [GT], F32); zt = mk(RW, [GT], F32)
            Dcv = mk(RW, [31, 128], BF16)
            cvb = 0
            for c in range(4):
                for jj in range(31):
                    P.op('dve', lambda E, c=c, jj=jj: E.tensor_scalar(out=Dcv.ap[:, jj, :], in0=identb.ap, scalar1=convwT.ap[:, c, jj:jj + 1], scalar2=None, op0=ALU.mult),
                         reads=[identb, convwT], writes=[(Dcv, jj * 128, jj * 128 + 128)])
                for g in range(NG):
                    pb = cvb % 3; cvb += 1
                    for jj in range(31):
                        P.op('pe', lambda E, c=c, jj=jj, pb=pb, g=g: E.matmul(banks[pb][:, 0:GT], lhsT=Dcv.ap[:, jj, :], rhs=uT.ap[:, c, g * GT + jj:g * GT + jj + GT],
                                                                          start=(jj == 0), stop=(jj == 30)),
                             reads=[Dcv, (uT, c * UE + g * GT, c * UE + g * GT + GT + 30)], writes=[BK(pb)])
                    P.op('act', lambda E, c=c, pb=pb, g=g: E.activation(out=mixT.ap[:, 4 + c, g * GT:(g + 1) * GT], in_=banks[pb][:, 0:GT], func=AF.Identity, bias=convbT.ap[:, c:c + 1]),
                         reads=[BK(pb), convbT], writes=[(mixT, (4 + c) * T + g * GT, (4 + c) * T + (g + 1) * GT)])
            for g in range(NG):
                for c in range(4):
                    mrange = (mixT, (4 + c) * T + g * GT, (4 + c) * T + (g + 1) * GT)
                    P.op('act', lambda E, c=c, g=g: E.activation(out=ysq.ap, in_=mixT.ap[:, 4 + c, g * GT:(g + 1) * GT], func=AF.Square), reads=[mrange], writes=[ysq])
                    P.op('pe', lambda E, c=c, g=g: E.matmul(banks[3][:, 0:GT], lhsT=onesb.ap, rhs=mixT.ap[:, 4 + c, g * GT:(g + 1) * GT], start=(c == 0), stop=(c == 3)),
                         reads=[onesb, mrange], writes=[BK(3)])
                    P.op('pe', lambda E, c=c: E.matmul(banks[4][:, 0:GT], lhsT=onesf.ap, rhs=ysq.ap, start=(c == 0), stop=(c == 3)),
                         reads=[onesf, ysq], writes=[BK(4)])
                P.op('act', lambda E: E.activation(out=mean_sb.ap, in_=banks[3][:, 0:GT], func=AF.Copy, scale=1.0 / 512), reads=[BK(3)], writes=[mean_sb])
                P.op('dve', lambda E: E.tensor_tensor(out=zt.ap, in0=mean_sb.ap, in1=mean_sb.ap, op=ALU.mult), reads=[mean_sb], writes=[zt])
                P.op('dve', lambda E: E.scalar_tensor_tensor(out=rstd_sb.ap, in0=banks[4][:, 0:GT], scalar=1.0 / 512, in1=zt.ap, op0=ALU.mult, op1=ALU.subtract),
                     reads=[BK(4), zt], writes=[rstd_sb])
                P.op('dve', lambda E: E.tensor_scalar(out=rstd_sb.ap, in0=rstd_sb.ap, scalar1=EPS, scalar2=None, op0=ALU.add), reads=[rstd_sb], writes=[rstd_sb])
                P.op('act', lambda E: E.activation(out=rstd_sb.ap, in_=rstd_sb.ap, func=AF.Sqrt), reads=[rstd_sb], writes=[rstd_sb])
                P.op('dve', lambda E: E.reciprocal(out=rstd_sb.ap, in_=rstd_sb.ap), reads=[rstd_sb], writes=[rstd_sb])
                for c in range(4):
                    mrange = (mixT, (4 + c) * T + g * GT, (4 + c) * T + (g + 1) * GT)
                    P.op('dve', lambda E, c=c, g=g: E.tensor_tensor(out=zt.ap, in0=mixT.ap[:, 4 + c, g * GT:(g + 1) * GT], in1=mean_sb.ap, op=ALU.subtract),
                         reads=[mrange, mean_sb], writes=[zt])
                    P.op('dve', lambda E: E.tensor_tensor(out=zt.ap, in0=zt.ap, in1=rstd_sb.ap, op=ALU.mult), reads=[zt, rstd_sb], writes=[zt])
                    P.op('act', lambda E, c=c, g=g: E.activation(out=mixT.ap[:, 4 + c, g * GT:(g + 1) * GT], in_=zt.ap, func=AF.Silu, scale=lngT.ap[:, c:c + 1], bias=lnbT.ap[:, c:c + 1]),
                         reads=[zt, lngT, lnbT], writes=[mrange])

            RW.reset()
            LMAX = NK
            NLS = 2 if kind == 0 else 1
            Ibs = [mk(RW, [LMAX], F32) for _ in range(NLS)]; Mbs = [mk(RW, [LMAX], BF16) for _ in range(NLS)]
            NMT = 2 if (kind == 0 and PIPE) else 1
            MTs = [mk(RW, [NKB, GT], BF16) for _ in range(NMT)]
            Rr = [mk(RW, [512], BF16) for _ in range(4)]
            SLOTS3 = [0, 1, 2]; SLOTS6 = [0, 1, 2, 3, 7, 4]
            NEB = 6
            Eb = [mk(RW, [GT], BF16) for _ in range(NEB)]; Pm = Eb
            Pm2 = [mk(RW, [GT], BF16) for _ in range(NEB)] if kind != 0 else Eb
            bcs = mk(RW, [GT], F32); Dg = mk(RW, [8, 128], BF16)
            NPIECE = NG
            wN = BIS_B / (2 ** NITER)
            DB = [3, 7]
            st_ = dict(att_rr=0, d_rr=0, e_rr=0)

            def gen_idx(qp, pr):
                MT = MTs[qp % NMT]
                tiles = list(range(pr, min(pr + NLS, TPG)))
                info = []
                for r in tiles:
                    i = qp * TPG + r
                    L = 128 * (i + 1) if kind == 0 else NKS
                    qs = i * TP
                    Ib = Ibs[r % NLS]; Mb = Mbs[r % NLS]
                    info.append((r, i, L, Ib, Mb))
                    for h in range(8):
                        P.op('dve', lambda E, h=h, i=i: E.tensor_scalar(out=Dg.ap[0:TP, h, 0:TP], in0=identb.ap[0:TP, 0:TP], scalar1=Wt.ap[0:TP, i, h:h + 1], scalar2=None, op0=ALU.mult),
                             reads=[identb, (Wt, i * 8, i * 8 + 8)], writes=[(Dg, h * 128, h * 128 + 128)])
                    nkp = (L + 511) // 512
                    for kp in range(nkp):
                        k0 = kp * 512
                        kw = min(512, L - k0)

                        def dmm(h):
                            pb = DB[st_['d_rr'] % 2]; st_['d_rr'] += 1
                            P.op('pe', lambda E, h=h, pb=pb, kw=kw, k0=k0, qs=qs: E.matmul(banks[pb][0:TP, 0:kw], lhsT=QIT.ap[:, h // 2, qs:qs + TP], rhs=KIz[h % 2].ap[:, k0:k0 + kw],
                                                                                 start=True, stop=True),
                                 reads=[(QIT, (h // 2) * T + qs, (h // 2) * T + qs + TP), (KIz[h % 2], k0, k0 + kw)], writes=[BK(pb)])
                            rb = Rr[h % 4]
                            if h % 2 == 0 or h == 7:
                                P.op('act', lambda E, pb=pb, rb=rb, kw=kw: E.activation(out=rb.ap[0:TP, 0:kw], in_=banks[pb][0:TP, 0:kw], func=AF.Relu), reads=[BK(pb)], writes=[rb])
                            else:
                                P.op('dve', lambda E, pb=pb, rb=rb, kw=kw: E.tensor_scalar(out=rb.ap[0:TP, 0:kw], in0=banks[pb][0:TP, 0:kw], scalar1=0.0, scalar2=None, op0=ALU.max), reads=[BK(pb)], writes=[rb])

                        def amm(h):
                            rb = Rr[h % 4]
                            P.op('pe', lambda E, h=h, rb=rb, kw=kw: E.matmul(banks[4][0:TP, 0:kw], lhsT=Dg.ap[0:TP, h, 0:TP], rhs=rb.ap[0:TP, 0:kw], start=(h == 0), stop=(h == 7)),
                                 reads=[(Dg, h * 128, h * 128 + 128), rb], writes=[BK(4)])
                        dmm(0)
                        for h in range(8):
                            if h + 1 < 8:
                                dmm(h + 1)
                            amm(h)
                            if h % 4 == 3:
                                yield
                        P.op('dve', lambda E, k0=k0, kw=kw, Ib=Ib: E.tensor_copy(out=Ib.ap[0:TP, k0:k0 + kw], in_=banks[4][0:TP, 0:kw]), reads=[BK(4)], writes=[(Ib, k0, k0 + kw)])
                    if kind == 0:
                        P.op('dve', lambda E, L=L, Ib=Ib: E.memset(Ib.ap[0:64, L - 64:L], -1e30), writes=[(Ib, L - 64, L)])
                    yield
                bis = [t_ for t_ in info if t_[2] > 256]
                for (r, i, L, Ib, Mb) in bis:
                    col = r % NLS
                    P.op('dve', lambda E, col=col: E.memset(negmid.ap[0:TP, col:col + 1], 0.0), writes=[('negmid', col)])
                if kind != 0 and bis:
                    (r, i, L, Ib, Mb) = bis[0]
                    LA = (L // 2) // 64 * 64
                    P.op('dve', lambda E: E.memset(negmid.ap[0:TP, 1:2], 0.0), writes=[('negmid', 1)])
                    for it in range(NITER):
                        wk = BIS_B / (2 ** it)
                        P.op('act', lambda E: E.activation(out=Mb.ap[0:TP, 0:LA], in_=Ib.ap[0:TP, 0:LA], func=AF.Sign, bias=negmid.ap[0:TP, 0:1], scale=1.0, accum_out=sacc.ap[0:TP, 0:1]),
                             reads=[(Ib, 0, LA), ('negmid', 0)], writes=[(Mb, 0, LA), ('sacc', 0)])
                        P.op('dve', lambda E: E.tensor_scalar(out=Mb.ap[0:TP, LA:L], in0=Ib.ap[0:TP, LA:L], scalar1=negmid.ap[0:TP, 1:2], scalar2=0.0, op0=ALU.is_ge, op1=ALU.add,
                                                              accum_out=sacc.ap[0:TP, 1:2]),
                             reads=[(Ib, LA, L), ('negmid', 1)], writes=[(Mb, LA, L), ('sacc', 1)])
                        P.op('dve', lambda E: E.scalar_tensor_tensor(out=dd.ap[0:TP, 1:2], in0=sacc.ap[0:TP, 1:2], scalar=2.0, in1=sacc.ap[0:TP, 0:1], op0=ALU.mult, op1=ALU.add),
                             reads=[('sacc', 0), ('sacc', 1)], writes=[('dd', 1)])
                        P.op('dve', lambda E: E.tensor_scalar(out=dd.ap[0:TP, 0:1], in0=dd.ap[0:TP, 1:2], scalar1=float(512 - LA), scalar2=0.5, op0=ALU.is_ge, op1=ALU.subtract),
                             reads=[('dd', 1)], writes=[('dd', 0)])
                        P.op('dve', lambda E, wk=wk: E.scalar_tensor_tensor(out=negmid.ap[0:TP, 0:1], in0=dd.ap[0:TP, 0:1], scalar=-wk, in1=negmid.ap[0:TP, 0:1], op0=ALU.mult, op1=ALU.add),
                             reads=[('dd', 0), ('negmid', 0)], writes=[('negmid', 0)])
                        P.op('dve', lambda E, wk=wk: E.scalar_tensor_tensor(out=negmid.ap[0:TP, 1:2], in0=dd.ap[0:TP, 0:1], scalar=wk, in1=negmid.ap[0:TP, 1:2], op0=ALU.mult, op1=ALU.add),
                             reads=[('dd', 0), ('negmid', 1)], writes=[('negmid', 1)])
                        yield
                    bis = []
                for it in range(NITER if bis else 0):
                    wk = BIS_B / (2 ** it)
                    for (r, i, L, Ib, Mb) in bis:
                        col = r % NLS
                        if col == 0:
                            P.op('act', lambda E, L=L, Ib=Ib, Mb=Mb, col=col: E.activation(out=Mb.ap[0:TP, 0:L], in_=Ib.ap[0:TP, 0:L], func=AF.Sign, bias=negmid.ap[0:TP, col:col + 1], scale=1.0,
                                                                                       accum_out=sacc.ap[0:TP, col:col + 1]),
                                 reads=[(Ib, 0, L), ('negmid', col)], writes=[(Mb, 0, L), ('sacc', col)])
                        else:
                            P.op('dve', lambda E, L=L, Ib=Ib, Mb=Mb, col=col: E.tensor_scalar(out=Mb.ap[0:TP, 0:L], in0=Ib.ap[0:TP, 0:L], scalar1=negmid.ap[0:TP, col:col + 1], scalar2=0.0,
                                                                                          op0=ALU.is_ge, op1=ALU.add, accum_out=sacc.ap[0:TP, col:col + 1]),
                                 reads=[(Ib, 0, L), ('negmid', col)], writes=[(Mb, 0, L), ('sacc', col)])
                    for (r, i, L, Ib, Mb) in bis:
                        col = r % NLS
                        Cc = float(512 - L) if col == 0 else 256.0
                        sg = -wk if col == 0 else wk
                        P.op('dve', lambda E, Cc=Cc, col=col: E.tensor_scalar(out=dd.ap[0:TP, col:col + 1], in0=sacc.ap[0:TP, col:col + 1], scalar1=Cc, scalar2=0.5, op0=ALU.is_ge, op1=ALU.subtract),
                             reads=[('sacc', col)], writes=[('dd', col)])
                        P.op('dve', lambda E, sg=sg, col=col: E.scalar_tensor_tensor(out=negmid.ap[0:TP, col:col + 1], in0=dd.ap[0:TP, col:col + 1], scalar=sg, in1=negmid.ap[0:TP, col:col + 1],
                                                                                  op0=ALU.mult, op1=ALU.add),
                             reads=[('dd', col), ('negmid', col)], writes=[('negmid', col)])
                    for _y in range(BIS_YIELDS):
                        yield
                for (r, i, L, Ib, Mb) in info:
                    col = r % NLS
                    if L > 256:
                        sgn = -1.0 if col == 0 else 1.0
                        P.op('dve', lambda E, col=col, sgn=sgn: E.tensor_scalar(out=thr.ap[0:TP, col:col + 1], in0=negmid.ap[0:TP, col:col + 1], scalar1=sgn, scalar2=-wN, op0=ALU.mult, op1=ALU.add),
                             reads=[('negmid', col)], writes=[('thr', col)])
                    else:
                        P.op('dve', lambda E, col=col: E.memset(thr.ap[0:TP, col:col + 1], -1e29), writes=[('thr', col)])
                    P.op('dve', (lambda E, L=L, Ib=Ib, Mb=Mb, col=col: E.tensor_scalar(out=Mb.ap[0:TP, 0:L], in0=Ib.ap[0:TP, 0:L], scalar1=thr.ap[0:TP, col:col + 1], scalar2=-30000.0, op0=ALU.is_lt, op1=ALU.mult)) if kind == 0 else
                         (lambda E, L=L, Ib=Ib, Mb=Mb, col=col: E.tensor_scalar(out=Mb.ap[0:TP, 0:L], in0=Ib.ap[0:TP, 0:L], scalar1=thr.ap[0:TP, col:col + 1], scalar2=None, op0=ALU.is_ge)),
                         reads=[(Ib, 0, L), ('thr', col)], writes=[(Mb, 0, L)])
                    nkb = (L + 127) // 128
                    kb = 0
                    half = 0
                    while kb < nkb:
                        nb = min(4, nkb - kb)
                        full = all(min(128, L - 128 * (kb + s_)) == 128 for s_ in range(nb))
                        if not full and nb > 1:
                            nb -= 1
                        base = half * 512
                        for s_ in range(nb):
                            kw_ = min(128, L - 128 * (kb + s_))
                            P.op('pe', lambda E, s_=s_, kw_=kw_, kb=kb, base=base, Mb=Mb: E.transpose(bkb(4)[0:kw_, base + s_ * 128: base + s_ * 128 + TP], Mb.ap[0:TP, (kb + s_) * 128:(kb + s_) * 128 + kw_], identb.ap[0:TP, 0:TP]),
                                 reads=[(Mb, (kb + s_) * 128, (kb + s_) * 128 + kw_), identb], writes=[BK(4)])
                        kw_ = min(128, L - 128 * kb) if nb == 1 else 128
                        srcv = bkb(4)[0:kw_, base:base + nb * 128].rearrange("p (a b) -> p a b", b=128)[:, :, 0:TP]
                        P.op('act', lambda E, kb=kb, nb=nb, kw_=kw_, srcv=srcv, r=r, MT=MT: E.activation(out=MT.ap[0:kw_, kb:kb + nb, r * TP:(r + 1) * TP], in_=srcv, func=AF.Copy),
                             reads=[BK(4)], writes=[(MT, kb * GT, (kb + nb) * GT)])
                        kb += nb
                        half ^= 1
                        yield

            def gen_att(qp):
                MT = MTs[qp % NMT]
                SL = SLOTS6 if (kind != 0 or qp == NPIECE - 1) else SLOTS3
                NSL_ = len(SL)
                SK_ = NSL_ - 1
                skey = lambda sl: BK(SL[sl])
                sview = lambda sl, rows, n: banks[SL[sl]][0:rows, 0:n]
                st_['att_rr'] = 0
                q0 = qp * GT
                kb_last = (qp * 4 + 3) if kind == 0 else (NKB - 1)
                tb0 = qp * 4 if kind == 0 else 32
                blocks = [(h, kb) for h in range(8) for kb in range(kb_last + 1)]

                def geom(kb):
                    kw_ = min(128, NK - 128 * kb)
                    if kind == 0:
                        jd = kb - 4 * qp
                        r0 = max(jd, 0)
                        return kw_, r0, r0 * 128, jd >= 0, 128
                    return kw_, 0, 0, (kb == NKB - 1), 64

                def front(h, kb):
                    kw_, r0, c0, diag, TPd = geom(kb)
                    hb = (h % 2) * 64; hc = h // 2
                    N = GT - c0
                    sb_ = st_['att_rr'] % NSL_; st_['att_rr'] += 1
                    P.op('pe', lambda E: E.matmul(sview(sb_, kw_, N), lhsT=KA.ap[:, hc, kb * 128:kb * 128 + kw_], rhs=QA.ap[:, h, q0 + c0:q0 + GT], start=True, stop=(kind != 0 and not diag)),
                         reads=[(KA, hc * NK + kb * 128, hc * NK + kb * 128 + kw_), (QA, h * T + q0, h * T + q0 + GT)], writes=[skey(sb_)])
                    if diag:
                        P.op('pe', lambda E: E.matmul(sview(sb_, kw_, TPd), lhsT=identb.ap[0:kw_, 0:kw_], rhs=cdiag.ap[0:kw_, h, 0:TPd], start=False, stop=(kind != 0)),
                             reads=[identb, cdiag], writes=[skey(sb_)])
                    if kind == 0:
                        P.op('pe', lambda E: E.matmul(sview(sb_, kw_, N), lhsT=identb.ap[0:kw_, 0:kw_], rhs=MT.ap[0:kw_, kb, c0:GT], start=False, stop=True),
                             reads=[identb, (MT, kb * GT, (kb + 1) * GT)], writes=[skey(sb_)])
                    return sb_

                def back(h, kb, sb_):
                    kw_, r0, c0, diag, TPd = geom(kb)
                    hb = (h % 2) * 64; hc = h // 2
                    ob = 5 + (h % 2)
                    N = GT - c0
                    eb = Eb[st_['e_rr'] % NEB]; pm = Pm[st_['e_rr'] % NEB]; st_['e_rr'] += 1
                    if kind == 0 and h < 2:
                        for r in range(r0, 4):
                            dl = kb - (tb0 + r) + 32
                            a0 = r * 128 - c0
                            P.op('act', lambda E, a0=a0, dl=dl: E.activation(out=eb.ap[0:kw_, a0:a0 + 128], in_=banks[SL[sb_]][0:kw_, a0:a0 + 128], func=AF.Exp,
                                                                             bias=btab.ap[0:kw_, h, dl:dl + 1], scale=1.0),
                                 reads=[skey(sb_), btab], writes=[eb])
                    else:
                        dl = kb - tb0 + 32
                        P.op('act', lambda E: E.activation(out=eb.ap[0:kw_, 0:N], in_=sview(sb_, kw_, N), func=AF.Exp, bias=btab.ap[0:kw_, h, dl:dl + 1], scale=1.0),
                             reads=[skey(sb_), btab], writes=[eb])
                    pv_in = eb
                    if kind != 0:
                        pv_in = Pm2[st_['e_rr'] % NEB]
                        P.op('dve', lambda E: E.tensor_tensor(out=pv_in.ap[0:kw_, 0:N], in0=eb.ap[0:kw_, 0:N], in1=MT.ap[0:kw_, kb, c0:GT], op=ALU.mult),
                             reads=[eb, (MT, kb * GT, (kb + 1) * GT)], writes=[pv_in])
                    P.op('pe', lambda E: E.matmul(banks[ob][0:65, c0:GT], lhsT=Vb.ap[0:kw_, kb, h, :], rhs=pv_in.ap[0:kw_, 0:N], start=(kb == 0), stop=(kb == kb_last)),
                         reads=[(Vb, kb * 520, kb * 520 + 520), pv_in], writes=[BK(ob)])
                    if kb == kb_last:
                        P.op('dve', lambda E: E.tensor_scalar(out=bcs.ap[64:65, 0:GT], in0=banks[ob][64:65, 0:GT], scalar1=1e-30, scalar2=None, op0=ALU.max), reads=[BK(ob)], writes=[bcs])
                        P.op('dve', lambda E: E.reciprocal(out=bcs.ap[64:65, 0:GT], in_=bcs.ap[64:65, 0:GT]), reads=[bcs], writes=[bcs])

                        def stage2():
                            nb_ = st_['att_rr'] % NSL_; st_['att_rr'] += 1
                            P.op('pe', lambda E: E.matmul(sview(nb_, 64, GT), lhsT=onesf.ap[64:65, 0:64], rhs=bcs.ap[64:65, 0:GT], start=True, stop=True), reads=[onesf, bcs], writes=[skey(nb_)])
                            P.op('act', lambda E: E.activation(out=bcs.ap[0:64, :], in_=sview(nb_, 64, GT), func=AF.Copy), reads=[skey(nb_)], writes=[bcs])
                            P.op('dve', lambda E: E.tensor_tensor(out=mixT.ap[hb:hb + 64, hc, q0:q0 + GT], in0=banks[ob][0:64, 0:GT], in1=bcs.ap[0:64, :], op=ALU.mult),
                                 reads=[BK(ob), bcs], writes=[(mixT, hc * T + q0, hc * T + q0 + GT)])
                        norm_q.append([NORM_DELAY, stage2])

                norm_q = []
                NORM_DELAY = 3
                if False:
                    P.op('dve', lambda E: E.memset(dd.ap[0:1, 0:1], 0.0), reads=[BK(0), BK(1), BK(2)], writes=[('pss', sl) for sl in range(NSL)] + [BK(0), BK(1), BK(2)])
                pend = []
                nf = 0
                for n in range(len(blocks)):
                    while nf < len(blocks) and nf <= n + SK_ - 1:
                        h_, kb_ = blocks[nf]
                        pend.append((h_, kb_, front(h_, kb_)))
                        nf += 1
                    back(*pend.pop(0))
                    for it_ in norm_q:
                        it_[0] -= 1
                    if norm_q and norm_q[0][0] <= 0:
                        norm_q.pop(0)[1]()
                    yield
                for it_ in list(norm_q):
                    it_[1]()
                norm_q.clear()
                if False:
                    P.op('dve', lambda E: E.memset(dd.ap[0:1, 0:1], 0.0), reads=[('pss', sl) for sl in range(NSL)], writes=[('pss', sl) for sl in range(NSL)] + [BK(0), BK(1), BK(2)])
                yield

            def run_streams(A, B, na, nb):
                ia = ib = 0
                a_done = A is None
                b_done = B is None
                while not (a_done and b_done):
                    pick_a = (not a_done) and (b_done or ia * max(nb, 1) <= ib * max(na, 1))
                    if pick_a:
                        try:
                            next(A); ia += 1
                        except StopIteration:
                            a_done = True
                    else:
                        try:
                            next(B); ib += 1
                        except StopIteration:
                            b_done = True

            def chain(*gens):
                for g_ in gens:
                    yield from g_

            def count_steps(make):
                return None

            pairs = list(range(0, TPG, NLS))
            if not PIPE:
                for qp in range(NPIECE):
                    run_streams(chain(*[gen_idx(qp, pr) for pr in pairs]), None, 1, 1)
                    run_streams(gen_att(qp), None, 1, 1)
            else:
                if kind == 0:
                    run_streams(chain(*[gen_idx(0, pr) for pr in pairs]), None, 1, 1)
                else:
                    load_kidx()
                    run_streams(chain(*[gen_idx(0, pr) for pr in pairs]), gen_load(), 60, 9)
                for qp in range(NPIECE):
                    kbl = (qp * 4 + 4) if kind == 0 else NKB
                    na = 8 * kbl + 1
                    if qp + 1 < NPIECE:
                        nb = 0
                        for r in range(TPG):
                            L_ = 128 * ((qp + 1) * TPG + r + 1)
                            nb += ((L_ + 511) // 512) * 2 + 1 + ((L_ + 127) // 128 + 3) // 4
                        nb += NITER * BIS_YIELDS * len(pairs)
                        run_streams(gen_att(qp), chain(*[gen_idx(qp + 1, pr) for pr in pairs]), na, nb)
                    else:
                        run_streams(gen_att(qp), None, na, 1)

            if 3 not in phases:
                return
            RU.reset(); RW.reset()
            fgbc = mk(RU, [D], F32); g1bc = mk(RU, [D], F32); g2bc = mk(RU, [D], F32)
            P.dma('sp', fgbc.ap, fgbc_d, writes=[fgbc])
            bcast_vec(g1T, g1bc)
            bcast_vec(g2T, g2bc)
            wo = mk(RU, [8, D], BF16)
            xg = [mk(RU, [D], F32) for _ in range(TPG)]
            h2T = mk(RU, [8, GT], BF16)
            hid = mk(RU, [32, GT], BF16)
            f1r = [mk(RW, [4, 8, 128], BF16) for _ in range(2)]
            f2r = [mk(RW, [8, 512], BF16) for _ in range(2)]
            xn2 = mk(RW, [D], F32); tmp5 = mk(RW, [512], F32); rl = mk(RW, [GT], F32); junk3 = mk(RW, [D], BF16)
            P.dma('sp', wo.ap, wsc_out.rearrange("(kc p) n -> p kc n", p=128), reads=WOUT, writes=[wo])
            f1_rr = 0; f2_rr = 0; f1b_rr = 0
            for g in range(NG):
                for tl in range(TPG):
                    tt = g * TPG + tl
                    xb = xg[tl]
                    P.dma('sp', xb.ap[0:TP, :], x_d[tt * TP:(tt + 1) * TP, :], writes=[xb])
                    for half in range(2):
                        for kc in range(8):
                            P.op('pe', lambda E, kc=kc, half=half, tt=tt: E.matmul(banks[half][0:TP, :], lhsT=mixT.ap[:, kc, tt * TP:(tt + 1) * TP], rhs=wo.ap[:, kc, half * 512:(half + 1) * 512],
                                                                             start=(kc == 0), stop=(kc == 7)),
                                 reads=[(mixT, kc * T + tt * TP, kc * T + (tt + 1) * TP), wo], writes=[BK(half)])
                        P.op('dve', lambda E, half=half: E.tensor_tensor(out=tmp5.ap[0:TP, :], in0=banks[half][0:TP, :], in1=g1bc.ap[0:TP, half * 512:(half + 1) * 512], op=ALU.mult),
                             reads=[BK(half), g1bc], writes=[tmp5])
                        P.op('dve', lambda E, half=half, xb=xb: E.tensor_tensor(out=xb.ap[0:TP, half * 512:(half + 1) * 512], in0=xb.ap[0:TP, half * 512:(half + 1) * 512], in1=tmp5.ap[0:TP, :], op=ALU.add),
                             reads=[tmp5, xb], writes=[xb])
                    P.op('act', lambda E, xb=xb, tl=tl: E.activation(out=junk3.ap[0:TP, :], in_=xb.ap[0:TP, :], func=AF.Square, accum_out=ss.ap[0:TP, tl:tl + 1]),
                         reads=[xb], writes=[junk3, (ss, tl, tl + 1)])
                    P.op('dve', lambda E, tl=tl: E.tensor_scalar(out=rs.ap[0:TP, tl:tl + 1], in0=ss.ap[0:TP, tl:tl + 1], scalar1=1.0 / D, scalar2=EPS, op0=ALU.mult, op1=ALU.add),
                         reads=[(ss, tl, tl + 1)], writes=[(rs, tl, tl + 1)])
                    P.op('act', lambda E, tl=tl: E.activation(out=rs.ap[0:TP, tl:tl + 1], in_=rs.ap[0:TP, tl:tl + 1], func=AF.Sqrt), reads=[(rs, tl, tl + 1)], writes=[(rs, tl, tl + 1)])
                    P.op('dve', lambda E, tl=tl: E.reciprocal(out=rs.ap[0:TP, tl:tl + 1], in_=rs.ap[0:TP, tl:tl + 1]), reads=[(rs, tl, tl + 1)], writes=[(rs, tl, tl + 1)])
                    P.op('dve', lambda E, xb=xb, tl=tl: E.tensor_scalar(out=xn2.ap[0:TP, :], in0=xb.ap[0:TP, :], scalar1=rs.ap[0:TP, tl:tl + 1], scalar2=None, op0=ALU.mult),
                         reads=[xb, (rs, tl, tl + 1)], writes=[xn2])
                    for kc in range(8):
                        bk = 2 + kc // 4
                        P.op('pe', lambda E, kc=kc, bk=bk: E.transpose(banks[bk][:, (kc % 4) * 128:(kc % 4) * 128 + TP], xn2.ap[0:TP, kc * 128:(kc + 1) * 128], identf.ap[0:TP, 0:TP]),
                             reads=[xn2, identf], writes=[BK(bk)])
                    for kc in range(8):
                        bk = 2 + kc // 4
                        P.op('act', lambda E, kc=kc, bk=bk, tl=tl: E.activation(out=h2T.ap[:, kc, tl * TP:(tl + 1) * TP], in_=banks[bk][:, (kc % 4) * 128:(kc % 4) * 128 + TP],
                                                                        func=AF.Identity, scale=G2T.ap[:, kc:kc + 1], bias=S2T.ap[:, kc:kc + 1]),
                             reads=[BK(bk), G2T, S2T], writes=[(h2T, kc * GT + tl * TP, kc * GT + (tl + 1) * TP)])
                for j4 in range(8):
                    fb = f1r[f1_rr % 2]; f1_rr += 1
                    P.dma('sp', fb.ap, wsc_ff1[j4 * 4:(j4 + 1) * 4].rearrange("j p kc n -> p j kc n"), reads=WFF1, writes=[fb])
                    for jj in range(4):
                        jh = j4 * 4 + jj
                        pb = f1b_rr % 4; f1b_rr += 1
                        for kc in range(8):
                            P.op('pe', lambda E, fb=fb, jj=jj, kc=kc, pb=pb: E.matmul(banks[pb][:, 0:GT], lhsT=fb.ap[:, jj, kc, :], rhs=h2T.ap[:, kc, :], start=(kc == 0), stop=(kc == 7)),
                                 reads=[fb, h2T], writes=[BK(pb)])
                        P.op('act', lambda E, pb=pb: E.activation(out=rl.ap, in_=banks[pb][:, 0:GT], func=AF.Relu), reads=[BK(pb)], writes=[rl])
                        P.op('dve', lambda E, jh=jh: E.tensor_tensor(out=hid.ap[:, jh, :], in0=rl.ap, in1=rl.ap, op=ALU.mult), reads=[rl], writes=[(hid, jh * GT, (jh + 1) * GT)])
                for half in range(2):
                    for j8 in range(4):
                        fb = f2r[f2_rr % 2]; f2_rr += 1
                        P.dma('sp', fb.ap, wsc_ff2[j8 * 1024:(j8 + 1) * 1024, half * 512:(half + 1) * 512].rearrange("(jj p) n -> p jj n", p=128), reads=WFF2, writes=[fb])
                        for jj in range(8):
                            jh = j8 * 8 + jj
                            for tl in range(TPG):
                                P.op('pe', lambda E, fb=fb, jj=jj, jh=jh, tl=tl: E.matmul(banks[4 + tl][0:TP, :], lhsT=hid.ap[:, jh, tl * TP:(tl + 1) * TP], rhs=fb.ap[:, jj, :],
                                                                                     start=(jh == 0), stop=(jh == 31)),
                                     reads=[(hid, jh * GT, (jh + 1) * GT), fb], writes=[BK(4 + tl)])
                    for tl in range(TPG):
                        xb = xg[tl]
                        P.op('dve', lambda E, half=half, tl=tl: E.tensor_tensor(out=tmp5.ap[0:TP, :], in0=banks[4 + tl][0:TP, :], in1=g2bc.ap[0:TP, half * 512:(half + 1) * 512], op=ALU.mult),
                             reads=[BK(4 + tl), g2bc], writes=[tmp5])
                        P.op('dve', lambda E, half=half, xb=xb: E.tensor_tensor(out=xb.ap[0:TP, half * 512:(half + 1) * 512], in0=xb.ap[0:TP, half * 512:(half + 1) * 512], in1=tmp5.ap[0:TP, :], op=ALU.add),
                             reads=[tmp5, xb], writes=[xb])
                for tl in range(TPG):
                    tt = g * TPG + tl
                    xb = xg[tl]
                    P.op('act', lambda E, xb=xb, tl=tl: E.activation(out=junk3.ap[0:TP, :], in_=xb.ap[0:TP, :], func=AF.Square, accum_out=ss.ap[0:TP, 8 + tl:9 + tl]),
                         reads=[xb], writes=[junk3, (ss, 8 + tl, 9 + tl)])
                    P.op('dve', lambda E, tl=tl: E.tensor_scalar(out=rs.ap[0:TP, 8 + tl:9 + tl], in0=ss.ap[0:TP, 8 + tl:9 + tl], scalar1=1.0 / D, scalar2=EPS, op0=ALU.mult, op1=ALU.add),
                         reads=[(ss, 8 + tl, 9 + tl)], writes=[(rs, 8 + tl, 9 + tl)])
                    P.op('act', lambda E, tl=tl: E.activation(out=rs.ap[0:TP, 8 + tl:9 + tl], in_=rs.ap[0:TP, 8 + tl:9 + tl], func=AF.Sqrt), reads=[(rs, 8 + tl, 9 + tl)], writes=[(rs, 8 + tl, 9 + tl)])
                    P.op('dve', lambda E, tl=tl: E.reciprocal(out=rs.ap[0:TP, 8 + tl:9 + tl], in_=rs.ap[0:TP, 8 + tl:9 + tl]), reads=[(rs, 8 + tl, 9 + tl)], writes=[(rs, 8 + tl, 9 + tl)])
                    P.op('dve', lambda E, xb=xb, tl=tl: E.scalar_tensor_tensor(out=xb.ap[0:TP, :], in0=xb.ap[0:TP, :], scalar=rs.ap[0:TP, 8 + tl:9 + tl], in1=fgbc.ap[0:TP, :], op0=ALU.mult, op1=ALU.mult),
                         reads=[xb, (rs, 8 + tl, 9 + tl), fgbc], writes=[xb])
                    P.dma('pool', o_y[tt * TP:(tt + 1) * TP, :], xb.ap[0:TP, :], reads=[xb], writes=[('o_y', j, tt)])

        for U in units:
            do_unit(U)
        P.finish()
        print("instructions:", P.nins, {e: len(v) for e, v in P.ops.items()})
        P.run_block()
    return nc


def _consts():
    bf = ml_dtypes.bfloat16
    sl = np.array([2.0 ** (-(h + 1)) for h in range(8)], np.float64)
    s_loc = np.arange(128)[:, None, None]
    t_loc = np.arange(128)[None, None, :]
    cdiag = (-2.0 * sl[None, :, None] * np.maximum(s_loc - t_loc, 0)).astype(np.float32)
    out = {"c_cdiag": cdiag.astype(bf)}
    s_loc1 = np.arange(128)[:, None, None].astype(np.float64)
    dl = (np.arange(36) - 32)[None, None, :].astype(np.float64)
    out["c_btab"] = (sl[None, :, None] * (s_loc1 + 128.0 * dl)).astype(np.float32)
    return out


_NC_CACHE = {}


def kernel(x_prompt, x_sample, c_prompt, c_sample, cache_k, cache_v, cache_kidx, state_conv,
           w_ada, b_ada, norm1_g, w_in, conv_w, conv_b, conv_ln_g, conv_ln_b, w_out, norm2_g,
           w_ff1, w_ff2, final_g):
    f = lambda a: np.ascontiguousarray(np.asarray(a, dtype=np.float32))
    x_prompt, x_sample, c_prompt, c_sample = f(x_prompt), f(x_sample), f(c_prompt), f(c_sample)
    cache_k, cache_v, cache_kidx, state_conv = f(cache_k)[0], f(cache_v)[0], f(cache_kidx)[0], f(state_conv)[0]
    if 'nc' not in _NC_CACHE:
        _NC_CACHE['nc'] = build()
    nc = _NC_CACHE['nc']
    consts = _consts()
    vecT = lambda v, n: np.ascontiguousarray(f(v).reshape(n, 128).T)
    shared = {
        "w_ada": f(w_ada)[0], "b_adaT": vecT(f(b_ada)[0], 48), "n1gT": vecT(f(norm1_g)[0], 8), "n2gT": vecT(f(norm2_g)[0], 8),
        "fgbc": np.ascontiguousarray(np.broadcast_to(f(final_g)[None, :], (128, D))),
        "w_in": f(w_in)[0], "w_out": f(w_out)[0], "w_ff1": f(w_ff1)[0], "w_ff2": f(w_ff2)[0],
        "convwT": np.ascontiguousarray(f(conv_w)[0].reshape(31, 4, 128).transpose(2, 1, 0)),
        "convbT": vecT(f(conv_b)[0], 4), "lngT": vecT(f(conv_ln_g)[0], 4), "lnbT": vecT(f(conv_ln_b)[0], 4),
    }
    shared.update(consts)
    in_maps = []
    for c in range(8):
        sl_ = slice(2 * c, 2 * c + 2)
        cc = np.concatenate([c_prompt[sl_], c_sample[sl_]], axis=0)
        m = dict(shared)
        m["xp"] = np.ascontiguousarray(x_prompt[sl_])
        m["xs"] = np.ascontiguousarray(x_sample[sl_])
        m["cT"] = np.ascontiguousarray(cc.reshape(4, 8, 128).transpose(2, 1, 0))
        m["ckT"] = np.ascontiguousarray(cache_k[sl_].transpose(0, 2, 3, 1))
        m["cv"] = np.ascontiguousarray(cache_v[sl_].reshape(2, PAST, 512))
        m["ckiT"] = np.ascontiguousarray(cache_kidx[sl_].transpose(0, 2, 1))
        m["scT"] = np.ascontiguousarray(state_conv[sl_].transpose(0, 2, 1))
        in_maps.append(m)
    res = run_bass_kernel_spmd(nc, in_maps, core_ids=list(range(8)))
    R = res.results
    cat = lambda k: np.concatenate([r[k] for r in R], axis=0)
    y_prompt = cat("yp"); y_sample = cat("ys")
    k_prompt = cat("kp").reshape(1, 16, SEQ, 8, 64); v_prompt = cat("vp").reshape(1, 16, SEQ, 8, 64)
    kidx_prompt = cat("kip").reshape(1, 16, SEQ, 64); conv_prompt = cat("cp").reshape(1, 16, 30, 512)
    k_sample = cat("ks").reshape(1, 16, DSEQ, 8, 64); v_sample = cat("vs").reshape(1, 16, DSEQ, 8, 64)
    kidx_sample = cat("kis").reshape(1, 16, DSEQ, 64); conv_sample = cat("cs").reshape(1, 16, 30, 512)
    return (y_prompt, y_sample, k_prompt, v_prompt, kidx_prompt, conv_prompt, k_sample, v_sample, kidx_sample, conv_sample)
```

```python
import numpy as np
import ml_dtypes
import concourse.bass as bass
import concourse.mybir as mybir
from concourse.bass_utils import run_bass_kernel_spmd
from contextlib import ExitStack

F32 = mybir.dt.float32
BF16 = mybir.dt.bfloat16
AF = mybir.ActivationFunctionType
ALU = mybir.AluOpType

ENGS = ['pe', 'act', 'dve', 'pool', 'sp']
EPOCH = 12000
NDMA_SEM = 22
BLK = 256

D = 1024
SEQ = 2048
DSEQ = 64
PAST = 4096
NKS = PAST + DSEQ
PROJ = 3144
EPS = 1e-6
NITER = 18
PIPE = True
BIS_YIELDS = 5
BIS_B = 8.0


class Buf:
    def __init__(self, ap, off, nbytes, esz):
        self.ap, self.off, self.nbytes, self.esz = ap, off, nbytes, esz

    def keys(self, lo=None, hi=None):
        if lo is None:
            a, b = self.off, self.off + self.nbytes
        else:
            a, b = self.off + lo * self.esz, self.off + hi * self.esz
        return [('sb', i) for i in range(a // BLK, (b - 1) // BLK + 1)]


def _expand(items):
    out = []
    for it in items:
        if isinstance(it, Buf):
            out.extend(it.keys())
        elif isinstance(it, tuple) and len(it) == 3 and isinstance(it[0], Buf):
            out.extend(it[0].keys(it[1], it[2]))
        else:
            out.append(it)
    return out


class Prog:
    def __init__(self, nc, stack):
        self.nc = nc
        self.stack = stack
        self.ops = {e: [] for e in ENGS}
        self.cnt = {e: 0 for e in ENGS}
        self.sems = {e: [] for e in ENGS}
        self.dma_sems = [stack.enter_context(nc.semaphore(f"dq{i}")) for i in range(NDMA_SEM)]
        self.dma_val = [0] * NDMA_SEM
        self.dma_rr = 0
        self.dma_rr_pool = 0
        self.waited = {e: {} for e in ENGS}
        self.last_w = {}
        self.readers = {}
        self.nins = 0

    def _sem(self, e, ep):
        while len(self.sems[e]) <= ep:
            self.sems[e].append(self.stack.enter_context(self.nc.semaphore(f"s_{e}{len(self.sems[e])}")))
        return self.sems[e][ep]

    def _deps(self, eng, reads, writes, is_dma):
        deps = set()
        for r in reads:
            t = self.last_w.get(r)
            if t is not None:
                deps.add(t)
        skip_same = (eng == 'pe') and not is_dma
        for w in writes:
            t = self.last_w.get(w)
            if t is not None and not (skip_same and t[0] == 'e' and t[1] == eng):
                deps.add(t)
            for t in self.readers.get(w, ()):
                if not (skip_same and t[0] == 'e' and t[1] == eng):
                    deps.add(t)
        return deps

    def _emit_waits(self, eng, deps):
        best = {}
        for t in deps:
            key = (t[0], t[1], t[2])
            if best.get(key, 0) < t[3]:
                best[key] = t[3]
        for key, val in best.items():
            if self.waited[eng].get(key, 0) >= val:
                continue
            self.waited[eng][key] = val
            sem = self._sem(key[1], key[2]) if key[0] == 'e' else self.dma_sems[key[1]]
            self.ops[eng].append(lambda E, sem=sem, val=val: E.wait_ge(sem, val))

    def _commit(self, tok, reads, writes):
        for r in reads:
            self.readers.setdefault(r, []).append(tok)
        for w in writes:
            self.last_w[w] = tok
            self.readers[w] = []

    def op(self, eng, fn, reads=(), writes=()):
        reads, writes = _expand(reads), _expand(writes)
        self._emit_waits(eng, self._deps(eng, reads, writes, False))
        n = self.cnt[eng]
        ep, idx = divmod(n, EPOCH)
        self.cnt[eng] = n + 1
        sem = self._sem(eng, ep)
        self.ops[eng].append(lambda E, fn=fn, sem=sem: fn(E).then_inc(sem, 1))
        tok = ('e', eng, ep, idx + 1)
        self._commit(tok, reads, writes)
        self.nins += 1
        return tok

    def dma(self, q, out, in_, reads=(), writes=(), **kw):
        reads, writes = _expand(reads), _expand(writes)
        half = NDMA_SEM // 2
        if q == 'pool':
            k = half + self.dma_rr_pool
            self.dma_rr_pool = (self.dma_rr_pool + 1) % (NDMA_SEM - half)
        else:
            k = self.dma_rr
            self.dma_rr = (k + 1) % half
        deps = self._deps(q, reads, writes, True)
        if self.dma_val[k] > 0:
            deps.add(('d', k, 0, self.dma_val[k]))
        self._emit_waits(q, deps)
        self.dma_val[k] += 16
        sem = self.dma_sems[k]
        self.ops[q].append(lambda E, sem=sem, out=out, in_=in_, kw=kw: E.dma_start(out=out, in_=in_, **kw).then_inc(sem, 16))
        tok = ('d', k, 0, self.dma_val[k])
        self._commit(tok, reads, writes)
        self.nins += 1
        return tok

    def finish(self):
        deps = set()
        for k in range(NDMA_SEM):
            if self.dma_val[k] > 0:
                deps.add(('d', k, 0, self.dma_val[k]))
        for e in ['pe', 'act', 'dve', 'pool']:
            n = self.cnt[e]
            if n > 0:
                ep, idx = divmod(n - 1, EPOCH)
                deps.add(('e', e, ep, idx + 1))
        self._emit_waits('sp', deps)

    def run_block(self):
        nc = self.nc
        ops = self.ops
        with nc.Block() as block:
            @block.sync
            def _(E):
                for f in ops['sp']:
                    f(E)

            @block.tensor
            def _(E):
                for f in ops['pe']:
                    f(E)

            @block.scalar
            def _(E):
                for f in ops['act']:
                    f(E)

            @block.vector
            def _(E):
                for f in ops['dve']:
                    f(E)

            @block.gpsimd
            def _(E):
                for f in ops['pool']:
                    f(E)


class Region:
    def __init__(self, base, size):
        self.base, self.size, self.off = base, size, 0

    def reset(self):
        self.off = 0

    def alloc(self, nbytes):
        al = BLK if nbytes >= 1024 else 32
        start = (self.base + self.off + al - 1) // al * al
        self.off = start - self.base + (nbytes + al - 1) // al * al
        assert self.off <= self.size, (self.off, self.size)
        return start


FM_COLS = ([(c * 128, 128) for c in range(4)] + [(512 + c * 128, 128) for c in range(4)] +
           [(1536 + c * 128, 128) for c in range(4)] + [(2048, 64)] +
           [(2120 + c * 128, 128) for c in range(4)] + [(2632 + c * 128, 128) for c in range(4)])

R_P_SZ = 10 * 1024
R_U_SZ = 90 * 1024
R_UW = (90 + 52) * 1024
R_U_K = [74 * 1024, 90 * 1024] if PIPE else [90 * 1024, 90 * 1024]
R_M_SZ = 32 * 1024
R_W_SZ = 70 * 1024
ARENA = R_P_SZ + R_U_SZ + R_M_SZ + R_W_SZ


def build(phases=(1, 2, 3)):
    nc = bass.Bass("TRN2", target_bir_lowering=False)
    din = lambda name, shape, dt=F32: nc.dram_tensor(name, list(shape), dt, kind="ExternalInput").ap()
    dout = lambda name, shape: nc.dram_tensor(name, list(shape), F32, kind="ExternalOutput").ap()
    dint = lambda name, shape, dt=BF16: nc.dram_tensor(name, list(shape), dt, kind="Internal").ap()

    xp = din("xp", [2, SEQ, D]); xs = din("xs", [2, DSEQ, D])
    cT_d = din("cT", [128, 8, 4])
    ckT = din("ckT", [2, 8, 64, PAST]); cv = din("cv", [2, PAST, 512]); ckiT = din("ckiT", [2, 64, PAST])
    scT_d = din("scT", [2, 512, 30])
    w_ada = din("w_ada", [D, 6 * D]); b_adaT = din("b_adaT", [128, 48])
    n1gT_d = din("n1gT", [128, 8]); n2gT_d = din("n2gT", [128, 8]); fgbc_d = din("fgbc", [128, D])
    w_in = din("w_in", [D, PROJ]); w_out = din("w_out", [D, D]); w_ff1 = din("w_ff1", [D, 4 * D]); w_ff2 = din("w_ff2", [4 * D, D])
    convwT_d = din("convwT", [128, 4, 31]); convbT_d = din("convbT", [128, 4]); lngT_d = din("lngT", [128, 4]); lnbT_d = din("lnbT", [128, 4])
    c_cdiag = din("c_cdiag", [128, 8, 128], BF16)
    c_btab = din("c_btab", [128, 8, 36])

    o_yp = dout("yp", [2, SEQ, D]); o_ys = dout("ys", [2, DSEQ, D])
    o_kp = dout("kp", [2, SEQ, 512]); o_vp = dout("vp", [2, SEQ, 512]); o_kip = dout("kip", [2, SEQ, 64]); o_cp = dout("cp", [2, 30, 512])
    o_ks = dout("ks", [2, DSEQ, 512]); o_vs = dout("vs", [2, DSEQ, 512]); o_kis = dout("kis", [2, DSEQ, 64]); o_cs = dout("cs", [2, 30, 512])

    wsc_fm = dint("wsc_fm", [21, 128, 8, 128])
    wsc_tm = dint("wsc_tm", [D, 1096])
    wsc_out = dint("wsc_out", [D, D])
    wsc_ff1 = dint("wsc_ff1", [32, 128, 8, 128])
    wsc_ff2 = dint("wsc_ff2", [4 * D, D])

    with ExitStack() as st:
        P = Prog(nc, st)
        arena = st.enter_context(nc.sbuf_tensor("arena", [128, ARENA // 2], BF16))
        banks = [st.enter_context(nc.psum_tensor(f"bank{i}", [128, 512], F32)) for i in range(8)]
        BK = lambda i: ('ps', i)
        bkb = lambda i: banks[i][:, :].bitcast(BF16)

        if True:
            RP = Region(0, R_P_SZ); RU = Region(R_P_SZ, R_U_SZ); RM = Region(R_P_SZ + R_U_SZ, R_M_SZ)
            RW = Region(R_P_SZ + R_U_SZ + R_M_SZ, R_W_SZ)

        def mk(reg, shape, dt):
            esz = 4 if dt == F32 else 2
            n = int(np.prod(shape))
            off = reg.alloc(n * esz)
            ap = arena[:, off // 2: off // 2 + n * esz // 2]
            if dt == F32:
                ap = ap.bitcast(F32)
            if len(shape) == 2:
                ap = ap.rearrange("p (a b) -> p a b", b=shape[1])
            elif len(shape) == 3:
                ap = ap.rearrange("p (a b c) -> p a b c", b=shape[1], c=shape[2])
            return Buf(ap, off, n * esz, esz)

        identf = mk(RP, [128], F32); identb = mk(RP, [128], BF16)
        ones64b = mk(RP, [64], BF16); onesb = mk(RP, [128], BF16); onesf = mk(RP, [128], F32)
        cdiag = mk(RP, [8, 128], BF16); btab = mk(RP, [8, 36], F32)
        modT = mk(RP, [48, 4], F32)
        cTs = mk(RP, [8, 4], F32); scs = mk(RP, [8, 4], F32)
        badaT = mk(RP, [48], F32); n1gT = mk(RP, [8], F32); n2gT = mk(RP, [8], F32)
        convwT = mk(RP, [4, 31], F32); convbT = mk(RP, [4], F32); lngT = mk(RP, [4], F32); lnbT = mk(RP, [4], F32)
        G1T = mk(RP, [8], F32); S1T = mk(RP, [8], F32); G2T = mk(RP, [8], F32); S2T = mk(RP, [8], F32)
        g1T = mk(RP, [8], F32); g2T = mk(RP, [8], F32)
        ss = mk(RP, [16], F32); rs = mk(RP, [16], F32)
        sacc = mk(RP, [2], F32); dd = mk(RP, [2], F32); negmid = mk(RP, [2], F32); thr = mk(RP, [2], F32)
        dgt = mk(RP, [128], F32)

        ident_tok = None

        for ci, (c0, cw) in enumerate(FM_COLS):
            if cw == 128:
                P.dma('pool', wsc_fm[ci].rearrange("p kc n -> kc p n"),
                      w_in[:, c0:c0 + 128].rearrange("(kc p) n -> kc p n", p=128), writes=[('wfm', ci)])
            else:
                for hh in range(2):
                    P.dma('pool', wsc_fm[ci][:, :, hh * 64:(hh + 1) * 64].rearrange("p kc n -> kc p n"),
                          w_in[:, c0:c0 + 64].rearrange("(kc p) n -> kc p n", p=128), writes=[('wfm', ci, hh)])
        P.dma('pool', wsc_tm[:, 0:1024], w_in[:, 512:1536], writes=['wtm_a'])
        P.dma('pool', wsc_tm[:, 1024:1096], w_in[:, 2048:2120], writes=['wtm_b'])
        for i in range(4):
            P.dma('pool', wsc_out[i * 256:(i + 1) * 256, :], w_out[i * 256:(i + 1) * 256, :], writes=[('wout', i)])
        for j in range(32):
            P.dma('pool', wsc_ff1[j].rearrange("p kc n -> kc p n"),
                  w_ff1[:, j * 128:(j + 1) * 128].rearrange("(kc p) n -> kc p n", p=128), writes=[('wff1', j)])
        for i in range(16):
            P.dma('pool', wsc_ff2[i * 256:(i + 1) * 256, :], w_ff2[i * 256:(i + 1) * 256, :], writes=[('wff2', i)])
        WFM = [('wfm', ci) for ci in range(21)] + [('wfm', 12, 0), ('wfm', 12, 1)]
        WOUT = [('wout', i) for i in range(4)]
        WFF1 = [('wff1', j) for j in range(32)]
        WFF2 = [('wff2', i) for i in range(16)]

        P.op('dve', lambda E: E.memset(identf.ap, 0.0), writes=[identf])
        P.op('pool', lambda E: E.affine_select(out=identf.ap, in_=identf.ap, pattern=[[-1, 128]], compare_op=ALU.not_equal,
                                                 fill=1.0, base=0, channel_multiplier=1), reads=[identf], writes=[identf])
        P.op('dve', lambda E: E.tensor_copy(out=identb.ap, in_=identf.ap), reads=[identf], writes=[identb])
        P.op('dve', lambda E: E.memset(onesb.ap, 1.0), writes=[onesb])
        P.op('dve', lambda E: E.memset(onesf.ap, 1.0), writes=[onesf])
        for (b, d_) in [(cdiag, c_cdiag), (btab, c_btab), (cTs, cT_d), (badaT, b_adaT), (n1gT, n1gT_d), (n2gT, n2gT_d), (convwT, convwT_d),
                        (convbT, convbT_d), (lngT, lngT_d), (lnbT, lnbT_d)]:
            P.dma('sp', b.ap, d_, writes=[b])

        P.op('act', lambda E: E.activation(out=scs.ap, in_=cTs.ap, func=AF.Silu), reads=[cTs], writes=[scs])
        RU.reset()
        wada_b = [mk(RU, [8, 1024], F32), mk(RU, [8, 1024], F32)]
        modps = banks[0][:, 0:192].rearrange("p (a b) -> p a b", b=4)
        for i in range(6):
            wb_ = wada_b[i % 2]
            P.dma('sp', wb_.ap, w_ada[:, i * 1024:(i + 1) * 1024].rearrange("(kc p) n -> p kc n", p=128), writes=[wb_])
            for cl in range(8):
                ch = i * 8 + cl
                for kc in range(8):
                    P.op('pe', lambda E, wb_=wb_, cl=cl, kc=kc, ch=ch: E.matmul(modps[:, ch, :], lhsT=wb_.ap[:, kc, cl * 128:(cl + 1) * 128],
                                                                             rhs=scs.ap[:, kc, :], start=(kc == 0), stop=(kc == 7)),
                         reads=[wb_, scs], writes=[BK(0)])
        P.op('dve', lambda E: E.tensor_tensor(out=modT.ap, in0=modps, in1=badaT.ap.unsqueeze(2).to_broadcast([128, 48, 4]), op=ALU.add),
             reads=[BK(0), badaT], writes=[modT])

        def bcast_vec(srcT, dst):
            for half in range(2):
                for q in range(4):
                    kc = half * 4 + q
                    P.op('dve', lambda E, kc=kc: E.tensor_scalar(out=dgt.ap, in0=identf.ap, scalar1=srcT.ap[:, kc:kc + 1], scalar2=None, op0=ALU.mult),
                         reads=[identf, srcT], writes=[dgt])
                    P.op('pe', lambda E, q=q: E.matmul(banks[1][:, q * 128:(q + 1) * 128], lhsT=onesf.ap, rhs=dgt.ap, start=True, stop=True),
                         reads=[onesf, dgt], writes=[BK(1)])
                P.op('act', lambda E, half=half: E.activation(out=dst.ap[:, half * 512:(half + 1) * 512], in_=banks[1][:, :], func=AF.Copy),
                     reads=[BK(1)], writes=[dst])

        units = [dict(kind=0, j=0, sq=0), dict(kind=0, j=1, sq=1), dict(kind=1, j=2, sq=0), dict(kind=1, j=3, sq=1)]
        def do_unit(U):
            kind, j, sq = U['kind'], U['j'], U['sq']
            T = SEQ if kind == 0 else DSEQ
            TP = 128 if kind == 0 else 64
            NT = T // TP
            GT = 512 if kind == 0 else 64
            NG = T // GT
            TPG = GT // TP
            NK = SEQ if kind == 0 else NKS
            NKB = (NK + 127) // 128
            KOFF = 0 if kind == 0 else PAST
            x_d = xp[sq] if kind == 0 else xs[sq]
            o_y = o_yp[sq] if kind == 0 else o_ys[sq]
            o_k = o_kp[sq] if kind == 0 else o_ks[sq]
            o_v = o_vp[sq] if kind == 0 else o_vs[sq]
            o_ki = o_kip[sq] if kind == 0 else o_kis[sq]
            o_c = o_cp[sq] if kind == 0 else o_cs[sq]
            UE = 30 + T

            for (dst, gsrc, off) in [(G1T, n1gT, 8), (G2T, n2gT, 32)]:
                P.op('dve', lambda E, dst=dst, gsrc=gsrc, off=off: E.scalar_tensor_tensor(out=dst.ap, in0=modT.ap[:, off:off + 8, j], scalar=1.0, in1=gsrc.ap,
                                                                                      op0=ALU.add, op1=ALU.mult), reads=[modT, gsrc], writes=[dst])
            for (dst, off) in [(S1T, 0), (S2T, 24), (g1T, 16), (g2T, 40)]:
                P.op('dve', lambda E, dst=dst, off=off: E.tensor_copy(out=dst.ap, in_=modT.ap[:, off:off + 8, j]), reads=[modT], writes=[dst])

            RU.reset(); RM.reset(); RW.reset()
            QA = mk(RU, [8, T], BF16); KA = mk(RU, [4, NK], BF16); Vb = mk(RU, [NKB, 8, 65], BF16)
            KIz = [mk(RU, [NK], BF16) for _ in range(2)]; QIT = mk(RU, [4, T], BF16)
            Wt = mk(RU, [16, 8], F32)
            mixT = mk(RM, [8, T], BF16)
            uT = mk(RW, [4, UE], BF16)

            P.op('pool', lambda E: E.memset(QA.ap, 0.0), writes=[QA])
            P.op('pool', lambda E: E.memset(Vb.ap, 1.0), writes=[Vb])
            P.op('pool', lambda E: E.memset(KIz[0].ap[64:128, :], 0.0), writes=[KIz[0]])
            P.op('pool', lambda E: E.memset(KIz[1].ap[0:64, :], 0.0), writes=[KIz[1]])

            RM.reset()
            wtm = mk(RM, [8, 1096], BF16); hT0 = mk(RM, [8, GT], BF16)
            fmr = [mk(RM, [8, 128], BF16) for _ in range(3)]
            xt = [mk(RW, [D], F32) for _ in range(2)]
            kout = mk(RW, [512], F32); vout = mk(RW, [512], F32); kiw = mk(RW, [72], F32)
            sig = mk(RW, [GT], F32); junk = mk(RW, [D], BF16)
            ulast = mk(RW, [4, 32], F32); cst = mk(RW, [512], F32)
            hTs = [hT0, mk(RW, [8, GT], BF16)]

            P.dma('sp', wtm.ap[:, :, 0:1024], wsc_tm[:, 0:1024].rearrange("(kc p) n -> p kc n", p=128), reads=['wtm_a'], writes=[wtm])
            P.dma('sp', wtm.ap[:, :, 1024:1096], wsc_tm[:, 1024:1096].rearrange("(kc p) n -> p kc n", p=128), reads=['wtm_b'], writes=[wtm])
            if kind == 0:
                P.op('pool', lambda E: E.memset(uT.ap[:, :, 0:30], 0.0), writes=[uT])
            else:
                stg_off = [RW.base + RW.size - 2 * PAST * 4, RW.base + RW.size - PAST * 4]
                stg = [Buf(arena[:, o_ // 2:o_ // 2 + PAST * 2].bitcast(F32), o_, PAST * 4, 4) for o_ in stg_off]
                P.dma('sp', stg[0].ap[0:64, :], ckiT[sq], writes=[stg[0]])
                P.dma('sp', stg[0].ap[64:128, :], ckiT[sq], writes=[stg[0]])
                P.dma('sp', stg[1].ap, ckT[sq, 0:2].rearrange("a d n -> (a d) n"), writes=[stg[1]])

                def load_kidx():
                    P.op('act', lambda E: E.activation(out=KIz[0].ap[0:64, 0:PAST], in_=stg[0].ap[0:64, :], func=AF.Copy), reads=[stg[0]], writes=[(KIz[0], 0, PAST)])
                    P.op('act', lambda E: E.activation(out=KIz[1].ap[64:128, 0:PAST], in_=stg[0].ap[64:128, :], func=AF.Copy), reads=[stg[0]], writes=[(KIz[1], 0, PAST)])

                def gen_load():
                    items = [('k', 1), ('k', 2), ('k', 3), ('v', 0), ('v', 1), ('v', 2), ('v', 3)]
                    loaded = [('k', 0, stg[1])]
                    free = [stg[0]]
                    ci = 0
                    while loaded or items:
                        if items and free:
                            typ, idx = items.pop(0)
                            sb2 = free.pop(0)
                            if typ == 'k':
                                P.dma('sp', sb2.ap, ckT[sq, 2 * idx:2 * idx + 2].rearrange("a d n -> (a d) n"), writes=[sb2])
                            else:
                                P.dma('sp', sb2.ap.rearrange("p (a f) -> p a f", f=512), cv[sq][idx * 1024:(idx + 1) * 1024, :].rearrange("(kb p) f -> p kb f", p=128), writes=[sb2])
                            loaded.append((typ, idx, sb2))
                        typ, idx, sb2 = loaded.pop(0)
                        eng_ = 'dve' if ci % 2 == 0 else 'act'
                        ci += 1
                        if typ == 'k':
                            if eng_ == 'dve':
                                P.op('dve', lambda E, sb2=sb2, idx=idx: E.tensor_copy(out=KA.ap[:, idx, 0:PAST], in_=sb2.ap), reads=[sb2], writes=[(KA, idx * NK, idx * NK + PAST)])
                            else:
                                P.op('act', lambda E, sb2=sb2, idx=idx: E.activation(out=KA.ap[:, idx, 0:PAST], in_=sb2.ap, func=AF.Copy), reads=[sb2], writes=[(KA, idx * NK, idx * NK + PAST)])
                        else:
                            outv = Vb.ap[:, idx * 8:(idx + 1) * 8, :, 0:64].rearrange("p a h d -> p (a h) d")
                            inv = sb2.ap.rearrange("p (ah d) -> p ah d", d=64)
                            if eng_ == 'dve':
                                P.op('dve', lambda E, outv=outv, inv=inv: E.tensor_copy(out=outv, in_=inv), reads=[sb2], writes=[(Vb, idx * 8 * 520, (idx + 1) * 8 * 520)])
                            else:
                                P.op('act', lambda E, outv=outv, inv=inv: E.activation(out=outv, in_=inv, func=AF.Copy), reads=[sb2], writes=[(Vb, idx * 8 * 520, (idx + 1) * 8 * 520)])
                        free.append(sb2)
                        yield
                P.dma('pool', uT.ap[:, :, 0:30], scT_d[sq].rearrange("(c p) t -> p c t", p=128), writes=[uT])
            P.op('pool', lambda E: E.memset(ulast.ap, 0.0), writes=[ulast])

            fm_rr = 0
            fmst = dict(rr=0)
            def p1_pro(g):
                hT = hTs[g % 2]
                for tl in range(TPG):
                    tt = g * TPG + tl
                    xb = xt[tt % 2]
                    P.dma('sp', xb.ap[0:TP, :], x_d[tt * TP:(tt + 1) * TP, :], writes=[xb])
                    P.op('act', lambda E, xb=xb, tt=tt: E.activation(out=junk.ap[0:TP, :], in_=xb.ap[0:TP, :], func=AF.Square, accum_out=ss.ap[0:TP, tt:tt + 1]),
                         reads=[xb], writes=[junk, (ss, tt, tt + 1)])
                    P.op('dve', lambda E, tt=tt: E.tensor_scalar(out=rs.ap[0:TP, tt:tt + 1], in0=ss.ap[0:TP, tt:tt + 1], scalar1=1.0 / D, scalar2=EPS, op0=ALU.mult, op1=ALU.add),
                         reads=[(ss, tt, tt + 1)], writes=[(rs, tt, tt + 1)])
                    P.op('act', lambda E, tt=tt: E.activation(out=rs.ap[0:TP, tt:tt + 1], in_=rs.ap[0:TP, tt:tt + 1], func=AF.Sqrt),
                         reads=[(rs, tt, tt + 1)], writes=[(rs, tt, tt + 1)])
                    P.op('dve', lambda E, tt=tt: E.reciprocal(out=rs.ap[0:TP, tt:tt + 1], in_=rs.ap[0:TP, tt:tt + 1]),
                         reads=[(rs, tt, tt + 1)], writes=[(rs, tt, tt + 1)])
                    P.op('dve', lambda E, xb=xb, tt=tt: E.tensor_scalar(out=xb.ap[0:TP, :], in0=xb.ap[0:TP, :], scalar1=rs.ap[0:TP, tt:tt + 1], scalar2=None, op0=ALU.mult),
                         reads=[xb, (rs, tt, tt + 1)], writes=[xb])
                    for kc in range(8):
                        bk = kc // 4
                        P.op('pe', lambda E, xb=xb, kc=kc, bk=bk: E.transpose(banks[bk][:, (kc % 4) * 128:(kc % 4) * 128 + TP], xb.ap[0:TP, kc * 128:(kc + 1) * 128], identf.ap[0:TP, 0:TP]),
                             reads=[xb, identf], writes=[BK(bk)])
                    for kc in range(8):
                        bk = kc // 4
                        P.op('act', lambda E, kc=kc, bk=bk, tl=tl: E.activation(out=hT.ap[:, kc, tl * TP:(tl + 1) * TP], in_=banks[bk][:, (kc % 4) * 128:(kc % 4) * 128 + TP],
                                                                        func=AF.Identity, scale=G1T.ap[:, kc:kc + 1], bias=S1T.ap[:, kc:kc + 1]),
                             reads=[BK(bk), G1T, S1T], writes=[(hT, kc * GT + tl * TP, kc * GT + (tl + 1) * TP)])
            def p1_fm(g):
                hT = hTs[g % 2]
                order = [0, 1, 2, 3, 4, 5, 6, 7, 8, 9, 10, 11, 12, 17, 13, 18, 14, 19, 15, 20, 16]
                t0 = g * GT
                for ci in order:
                    wb_ = fmr[fmst['rr'] % 3]
                    pb = 2 + (fmst['rr'] % 3)
                    fmst['rr'] += 1
                    P.dma('sp', wb_.ap, wsc_fm[ci], reads=WFM, writes=[wb_])
                    for kc in range(8):
                        P.op('pe', lambda E, wb_=wb_, kc=kc, pb=pb: E.matmul(banks[pb][:, 0:GT], lhsT=wb_.ap[:, kc, :], rhs=hT.ap[:, kc, :], start=(kc == 0), stop=(kc == 7)),
                             reads=[wb_, hT], writes=[BK(pb)])
                    src = banks[pb][:, 0:GT]
                    if ci < 4:
                        c = ci
                        P.op('act', lambda E, src=src, c=c, t0=t0: E.activation(out=QA.ap[0:64, 2 * c, t0:t0 + GT], in_=src[0:64, :], func=AF.Copy, scale=0.125),
                             reads=[BK(pb)], writes=[(QA, 2 * c * T + t0, 2 * c * T + t0 + GT)])
                        P.op('act', lambda E, src=src, c=c, t0=t0: E.activation(out=QA.ap[64:128, 2 * c + 1, t0:t0 + GT], in_=src[64:128, :], func=AF.Copy, scale=0.125),
                             reads=[BK(pb)], writes=[((QA, (2 * c + 1) * T + t0, (2 * c + 1) * T + t0 + GT))])
                    elif ci < 8:
                        c = ci - 4
                        P.op('dve', lambda E, src=src, c=c, t0=t0: E.tensor_copy(out=KA.ap[:, c, KOFF + t0:KOFF + t0 + GT], in_=src),
                             reads=[BK(pb)], writes=[(KA, c * NK + KOFF + t0, c * NK + KOFF + t0 + GT)])
                    elif ci < 12:
                        c = ci - 8
                        P.op('act', lambda E, src=src, c=c, t0=t0: E.activation(out=QIT.ap[:, c, t0:t0 + GT], in_=src, func=AF.Copy),
                             reads=[BK(pb)], writes=[(QIT, c * T + t0, c * T + t0 + GT)])
                    elif ci == 12:
                        P.op('dve', lambda E, src=src, t0=t0: E.tensor_copy(out=KIz[0].ap[0:64, KOFF + t0:KOFF + t0 + GT], in_=src[0:64, :]),
                             reads=[BK(pb)], writes=[(KIz[0], KOFF + t0, KOFF + t0 + GT)])
                        P.op('dve', lambda E, src=src, t0=t0: E.tensor_copy(out=KIz[1].ap[64:128, KOFF + t0:KOFF + t0 + GT], in_=src[64:128, :]),
                             reads=[BK(pb)], writes=[(KIz[1], KOFF + t0, KOFF + t0 + GT)])
                    elif ci >= 17:
                        P.op('act', lambda E, src=src: E.activation(out=sig.ap, in_=src, func=AF.Sigmoid), reads=[BK(pb)], writes=[sig])
                    else:
                        c = ci - 13
                        P.op('dve', lambda E, src=src, c=c, t0=t0: E.tensor_tensor(out=uT.ap[:, c, 30 + t0:30 + t0 + GT], in0=src, in1=sig.ap, op=ALU.mult),
                             reads=[BK(pb), sig], writes=[(uT, c * UE + 30 + t0, c * UE + 30 + t0 + GT)])
                        if g == NG - 1:
                            P.op('dve', lambda E, src=src, c=c: E.tensor_tensor(out=ulast.ap[:, c, 0:30], in0=src[:, GT - 30:GT], in1=sig.ap[:, GT - 30:GT], op=ALU.mult),
                                 reads=[BK(pb), sig], writes=[ulast])
            def p1_tm(g):
                hT = hTs[g % 2]
                for tl in range(TPG):
                    tt = g * TPG + tl
                    for (pb, c0_, cw_) in [(5, 0, 512), (6, 512, 512), (7, 1024, 72)]:
                        for kc in range(8):
                            P.op('pe', lambda E, kc=kc, pb=pb, c0_=c0_, cw_=cw_, tl=tl: E.matmul(banks[pb][0:TP, 0:cw_], lhsT=hT.ap[:, kc, tl * TP:(tl + 1) * TP],
                                                                                           rhs=wtm.ap[:, kc, c0_:c0_ + cw_], start=(kc == 0), stop=(kc == 7)),
                                 reads=[hT, wtm], writes=[BK(pb)])
                    P.op('act', lambda E: E.activation(out=kout.ap[0:TP, :], in_=banks[5][0:TP, :], func=AF.Copy), reads=[BK(5)], writes=[kout])
                    P.dma('pool', o_k[tt * TP:(tt + 1) * TP, :], kout.ap[0:TP, :], reads=[kout], writes=[('o_k', j, tt)])
                    P.op('dve', lambda E: E.tensor_copy(out=vout.ap[0:TP, :], in_=banks[6][0:TP, :]), reads=[BK(6)], writes=[vout])
                    P.dma('pool', o_v[tt * TP:(tt + 1) * TP, :], vout.ap[0:TP, :], reads=[vout], writes=[('o_v', j, tt)])
                    kbv = tt if kind == 0 else 32
                    P.op('pool', lambda E, kbv=kbv: E.tensor_copy(out=Vb.ap[0:TP, kbv, :, 0:64], in_=vout.ap[0:TP, :].rearrange("p (h d) -> p h d", d=64)),
                         reads=[vout], writes=[(Vb, kbv * 520, kbv * 520 + 520)])
                    P.op('dve', lambda E: E.tensor_copy(out=kiw.ap[0:TP, :], in_=banks[7][0:TP, 0:72]), reads=[BK(7)], writes=[kiw])
                    P.dma('pool', o_ki[tt * TP:(tt + 1) * TP, :], kiw.ap[0:TP, 0:64], reads=[kiw], writes=[('o_ki', j, tt)])
                    P.op('dve', lambda E, tt=tt: E.tensor_scalar(out=Wt.ap[0:TP, tt, :], in0=kiw.ap[0:TP, 64:72], scalar1=float(1.0 / (8.0 * np.sqrt(8.0))), scalar2=None, op0=ALU.mult),
                         reads=[kiw], writes=[(Wt, tt * 8, tt * 8 + 8)])
            p1_pro(0)
            for g in range(NG):
                p1_fm(g)
                if g + 1 < NG:
                    p1_pro(g + 1)
                p1_tm(g)
            for c in range(4):
                P.op('pe', lambda E, c=c: E.transpose(banks[0][0:32, c * 128:(c + 1) * 128], ulast.ap[:, c, :], identf.ap), reads=[ulast, identf], writes=[BK(0)])
            P.op('act', lambda E: E.activation(out=cst.ap[0:32, :], in_=banks[0][0:32, :], func=AF.Copy), reads=[BK(0)], writes=[cst])
            P.dma('pool', o_c, cst.ap[0:30, :], reads=[cst], writes=[('o_c', j)])

            if 2 not in phases:
                return
            RM.reset()
            mixT = mk(RM, [8, T], BF16)
            RW.reset()
            uT = mk(RW, [4, UE], BF16)
            ysq = mk(RW, [GT], F32); mean_sb = mk(RW, [GT], F32); rstd_sb = mk(RW, [GT], F32); zt = mk(RW, [GT], F32)
            Dcv = mk(RW, [31, 128], BF16)
            cvb = 0
            for c in range(4):
                for jj in range(31):
                    P.op('dve', lambda E, c=c, jj=jj: E.tensor_scalar(out=Dcv.ap[:, jj, :], in0=identb.ap, scalar1=convwT.ap[:, c, jj:jj + 1], scalar2=None, op0=ALU.mult),
                         reads=[identb, convwT], writes=[(Dcv, jj * 128, jj * 128 + 128)])
                for g in range(NG):
                    pb = cvb % 3; cvb += 1
                    for jj in range(31):
                        P.op('pe', lambda E, c=c, jj=jj, pb=pb, g=g: E.matmul(banks[pb][:, 0:GT], lhsT=Dcv.ap[:, jj, :], rhs=uT.ap[:, c, g * GT + jj:g * GT + jj + GT],
                                                                          start=(jj == 0), stop=(jj == 30)),
                             reads=[Dcv, (uT, c * UE + g * GT, c * UE + g * GT + GT + 30)], writes=[BK(pb)])
                    P.op('act', lambda E, c=c, pb=pb, g=g: E.activation(out=mixT.ap[:, 4 + c, g * GT:(g + 1) * GT], in_=banks[pb][:, 0:GT], func=AF.Identity, bias=convbT.ap[:, c:c + 1]),
                         reads=[BK(pb), convbT], writes=[(mixT, (4 + c) * T + g * GT, (4 + c) * T + (g + 1) * GT)])
            for g in range(NG):
                for c in range(4):
                    mrange = (mixT, (4 + c) * T + g * GT, (4 + c) * T + (g + 1) * GT)
                    P.op('act', lambda E, c=c, g=g: E.activation(out=ysq.ap, in_=mixT.ap[:, 4 + c, g * GT:(g + 1) * GT], func=AF.Square), reads=[mrange], writes=[ysq])
                    P.op('pe', lambda E, c=c, g=g: E.matmul(banks[3][:, 0:GT], lhsT=onesb.ap, rhs=mixT.ap[:, 4 + c, g * GT:(g + 1) * GT], start=(c == 0), stop=(c == 3)),
                         reads=[onesb, mrange], writes=[BK(3)])
                    P.op('pe', lambda E, c=c: E.matmul(banks[4][:, 0:GT], lhsT=onesf.ap, rhs=ysq.ap, start=(c == 0), stop=(c == 3)),
                         reads=[onesf, ysq], writes=[BK(4)])
                P.op('act', lambda E: E.activation(out=mean_sb.ap, in_=banks[3][:, 0:GT], func=AF.Copy, scale=1.0 / 512), reads=[BK(3)], writes=[mean_sb])
                P.op('dve', lambda E: E.tensor_tensor(out=zt.ap, in0=mean_sb.ap, in1=mean_sb.ap, op=ALU.mult), reads=[mean_sb], writes=[zt])
                P.op('dve', lambda E: E.scalar_tensor_tensor(out=rstd_sb.ap, in0=banks[4][:, 0:GT], scalar=1.0 / 512, in1=zt.ap, op0=ALU.mult, op1=ALU.subtract),
                     reads=[BK(4), zt], writes=[rstd_sb])
                P.op('dve', lambda E: E.tensor_scalar(out=rstd_sb.ap, in0=rstd_sb.ap, scalar1=EPS, scalar2=None, op0=ALU.add), reads=[rstd_sb], writes=[rstd_sb])
                P.op('act', lambda E: E.activation(out=rstd_sb.ap, in_=rstd_sb.ap, func=AF.Sqrt), reads=[rstd_sb], writes=[rstd_sb])
                P.op('dve', lambda E: E.reciprocal(out=rstd_sb.ap, in_=rstd_sb.ap), reads=[rstd_sb], writes=[rstd_sb])
                for c in range(4):
                    mrange = (mixT, (4 + c) * T + g * GT, (4 + c) * T + (g + 1) * GT)
                    P.op('dve', lambda E, c=c, g=g: E.tensor_tensor(out=zt.ap, in0=mixT.ap[:, 4 + c, g * GT:(g + 1) * GT], in1=mean_sb.ap, op=ALU.subtract),
                         reads=[mrange, mean_sb], writes=[zt])
                    P.op('dve', lambda E: E.tensor_tensor(out=zt.ap, in0=zt.ap, in1=rstd_sb.ap, op=ALU.mult), reads=[zt, rstd_sb], writes=[zt])
                    P.op('act', lambda E, c=c, g=g: E.activation(out=mixT.ap[:, 4 + c, g * GT:(g + 1) * GT], in_=zt.ap, func=AF.Silu, scale=lngT.ap[:, c:c + 1], bias=lnbT.ap[:, c:c + 1]),
                         reads=[zt, lngT, lnbT], writes=[mrange])

            RW.reset()
            LMAX = NK
            NLS = 2 if kind == 0 else 1
            Ibs = [mk(RW, [LMAX], F32) for _ in range(NLS)]; Mbs = [mk(RW, [LMAX], BF16) for _ in range(NLS)]
            NMT = 2 if (kind == 0 and PIPE) else 1
            MTs = [mk(RW, [NKB, GT], BF16) for _ in range(NMT)]
            Rr = [mk(RW, [512], BF16) for _ in range(4)]
            SLOTS3 = [0, 1, 2]; SLOTS6 = [0, 1, 2, 3, 7, 4]
            NEB = 6
            Eb = [mk(RW, [GT], BF16) for _ in range(NEB)]; Pm = Eb
            bcs = mk(RW, [GT], F32); Dg = mk(RW, [8, 128], BF16)
            NPIECE = NG
            wN = BIS_B / (2 ** NITER)
            DB = [3, 7]
            st_ = dict(att_rr=0, d_rr=0, e_rr=0)

            def gen_idx(qp, pr):
                MT = MTs[qp % NMT]
                tiles = list(range(pr, min(pr + NLS, TPG)))
                info = []
                for r in tiles:
                    i = qp * TPG + r
                    L = 128 * (i + 1) if kind == 0 else NKS
                    qs = i * TP
                    Ib = Ibs[r % NLS]; Mb = Mbs[r % NLS]
                    info.append((r, i, L, Ib, Mb))
                    for h in range(8):
                        P.op('dve', lambda E, h=h, i=i: E.tensor_scalar(out=Dg.ap[0:TP, h, 0:TP], in0=identb.ap[0:TP, 0:TP], scalar1=Wt.ap[0:TP, i, h:h + 1], scalar2=None, op0=ALU.mult),
                             reads=[identb, (Wt, i * 8, i * 8 + 8)], writes=[(Dg, h * 128, h * 128 + 128)])
                    nkp = (L + 511) // 512
                    for kp in range(nkp):
                        k0 = kp * 512
                        kw = min(512, L - k0)

                        def dmm(h):
                            pb = DB[st_['d_rr'] % 2]; st_['d_rr'] += 1
                            P.op('pe', lambda E, h=h, pb=pb, kw=kw, k0=k0, qs=qs: E.matmul(banks[pb][0:TP, 0:kw], lhsT=QIT.ap[:, h // 2, qs:qs + TP], rhs=KIz[h % 2].ap[:, k0:k0 + kw],
                                                                                 start=True, stop=True),
                                 reads=[(QIT, (h // 2) * T + qs, (h // 2) * T + qs + TP), (KIz[h % 2], k0, k0 + kw)], writes=[BK(pb)])
                            rb = Rr[h % 4]
                            if h % 2 == 0 or h == 7:
                                P.op('act', lambda E, pb=pb, rb=rb, kw=kw: E.activation(out=rb.ap[0:TP, 0:kw], in_=banks[pb][0:TP, 0:kw], func=AF.Relu), reads=[BK(pb)], writes=[rb])
                            else:
                                P.op('dve', lambda E, pb=pb, rb=rb, kw=kw: E.tensor_scalar(out=rb.ap[0:TP, 0:kw], in0=banks[pb][0:TP, 0:kw], scalar1=0.0, scalar2=None, op0=ALU.max), reads=[BK(pb)], writes=[rb])

                        def amm(h):
                            rb = Rr[h % 4]
                            P.op('pe', lambda E, h=h, rb=rb, kw=kw: E.matmul(banks[4][0:TP, 0:kw], lhsT=Dg.ap[0:TP, h, 0:TP], rhs=rb.ap[0:TP, 0:kw], start=(h == 0), stop=(h == 7)),
                                 reads=[(Dg, h * 128, h * 128 + 128), rb], writes=[BK(4)])
                        dmm(0)
                        for h in range(8):
                            if h + 1 < 8:
                                dmm(h + 1)
                            amm(h)
                            if h % 4 == 3:
                                yield
                        P.op('dve', lambda E, k0=k0, kw=kw, Ib=Ib: E.tensor_copy(out=Ib.ap[0:TP, k0:k0 + kw], in_=banks[4][0:TP, 0:kw]), reads=[BK(4)], writes=[(Ib, k0, k0 + kw)])
                    if kind == 0:
                        P.op('dve', lambda E, L=L, Ib=Ib: E.memset(Ib.ap[0:64, L - 64:L], -1e30), writes=[(Ib, L - 64, L)])
                    yield
                bis = [t_ for t_ in info if t_[2] > 256]
                for (r, i, L, Ib, Mb) in bis:
                    col = r % NLS
                    P.op('dve', lambda E, col=col: E.memset(negmid.ap[0:TP, col:col + 1], 0.0), writes=[('negmid', col)])
                if kind != 0 and bis:
                    (r, i, L, Ib, Mb) = bis[0]
                    LA = (L // 2) // 64 * 64
                    P.op('dve', lambda E: E.memset(negmid.ap[0:TP, 1:2], 0.0), writes=[('negmid', 1)])
                    for it in range(NITER):
                        wk = BIS_B / (2 ** it)
                        P.op('act', lambda E: E.activation(out=Mb.ap[0:TP, 0:LA], in_=Ib.ap[0:TP, 0:LA], func=AF.Sign, bias=negmid.ap[0:TP, 0:1], scale=1.0, accum_out=sacc.ap[0:TP, 0:1]),
                             reads=[(Ib, 0, LA), ('negmid', 0)], writes=[(Mb, 0, LA), ('sacc', 0)])
                        P.op('dve', lambda E: E.tensor_scalar(out=Mb.ap[0:TP, LA:L], in0=Ib.ap[0:TP, LA:L], scalar1=negmid.ap[0:TP, 1:2], scalar2=0.0, op0=ALU.is_ge, op1=ALU.add,
                                                              accum_out=sacc.ap[0:TP, 1:2]),
                             reads=[(Ib, LA, L), ('negmid', 1)], writes=[(Mb, LA, L), ('sacc', 1)])
                        P.op('dve', lambda E: E.scalar_tensor_tensor(out=dd.ap[0:TP, 1:2], in0=sacc.ap[0:TP, 1:2], scalar=2.0, in1=sacc.ap[0:TP, 0:1], op0=ALU.mult, op1=ALU.add),
                             reads=[('sacc', 0), ('sacc', 1)], writes=[('dd', 1)])
                        P.op('dve', lambda E: E.tensor_scalar(out=dd.ap[0:TP, 0:1], in0=dd.ap[0:TP, 1:2], scalar1=float(512 - LA), scalar2=0.5, op0=ALU.is_ge, op1=ALU.subtract),
                             reads=[('dd', 1)], writes=[('dd', 0)])
                        P.op('dve', lambda E, wk=wk: E.scalar_tensor_tensor(out=negmid.ap[0:TP, 0:1], in0=dd.ap[0:TP, 0:1], scalar=-wk, in1=negmid.ap[0:TP, 0:1], op0=ALU.mult, op1=ALU.add),
                             reads=[('dd', 0), ('negmid', 0)], writes=[('negmid', 0)])
                        P.op('dve', lambda E, wk=wk: E.scalar_tensor_tensor(out=negmid.ap[0:TP, 1:2], in0=dd.ap[0:TP, 0:1], scalar=wk, in1=negmid.ap[0:TP, 1:2], op0=ALU.mult, op1=ALU.add),
                             reads=[('dd', 0), ('negmid', 1)], writes=[('negmid', 1)])
                        yield
                    bis = []
                for it in range(NITER if bis else 0):
                    wk = BIS_B / (2 ** it)
                    for (r, i, L, Ib, Mb) in bis:
                        col = r % NLS
                        if col == 0:
                            P.op('act', lambda E, L=L, Ib=Ib, Mb=Mb, col=col: E.activation(out=Mb.ap[0:TP, 0:L], in_=Ib.ap[0:TP, 0:L], func=AF.Sign, bias=negmid.ap[0:TP, col:col + 1], scale=1.0,
                                                                                       accum_out=sacc.ap[0:TP, col:col + 1]),
                                 reads=[(Ib, 0, L), ('negmid', col)], writes=[(Mb, 0, L), ('sacc', col)])
                        else:
                            P.op('dve', lambda E, L=L, Ib=Ib, Mb=Mb, col=col: E.tensor_scalar(out=Mb.ap[0:TP, 0:L], in0=Ib.ap[0:TP, 0:L], scalar1=negmid.ap[0:TP, col:col + 1], scalar2=0.0,
                                                                                          op0=ALU.is_ge, op1=ALU.add, accum_out=sacc.ap[0:TP, col:col + 1]),
                                 reads=[(Ib, 0, L), ('negmid', col)], writes=[(Mb, 0, L), ('sacc', col)])
                    for (r, i, L, Ib, Mb) in bis:
                        col = r % NLS
                        Cc = float(512 - L) if col == 0 else 256.0
                        sg = -wk if col == 0 else wk
                        P.op('dve', lambda E, Cc=Cc, col=col: E.tensor_scalar(out=dd.ap[0:TP, col:col + 1], in0=sacc.ap[0:TP, col:col + 1], scalar1=Cc, scalar2=0.5, op0=ALU.is_ge, op1=ALU.subtract),
                             reads=[('sacc', col)], writes=[('dd', col)])
                        P.op('dve', lambda E, sg=sg, col=col: E.scalar_tensor_tensor(out=negmid.ap[0:TP, col:col + 1], in0=dd.ap[0:TP, col:col + 1], scalar=sg, in1=negmid.ap[0:TP, col:col + 1],
                                                                                  op0=ALU.mult, op1=ALU.add),
                             reads=[('dd', col), ('negmid', col)], writes=[('negmid', col)])
                    for _y in range(BIS_YIELDS):
                        yield
                for (r, i, L, Ib, Mb) in info:
                    col = r % NLS
                    if L > 256:
                        sgn = -1.0 if col == 0 else 1.0
                        P.op('dve', lambda E, col=col, sgn=sgn: E.tensor_scalar(out=thr.ap[0:TP, col:col + 1], in0=negmid.ap[0:TP, col:col + 1], scalar1=sgn, scalar2=-wN, op0=ALU.mult, op1=ALU.add),
                             reads=[('negmid', col)], writes=[('thr', col)])
                    else:
                        P.op('dve', lambda E, col=col: E.memset(thr.ap[0:TP, col:col + 1], -1e29), writes=[('thr', col)])
                    P.op('dve', lambda E, L=L, Ib=Ib, Mb=Mb, col=col: E.tensor_scalar(out=Mb.ap[0:TP, 0:L], in0=Ib.ap[0:TP, 0:L], scalar1=thr.ap[0:TP, col:col + 1], scalar2=-30000.0, op0=ALU.is_lt, op1=ALU.mult),
                         reads=[(Ib, 0, L), ('thr', col)], writes=[(Mb, 0, L)])
                    nkb = (L + 127) // 128
                    kb = 0
                    half = 0
                    while kb < nkb:
                        nb = min(4, nkb - kb)
                        full = all(min(128, L - 128 * (kb + s_)) == 128 for s_ in range(nb))
                        if not full and nb > 1:
                            nb -= 1
                        base = half * 512
                        for s_ in range(nb):
                            kw_ = min(128, L - 128 * (kb + s_))
                            P.op('pe', lambda E, s_=s_, kw_=kw_, kb=kb, base=base, Mb=Mb: E.transpose(bkb(4)[0:kw_, base + s_ * 128: base + s_ * 128 + TP], Mb.ap[0:TP, (kb + s_) * 128:(kb + s_) * 128 + kw_], identb.ap[0:TP, 0:TP]),
                                 reads=[(Mb, (kb + s_) * 128, (kb + s_) * 128 + kw_), identb], writes=[BK(4)])
                        kw_ = min(128, L - 128 * kb) if nb == 1 else 128
                        srcv = bkb(4)[0:kw_, base:base + nb * 128].rearrange("p (a b) -> p a b", b=128)[:, :, 0:TP]
                        P.op('act', lambda E, kb=kb, nb=nb, kw_=kw_, srcv=srcv, r=r, MT=MT: E.activation(out=MT.ap[0:kw_, kb:kb + nb, r * TP:(r + 1) * TP], in_=srcv, func=AF.Copy),
                             reads=[BK(4)], writes=[(MT, kb * GT, (kb + nb) * GT)])
                        kb += nb
                        half ^= 1
                        yield

            def gen_att(qp):
                MT = MTs[qp % NMT]
                SL = SLOTS6 if (kind != 0 or qp == NPIECE - 1) else SLOTS3
                NSL_ = len(SL)
                SK_ = NSL_ - 1
                skey = lambda sl: BK(SL[sl])
                sview = lambda sl, rows, n: banks[SL[sl]][0:rows, 0:n]
                st_['att_rr'] = 0
                q0 = qp * GT
                kb_last = (qp * 4 + 3) if kind == 0 else (NKB - 1)
                tb0 = qp * 4 if kind == 0 else 32
                blocks = [(h, kb) for h in range(8) for kb in range(kb_last + 1)]

                def geom(kb):
                    kw_ = min(128, NK - 128 * kb)
                    if kind == 0:
                        jd = kb - 4 * qp
                        r0 = max(jd, 0)
                        return kw_, r0, r0 * 128, jd >= 0, 128
                    return kw_, 0, 0, (kb == NKB - 1), 64

                def front(h, kb):
                    kw_, r0, c0, diag, TPd = geom(kb)
                    hb = (h % 2) * 64; hc = h // 2
                    N = GT - c0
                    sb_ = st_['att_rr'] % NSL_; st_['att_rr'] += 1
                    P.op('pe', lambda E: E.matmul(sview(sb_, kw_, N), lhsT=KA.ap[:, hc, kb * 128:kb * 128 + kw_], rhs=QA.ap[:, h, q0 + c0:q0 + GT], start=True, stop=False),
                         reads=[(KA, hc * NK + kb * 128, hc * NK + kb * 128 + kw_), (QA, h * T + q0, h * T + q0 + GT)], writes=[skey(sb_)])
                    if diag:
                        P.op('pe', lambda E: E.matmul(sview(sb_, kw_, TPd), lhsT=identb.ap[0:kw_, 0:kw_], rhs=cdiag.ap[0:kw_, h, 0:TPd], start=False, stop=False),
                             reads=[identb, cdiag], writes=[skey(sb_)])
                    P.op('pe', lambda E: E.matmul(sview(sb_, kw_, N), lhsT=identb.ap[0:kw_, 0:kw_], rhs=MT.ap[0:kw_, kb, c0:GT], start=False, stop=True),
                         reads=[identb, (MT, kb * GT, (kb + 1) * GT)], writes=[skey(sb_)])
                    return sb_

                def back(h, kb, sb_):
                    kw_, r0, c0, diag, TPd = geom(kb)
                    hb = (h % 2) * 64; hc = h // 2
                    ob = 5 + (h % 2)
                    N = GT - c0
                    eb = Eb[st_['e_rr'] % NEB]; pm = Pm[st_['e_rr'] % NEB]; st_['e_rr'] += 1
                    if kind == 0 and h < 2:
                        for r in range(r0, 4):
                            dl = kb - (tb0 + r) + 32
                            a0 = r * 128 - c0
                            P.op('act', lambda E, a0=a0, dl=dl: E.activation(out=eb.ap[0:kw_, a0:a0 + 128], in_=banks[SL[sb_]][0:kw_, a0:a0 + 128], func=AF.Exp,
                                                                             bias=btab.ap[0:kw_, h, dl:dl + 1], scale=1.0),
                                 reads=[skey(sb_), btab], writes=[eb])
                    else:
                        dl = kb - tb0 + 32
                        P.op('act', lambda E: E.activation(out=eb.ap[0:kw_, 0:N], in_=sview(sb_, kw_, N), func=AF.Exp, bias=btab.ap[0:kw_, h, dl:dl + 1], scale=1.0),
                             reads=[skey(sb_), btab], writes=[eb])
                    P.op('pe', lambda E: E.matmul(banks[ob][0:65, c0:GT], lhsT=Vb.ap[0:kw_, kb, h, :], rhs=eb.ap[0:kw_, 0:N], start=(kb == 0), stop=(kb == kb_last)),
                         reads=[(Vb, kb * 520, kb * 520 + 520), eb], writes=[BK(ob)])
                    if kb == kb_last:
                        P.op('dve', lambda E: E.tensor_scalar(out=bcs.ap[64:65, 0:GT], in0=banks[ob][64:65, 0:GT], scalar1=1e-30, scalar2=None, op0=ALU.max), reads=[BK(ob)], writes=[bcs])
                        P.op('dve', lambda E: E.reciprocal(out=bcs.ap[64:65, 0:GT], in_=bcs.ap[64:65, 0:GT]), reads=[bcs], writes=[bcs])

                        def stage2():
                            nb_ = st_['att_rr'] % NSL_; st_['att_rr'] += 1
                            P.op('pe', lambda E: E.matmul(sview(nb_, 64, GT), lhsT=onesf.ap[64:65, 0:64], rhs=bcs.ap[64:65, 0:GT], start=True, stop=True), reads=[onesf, bcs], writes=[skey(nb_)])
                            P.op('act', lambda E: E.activation(out=bcs.ap[0:64, :], in_=sview(nb_, 64, GT), func=AF.Copy), reads=[skey(nb_)], writes=[bcs])
                            P.op('dve', lambda E: E.tensor_tensor(out=mixT.ap[hb:hb + 64, hc, q0:q0 + GT], in0=banks[ob][0:64, 0:GT], in1=bcs.ap[0:64, :], op=ALU.mult),
                                 reads=[BK(ob), bcs], writes=[(mixT, hc * T + q0, hc * T + q0 + GT)])
                        norm_q.append([NORM_DELAY, stage2])

                norm_q = []
                NORM_DELAY = 3
                if False:
                    P.op('dve', lambda E: E.memset(dd.ap[0:1, 0:1], 0.0), reads=[BK(0), BK(1), BK(2)], writes=[('pss', sl) for sl in range(NSL)] + [BK(0), BK(1), BK(2)])
                pend = []
                nf = 0
                for n in range(len(blocks)):
                    while nf < len(blocks) and nf <= n + SK_ - 1:
                        h_, kb_ = blocks[nf]
                        pend.append((h_, kb_, front(h_, kb_)))
                        nf += 1
                    back(*pend.pop(0))
                    for it_ in norm_q:
                        it_[0] -= 1
                    if norm_q and norm_q[0][0] <= 0:
                        norm_q.pop(0)[1]()
                    yield
                for it_ in list(norm_q):
                    it_[1]()
                norm_q.clear()
                if False:
                    P.op('dve', lambda E: E.memset(dd.ap[0:1, 0:1], 0.0), reads=[('pss', sl) for sl in range(NSL)], writes=[('pss', sl) for sl in range(NSL)] + [BK(0), BK(1), BK(2)])
                yield

            def run_streams(A, B, na, nb):
                ia = ib = 0
                a_done = A is None
                b_done = B is None
                while not (a_done and b_done):
                    pick_a = (not a_done) and (b_done or ia * max(nb, 1) <= ib * max(na, 1))
                    if pick_a:
                        try:
                            next(A); ia += 1
                        except StopIteration:
                            a_done = True
                    else:
                        try:
                            next(B); ib += 1
                        except StopIteration:
                            b_done = True

            def chain(*gens):
                for g_ in gens:
                    yield from g_

            def count_steps(make):
                return None

            pairs = list(range(0, TPG, NLS))
            if not PIPE:
                for qp in range(NPIECE):
                    run_streams(chain(*[gen_idx(qp, pr) for pr in pairs]), None, 1, 1)
                    run_streams(gen_att(qp), None, 1, 1)
            else:
                if kind == 0:
                    run_streams(chain(*[gen_idx(0, pr) for pr in pairs]), None, 1, 1)
                else:
                    load_kidx()
                    run_streams(chain(*[gen_idx(0, pr) for pr in pairs]), gen_load(), 60, 9)
                for qp in range(NPIECE):
                    kbl = (qp * 4 + 4) if kind == 0 else NKB
                    na = 8 * kbl + 1
                    if qp + 1 < NPIECE:
                        nb = 0
                        for r in range(TPG):
                            L_ = 128 * ((qp + 1) * TPG + r + 1)
                            nb += ((L_ + 511) // 512) * 2 + 1 + ((L_ + 127) // 128 + 3) // 4
                        nb += NITER * BIS_YIELDS * len(pairs)
                        run_streams(gen_att(qp), chain(*[gen_idx(qp + 1, pr) for pr in pairs]), na, nb)
                    else:
                        run_streams(gen_att(qp), None, na, 1)

            if 3 not in phases:
                return
            RU.reset(); RW.reset()
            fgbc = mk(RU, [D], F32); g1bc = mk(RU, [D], F32); g2bc = mk(RU, [D], F32)
            P.dma('sp', fgbc.ap, fgbc_d, writes=[fgbc])
            bcast_vec(g1T, g1bc)
            bcast_vec(g2T, g2bc)
            wo = mk(RU, [8, D], BF16)
            xg = [mk(RU, [D], F32) for _ in range(TPG)]
            h2T = mk(RU, [8, GT], BF16)
            hid = mk(RU, [32, GT], BF16)
            f1r = [mk(RW, [4, 8, 128], BF16) for _ in range(2)]
            f2r = [mk(RW, [8, 512], BF16) for _ in range(2)]
            xn2 = mk(RW, [D], F32); tmp5 = mk(RW, [512], F32); rl = mk(RW, [GT], F32); junk3 = mk(RW, [D], BF16)
            P.dma('sp', wo.ap, wsc_out.rearrange("(kc p) n -> p kc n", p=128), reads=WOUT, writes=[wo])
            f1_rr = 0; f2_rr = 0; f1b_rr = 0
            for g in range(NG):
                for tl in range(TPG):
                    tt = g * TPG + tl
                    xb = xg[tl]
                    P.dma('sp', xb.ap[0:TP, :], x_d[tt * TP:(tt + 1) * TP, :], writes=[xb])
                    for half in range(2):
                        for kc in range(8):
                            P.op('pe', lambda E, kc=kc, half=half, tt=tt: E.matmul(banks[half][0:TP, :], lhsT=mixT.ap[:, kc, tt * TP:(tt + 1) * TP], rhs=wo.ap[:, kc, half * 512:(half + 1) * 512],
                                                                             start=(kc == 0), stop=(kc == 7)),
                                 reads=[(mixT, kc * T + tt * TP, kc * T + (tt + 1) * TP), wo], writes=[BK(half)])
                        P.op('dve', lambda E, half=half: E.tensor_tensor(out=tmp5.ap[0:TP, :], in0=banks[half][0:TP, :], in1=g1bc.ap[0:TP, half * 512:(half + 1) * 512], op=ALU.mult),
                             reads=[BK(half), g1bc], writes=[tmp5])
                        P.op('dve', lambda E, half=half, xb=xb: E.tensor_tensor(out=xb.ap[0:TP, half * 512:(half + 1) * 512], in0=xb.ap[0:TP, half * 512:(half + 1) * 512], in1=tmp5.ap[0:TP, :], op=ALU.add),
                             reads=[tmp5, xb], writes=[xb])
                    P.op('act', lambda E, xb=xb, tl=tl: E.activation(out=junk3.ap[0:TP, :], in_=xb.ap[0:TP, :], func=AF.Square, accum_out=ss.ap[0:TP, tl:tl + 1]),
                         reads=[xb], writes=[junk3, (ss, tl, tl + 1)])
                    P.op('dve', lambda E, tl=tl: E.tensor_scalar(out=rs.ap[0:TP, tl:tl + 1], in0=ss.ap[0:TP, tl:tl + 1], scalar1=1.0 / D, scalar2=EPS, op0=ALU.mult, op1=ALU.add),
                         reads=[(ss, tl, tl + 1)], writes=[(rs, tl, tl + 1)])
                    P.op('act', lambda E, tl=tl: E.activation(out=rs.ap[0:TP, tl:tl + 1], in_=rs.ap[0:TP, tl:tl + 1], func=AF.Sqrt), reads=[(rs, tl, tl + 1)], writes=[(rs, tl, tl + 1)])
                    P.op('dve', lambda E, tl=tl: E.reciprocal(out=rs.ap[0:TP, tl:tl + 1], in_=rs.ap[0:TP, tl:tl + 1]), reads=[(rs, tl, tl + 1)], writes=[(rs, tl, tl + 1)])
                    P.op('dve', lambda E, xb=xb, tl=tl: E.tensor_scalar(out=xn2.ap[0:TP, :], in0=xb.ap[0:TP, :], scalar1=rs.ap[0:TP, tl:tl + 1], scalar2=None, op0=ALU.mult),
                         reads=[xb, (rs, tl, tl + 1)], writes=[xn2])
                    for kc in range(8):
                        bk = 2 + kc // 4
                        P.op('pe', lambda E, kc=kc, bk=bk: E.transpose(banks[bk][:, (kc % 4) * 128:(kc % 4) * 128 + TP], xn2.ap[0:TP, kc * 128:(kc + 1) * 128], identf.ap[0:TP, 0:TP]),
                             reads=[xn2, identf], writes=[BK(bk)])
                    for kc in range(8):
                        bk = 2 + kc // 4
                        P.op('act', lambda E, kc=kc, bk=bk, tl=tl: E.activation(out=h2T.ap[:, kc, tl * TP:(tl + 1) * TP], in_=banks[bk][:, (kc % 4) * 128:(kc % 4) * 128 + TP],
                                                                        func=AF.Identity, scale=G2T.ap[:, kc:kc + 1], bias=S2T.ap[:, kc:kc + 1]),
                             reads=[BK(bk), G2T, S2T], writes=[(h2T, kc * GT + tl * TP, kc * GT + (tl + 1) * TP)])
                for j4 in range(8):
                    fb = f1r[f1_rr % 2]; f1_rr += 1
                    P.dma('sp', fb.ap, wsc_ff1[j4 * 4:(j4 + 1) * 4].rearrange("j p kc n -> p j kc n"), reads=WFF1, writes=[fb])
                    for jj in range(4):
                        jh = j4 * 4 + jj
                        pb = f1b_rr % 4; f1b_rr += 1
                        for kc in range(8):
                            P.op('pe', lambda E, fb=fb, jj=jj, kc=kc, pb=pb: E.matmul(banks[pb][:, 0:GT], lhsT=fb.ap[:, jj, kc, :], rhs=h2T.ap[:, kc, :], start=(kc == 0), stop=(kc == 7)),
                                 reads=[fb, h2T], writes=[BK(pb)])
                        P.op('act', lambda E, pb=pb: E.activation(out=rl.ap, in_=banks[pb][:, 0:GT], func=AF.Relu), reads=[BK(pb)], writes=[rl])
                        P.op('dve', lambda E, jh=jh: E.tensor_tensor(out=hid.ap[:, jh, :], in0=rl.ap, in1=rl.ap, op=ALU.mult), reads=[rl], writes=[(hid, jh * GT, (jh + 1) * GT)])
                for half in range(2):
                    for j8 in range(4):
                        fb = f2r[f2_rr % 2]; f2_rr += 1
                        P.dma('sp', fb.ap, wsc_ff2[j8 * 1024:(j8 + 1) * 1024, half * 512:(half + 1) * 512].rearrange("(jj p) n -> p jj n", p=128), reads=WFF2, writes=[fb])
                        for jj in range(8):
                            jh = j8 * 8 + jj
                            for tl in range(TPG):
                                P.op('pe', lambda E, fb=fb, jj=jj, jh=jh, tl=tl: E.matmul(banks[4 + tl][0:TP, :], lhsT=hid.ap[:, jh, tl * TP:(tl + 1) * TP], rhs=fb.ap[:, jj, :],
                                                                                     start=(jh == 0), stop=(jh == 31)),
                                     reads=[(hid, jh * GT, (jh + 1) * GT), fb], writes=[BK(4 + tl)])
                    for tl in range(TPG):
                        xb = xg[tl]
                        P.op('dve', lambda E, half=half, tl=tl: E.tensor_tensor(out=tmp5.ap[0:TP, :], in0=banks[4 + tl][0:TP, :], in1=g2bc.ap[0:TP, half * 512:(half + 1) * 512], op=ALU.mult),
                             reads=[BK(4 + tl), g2bc], writes=[tmp5])
                        P.op('dve', lambda E, half=half, xb=xb: E.tensor_tensor(out=xb.ap[0:TP, half * 512:(half + 1) * 512], in0=xb.ap[0:TP, half * 512:(half + 1) * 512], in1=tmp5.ap[0:TP, :], op=ALU.add),
                             reads=[tmp5, xb], writes=[xb])
                for tl in range(TPG):
                    tt = g * TPG + tl
                    xb = xg[tl]
                    P.op('act', lambda E, xb=xb, tl=tl: E.activation(out=junk3.ap[0:TP, :], in_=xb.ap[0:TP, :], func=AF.Square, accum_out=ss.ap[0:TP, 8 + tl:9 + tl]),
                         reads=[xb], writes=[junk3, (ss, 8 + tl, 9 + tl)])
                    P.op('dve', lambda E, tl=tl: E.tensor_scalar(out=rs.ap[0:TP, 8 + tl:9 + tl], in0=ss.ap[0:TP, 8 + tl:9 + tl], scalar1=1.0 / D, scalar2=EPS, op0=ALU.mult, op1=ALU.add),
                         reads=[(ss, 8 + tl, 9 + tl)], writes=[(rs, 8 + tl, 9 + tl)])
                    P.op('act', lambda E, tl=tl: E.activation(out=rs.ap[0:TP, 8 + tl:9 + tl], in_=rs.ap[0:TP, 8 + tl:9 + tl], func=AF.Sqrt), reads=[(rs, 8 + tl, 9 + tl)], writes=[(rs, 8 + tl, 9 + tl)])
                    P.op('dve', lambda E, tl=tl: E.reciprocal(out=rs.ap[0:TP, 8 + tl:9 + tl], in_=rs.ap[0:TP, 8 + tl:9 + tl]), reads=[(rs, 8 + tl, 9 + tl)], writes=[(rs, 8 + tl, 9 + tl)])
                    P.op('dve', lambda E, xb=xb, tl=tl: E.scalar_tensor_tensor(out=xb.ap[0:TP, :], in0=xb.ap[0:TP, :], scalar=rs.ap[0:TP, 8 + tl:9 + tl], in1=fgbc.ap[0:TP, :], op0=ALU.mult, op1=ALU.mult),
                         reads=[xb, (rs, 8 + tl, 9 + tl), fgbc], writes=[xb])
                    P.dma('pool', o_y[tt * TP:(tt + 1) * TP, :], xb.ap[0:TP, :], reads=[xb], writes=[('o_y', j, tt)])

        for U in units:
            do_unit(U)
        P.finish()
        print("instructions:", P.nins, {e: len(v) for e, v in P.ops.items()})
        P.run_block()
    return nc


def _consts():
    bf = ml_dtypes.bfloat16
    sl = np.array([2.0 ** (-(h + 1)) for h in range(8)], np.float64)
    s_loc = np.arange(128)[:, None, None]
    t_loc = np.arange(128)[None, None, :]
    cdiag = (-2.0 * sl[None, :, None] * np.maximum(s_loc - t_loc, 0)).astype(np.float32)
    out = {"c_cdiag": cdiag.astype(bf)}
    s_loc1 = np.arange(128)[:, None, None].astype(np.float64)
    dl = (np.arange(36) - 32)[None, None, :].astype(np.float64)
    out["c_btab"] = (sl[None, :, None] * (s_loc1 + 128.0 * dl)).astype(np.float32)
    return out


_NC_CACHE = {}


def kernel(x_prompt, x_sample, c_prompt, c_sample, cache_k, cache_v, cache_kidx, state_conv,
           w_ada, b_ada, norm1_g, w_in, conv_w, conv_b, conv_ln_g, conv_ln_b, w_out, norm2_g,
           w_ff1, w_ff2, final_g):
    f = lambda a: np.ascontiguousarray(np.asarray(a, dtype=np.float32))
    x_prompt, x_sample, c_prompt, c_sample = f(x_prompt), f(x_sample), f(c_prompt), f(c_sample)
    cache_k, cache_v, cache_kidx, state_conv = f(cache_k)[0], f(cache_v)[0], f(cache_kidx)[0], f(state_conv)[0]
    if 'nc' not in _NC_CACHE:
        _NC_CACHE['nc'] = build()
    nc = _NC_CACHE['nc']
    consts = _consts()
    vecT = lambda v, n: np.ascontiguousarray(f(v).reshape(n, 128).T)
    shared = {
        "w_ada": f(w_ada)[0], "b_adaT": vecT(f(b_ada)[0], 48), "n1gT": vecT(f(norm1_g)[0], 8), "n2gT": vecT(f(norm2_g)[0], 8),
        "fgbc": np.ascontiguousarray(np.broadcast_to(f(final_g)[None, :], (128, D))),
        "w_in": f(w_in)[0], "w_out": f(w_out)[0], "w_ff1": f(w_ff1)[0], "w_ff2": f(w_ff2)[0],
        "convwT": np.ascontiguousarray(f(conv_w)[0].reshape(31, 4, 128).transpose(2, 1, 0)),
        "convbT": vecT(f(conv_b)[0], 4), "lngT": vecT(f(conv_ln_g)[0], 4), "lnbT": vecT(f(conv_ln_b)[0], 4),
    }
    shared.update(consts)
    in_maps = []
    for c in range(8):
        sl_ = slice(2 * c, 2 * c + 2)
        cc = np.concatenate([c_prompt[sl_], c_sample[sl_]], axis=0)
        m = dict(shared)
        m["xp"] = np.ascontiguousarray(x_prompt[sl_])
        m["xs"] = np.ascontiguousarray(x_sample[sl_])
        m["cT"] = np.ascontiguousarray(cc.reshape(4, 8, 128).transpose(2, 1, 0))
        m["ckT"] = np.ascontiguousarray(cache_k[sl_].transpose(0, 2, 3, 1))
        m["cv"] = np.ascontiguousarray(cache_v[sl_].reshape(2, PAST, 512))
        m["ckiT"] = np.ascontiguousarray(cache_kidx[sl_].transpose(0, 2, 1))
        m["scT"] = np.ascontiguousarray(state_conv[sl_].transpose(0, 2, 1))
        in_maps.append(m)
    res = run_bass_kernel_spmd(nc, in_maps, core_ids=list(range(8)))
    R = res.results
    cat = lambda k: np.concatenate([r[k] for r in R], axis=0)
    y_prompt = cat("yp"); y_sample = cat("ys")
    k_prompt = cat("kp").reshape(1, 16, SEQ, 8, 64); v_prompt = cat("vp").reshape(1, 16, SEQ, 8, 64)
    kidx_prompt = cat("kip").reshape(1, 16, SEQ, 64); conv_prompt = cat("cp").reshape(1, 16, 30, 512)
    k_sample = cat("ks").reshape(1, 16, DSEQ, 8, 64); v_sample = cat("vs").reshape(1, 16, DSEQ, 8, 64)
    kidx_sample = cat("kis").reshape(1, 16, DSEQ, 64); conv_sample = cat("cs").reshape(1, 16, 30, 512)
    return (y_prompt, y_sample, k_prompt, v_prompt, kidx_prompt, conv_prompt, k_sample, v_sample, kidx_sample, conv_sample)
```

```python
import numpy as np
import ml_dtypes
import concourse.bass as bass
import concourse.mybir as mybir
from concourse.bass_utils import run_bass_kernel_spmd
from contextlib import ExitStack

F32 = mybir.dt.float32
BF16 = mybir.dt.bfloat16
AF = mybir.ActivationFunctionType
ALU = mybir.AluOpType

ENGS = ['pe', 'act', 'dve', 'pool', 'sp']
EPOCH = 12000
NDMA_SEM = 22
BLK = 256

D = 1024
SEQ = 2048
DSEQ = 64
PAST = 4096
NKS = PAST + DSEQ
PROJ = 3144
EPS = 1e-6
NITER = 18
PIPE = True
BIS_YIELDS = 8
BIS_B = 8.0


class Buf:
    def __init__(self, ap, off, nbytes, esz):
        self.ap, self.off, self.nbytes, self.esz = ap, off, nbytes, esz

    def keys(self, lo=None, hi=None):
        if lo is None:
            a, b = self.off, self.off + self.nbytes
        else:
            a, b = self.off + lo * self.esz, self.off + hi * self.esz
        return [('sb', i) for i in range(a // BLK, (b - 1) // BLK + 1)]


def _expand(items):
    out = []
    for it in items:
        if isinstance(it, Buf):
            out.extend(it.keys())
        elif isinstance(it, tuple) and len(it) == 3 and isinstance(it[0], Buf):
            out.extend(it[0].keys(it[1], it[2]))
        else:
            out.append(it)
    return out


class Prog:
    def __init__(self, nc, stack):
        self.nc = nc
        self.stack = stack
        self.ops = {e: [] for e in ENGS}
        self.cnt = {e: 0 for e in ENGS}
        self.sems = {e: [] for e in ENGS}
        self.dma_sems = [stack.enter_context(nc.semaphore(f"dq{i}")) for i in range(NDMA_SEM)]
        self.dma_val = [0] * NDMA_SEM
        self.dma_rr = 0
        self.dma_rr_pool = 0
        self.waited = {e: {} for e in ENGS}
        self.last_w = {}
        self.readers = {}
        self.nins = 0

    def _sem(self, e, ep):
        while len(self.sems[e]) <= ep:
            self.sems[e].append(self.stack.enter_context(self.nc.semaphore(f"s_{e}{len(self.sems[e])}")))
        return self.sems[e][ep]

    def _deps(self, eng, reads, writes, is_dma):
        deps = set()
        for r in reads:
            t = self.last_w.get(r)
            if t is not None:
                deps.add(t)
        skip_same = (eng == 'pe') and not is_dma
        for w in writes:
            t = self.last_w.get(w)
            if t is not None and not (skip_same and t[0] == 'e' and t[1] == eng):
                deps.add(t)
            for t in self.readers.get(w, ()):
                if not (skip_same and t[0] == 'e' and t[1] == eng):
                    deps.add(t)
        return deps

    def _emit_waits(self, eng, deps):
        best = {}
        for t in deps:
            key = (t[0], t[1], t[2])
            if best.get(key, 0) < t[3]:
                best[key] = t[3]
        for key, val in best.items():
            if self.waited[eng].get(key, 0) >= val:
                continue
            self.waited[eng][key] = val
            sem = self._sem(key[1], key[2]) if key[0] == 'e' else self.dma_sems[key[1]]
            self.ops[eng].append(lambda E, sem=sem, val=val: E.wait_ge(sem, val))

    def _commit(self, tok, reads, writes):
        for r in reads:
            self.readers.setdefault(r, []).append(tok)
        for w in writes:
            self.last_w[w] = tok
            self.readers[w] = []

    def op(self, eng, fn, reads=(), writes=()):
        reads, writes = _expand(reads), _expand(writes)
        self._emit_waits(eng, self._deps(eng, reads, writes, False))
        n = self.cnt[eng]
        ep, idx = divmod(n, EPOCH)
        self.cnt[eng] = n + 1
        sem = self._sem(eng, ep)
        self.ops[eng].append(lambda E, fn=fn, sem=sem: fn(E).then_inc(sem, 1))
        tok = ('e', eng, ep, idx + 1)
        self._commit(tok, reads, writes)
        self.nins += 1
        return tok

    def dma(self, q, out, in_, reads=(), writes=(), **kw):
        reads, writes = _expand(reads), _expand(writes)
        half = NDMA_SEM // 2
        if q == 'pool':
            k = half + self.dma_rr_pool
            self.dma_rr_pool = (self.dma_rr_pool + 1) % (NDMA_SEM - half)
        else:
            k = self.dma_rr
            self.dma_rr = (k + 1) % half
        deps = self._deps(q, reads, writes, True)
        if self.dma_val[k] > 0:
            deps.add(('d', k, 0, self.dma_val[k]))
        self._emit_waits(q, deps)
        self.dma_val[k] += 16
        sem = self.dma_sems[k]
        self.ops[q].append(lambda E, sem=sem, out=out, in_=in_, kw=kw: E.dma_start(out=out, in_=in_, **kw).then_inc(sem, 16))
        tok = ('d', k, 0, self.dma_val[k])
        self._commit(tok, reads, writes)
        self.nins += 1
        return tok

    def finish(self):
        deps = set()
        for k in range(NDMA_SEM):
            if self.dma_val[k] > 0:
                deps.add(('d', k, 0, self.dma_val[k]))
        for e in ['pe', 'act', 'dve', 'pool']:
            n = self.cnt[e]
            if n > 0:
                ep, idx = divmod(n - 1, EPOCH)
                deps.add(('e', e, ep, idx + 1))
        self._emit_waits('sp', deps)

    def run_block(self):
        nc = self.nc
        ops = self.ops
        with nc.Block() as block:
            @block.sync
            def _(E):
                for f in ops['sp']:
                    f(E)

            @block.tensor
            def _(E):
                for f in ops['pe']:
                    f(E)

            @block.scalar
            def _(E):
                for f in ops['act']:
                    f(E)

            @block.vector
            def _(E):
                for f in ops['dve']:
                    f(E)

            @block.gpsimd
            def _(E):
                for f in ops['pool']:
                    f(E)


class Region:
    def __init__(self, base, size):
        self.base, self.size, self.off = base, size, 0

    def reset(self):
        self.off = 0

    def alloc(self, nbytes):
        al = BLK if nbytes >= 1024 else 32
        start = (self.base + self.off + al - 1) // al * al
        self.off = start - self.base + (nbytes + al - 1) // al * al
        assert self.off <= self.size, (self.off, self.size)
        return start


FM_COLS = ([(c * 128, 128) for c in range(4)] + [(512 + c * 128, 128) for c in range(4)] +
           [(1536 + c * 128, 128) for c in range(4)] + [(2048, 64)] +
           [(2120 + c * 128, 128) for c in range(4)] + [(2632 + c * 128, 128) for c in range(4)])

R_P_SZ = 10 * 1024
R_U_SZ = 90 * 1024
R_UW = (90 + 52) * 1024
R_U_K = [74 * 1024, 90 * 1024] if PIPE else [90 * 1024, 90 * 1024]
R_M_SZ = 32 * 1024
R_W_SZ = 70 * 1024
ARENA = R_P_SZ + R_U_SZ + R_M_SZ + R_W_SZ


def build(phases=(1, 2, 3)):
    nc = bass.Bass("TRN2", target_bir_lowering=False)
    din = lambda name, shape, dt=F32: nc.dram_tensor(name, list(shape), dt, kind="ExternalInput").ap()
    dout = lambda name, shape: nc.dram_tensor(name, list(shape), F32, kind="ExternalOutput").ap()
    dint = lambda name, shape, dt=BF16: nc.dram_tensor(name, list(shape), dt, kind="Internal").ap()

    xp = din("xp", [2, SEQ, D]); xs = din("xs", [2, DSEQ, D])
    cT_d = din("cT", [128, 8, 4])
    ckT = din("ckT", [2, 8, 64, PAST]); cv = din("cv", [2, PAST, 512]); ckiT = din("ckiT", [2, 64, PAST])
    scT_d = din("scT", [2, 512, 30])
    w_ada = din("w_ada", [D, 6 * D]); b_adaT = din("b_adaT", [128, 48])
    n1gT_d = din("n1gT", [128, 8]); n2gT_d = din("n2gT", [128, 8]); fgbc_d = din("fgbc", [128, D])
    w_in = din("w_in", [D, PROJ]); w_out = din("w_out", [D, D]); w_ff1 = din("w_ff1", [D, 4 * D]); w_ff2 = din("w_ff2", [4 * D, D])
    convwT_d = din("convwT", [128, 4, 31]); convbT_d = din("convbT", [128, 4]); lngT_d = din("lngT", [128, 4]); lnbT_d = din("lnbT", [128, 4])
    c_cdiag = din("c_cdiag", [128, 8, 128], BF16)
    c_btab = din("c_btab", [128, 8, 36])

    o_yp = dout("yp", [2, SEQ, D]); o_ys = dout("ys", [2, DSEQ, D])
    o_kp = dout("kp", [2, SEQ, 512]); o_vp = dout("vp", [2, SEQ, 512]); o_kip = dout("kip", [2, SEQ, 64]); o_cp = dout("cp", [2, 30, 512])
    o_ks = dout("ks", [2, DSEQ, 512]); o_vs = dout("vs", [2, DSEQ, 512]); o_kis = dout("kis", [2, DSEQ, 64]); o_cs = dout("cs", [2, 30, 512])

    wsc_fm = dint("wsc_fm", [21, 128, 8, 128])
    wsc_tm = dint("wsc_tm", [D, 1096])
    wsc_out = dint("wsc_out", [D, D])
    wsc_ff1 = dint("wsc_ff1", [32, 128, 8, 128])
    wsc_ff2 = dint("wsc_ff2", [4 * D, D])

    with ExitStack() as st:
        P = Prog(nc, st)
        arena = st.enter_context(nc.sbuf_tensor("arena", [128, ARENA // 2], BF16))
        banks = [st.enter_context(nc.psum_tensor(f"bank{i}", [128, 512], F32)) for i in range(8)]
        BK = lambda i: ('ps', i)
        bkb = lambda i: banks[i][:, :].bitcast(BF16)

        if True:
            RP = Region(0, R_P_SZ); RU = Region(R_P_SZ, R_U_SZ); RM = Region(R_P_SZ + R_U_SZ, R_M_SZ)
            RW = Region(R_P_SZ + R_U_SZ + R_M_SZ, R_W_SZ)

        def mk(reg, shape, dt):
            esz = 4 if dt == F32 else 2
            n = int(np.prod(shape))
            off = reg.alloc(n * esz)
            ap = arena[:, off // 2: off // 2 + n * esz // 2]
            if dt == F32:
                ap = ap.bitcast(F32)
            if len(shape) == 2:
                ap = ap.rearrange("p (a b) -> p a b", b=shape[1])
            elif len(shape) == 3:
                ap = ap.rearrange("p (a b c) -> p a b c", b=shape[1], c=shape[2])
            return Buf(ap, off, n * esz, esz)

        identf = mk(RP, [128], F32); identb = mk(RP, [128], BF16)
        ones64b = mk(RP, [64], BF16); onesb = mk(RP, [128], BF16); onesf = mk(RP, [128], F32)
        cdiag = mk(RP, [8, 128], BF16); btab = mk(RP, [8, 36], F32)
        modT = mk(RP, [48, 4], F32)
        cTs = mk(RP, [8, 4], F32); scs = mk(RP, [8, 4], F32)
        badaT = mk(RP, [48], F32); n1gT = mk(RP, [8], F32); n2gT = mk(RP, [8], F32)
        convwT = mk(RP, [4, 31], F32); convbT = mk(RP, [4], F32); lngT = mk(RP, [4], F32); lnbT = mk(RP, [4], F32)
        G1T = mk(RP, [8], F32); S1T = mk(RP, [8], F32); G2T = mk(RP, [8], F32); S2T = mk(RP, [8], F32)
        g1T = mk(RP, [8], F32); g2T = mk(RP, [8], F32)
        ss = mk(RP, [16], F32); rs = mk(RP, [16], F32)
        sacc = mk(RP, [2], F32); dd = mk(RP, [2], F32); negmid = mk(RP, [2], F32); thr = mk(RP, [2], F32)
        dgt = mk(RP, [128], F32)

        ident_tok = None

        for ci, (c0, cw) in enumerate(FM_COLS):
            if cw == 128:
                P.dma('pool', wsc_fm[ci].rearrange("p kc n -> kc p n"),
                      w_in[:, c0:c0 + 128].rearrange("(kc p) n -> kc p n", p=128), writes=[('wfm', ci)])
            else:
                for hh in range(2):
                    P.dma('pool', wsc_fm[ci][:, :, hh * 64:(hh + 1) * 64].rearrange("p kc n -> kc p n"),
                          w_in[:, c0:c0 + 64].rearrange("(kc p) n -> kc p n", p=128), writes=[('wfm', ci, hh)])
        P.dma('pool', wsc_tm[:, 0:1024], w_in[:, 512:1536], writes=['wtm_a'])
        P.dma('pool', wsc_tm[:, 1024:1096], w_in[:, 2048:2120], writes=['wtm_b'])
        for i in range(4):
            P.dma('pool', wsc_out[i * 256:(i + 1) * 256, :], w_out[i * 256:(i + 1) * 256, :], writes=[('wout', i)])
        for j in range(32):
            P.dma('pool', wsc_ff1[j].rearrange("p kc n -> kc p n"),
                  w_ff1[:, j * 128:(j + 1) * 128].rearrange("(kc p) n -> kc p n", p=128), writes=[('wff1', j)])
        for i in range(16):
            P.dma('pool', wsc_ff2[i * 256:(i + 1) * 256, :], w_ff2[i * 256:(i + 1) * 256, :], writes=[('wff2', i)])
        WFM = [('wfm', ci) for ci in range(21)] + [('wfm', 12, 0), ('wfm', 12, 1)]
        WOUT = [('wout', i) for i in range(4)]
        WFF1 = [('wff1', j) for j in range(32)]
        WFF2 = [('wff2', i) for i in range(16)]

        P.op('dve', lambda E: E.memset(identf.ap, 0.0), writes=[identf])
        P.op('pool', lambda E: E.affine_select(out=identf.ap, in_=identf.ap, pattern=[[-1, 128]], compare_op=ALU.not_equal,
                                                 fill=1.0, base=0, channel_multiplier=1), reads=[identf], writes=[identf])
        P.op('dve', lambda E: E.tensor_copy(out=identb.ap, in_=identf.ap), reads=[identf], writes=[identb])
        P.op('dve', lambda E: E.memset(onesb.ap, 1.0), writes=[onesb])
        P.op('dve', lambda E: E.memset(onesf.ap, 1.0), writes=[onesf])
        for (b, d_) in [(cdiag, c_cdiag), (btab, c_btab), (cTs, cT_d), (badaT, b_adaT), (n1gT, n1gT_d), (n2gT, n2gT_d), (convwT, convwT_d),
                        (convbT, convbT_d), (lngT, lngT_d), (lnbT, lnbT_d)]:
            P.dma('sp', b.ap, d_, writes=[b])

        P.op('act', lambda E: E.activation(out=scs.ap, in_=cTs.ap, func=AF.Silu), reads=[cTs], writes=[scs])
        RU.reset()
        wada_b = [mk(RU, [8, 1024], F32), mk(RU, [8, 1024], F32)]
        modps = banks[0][:, 0:192].rearrange("p (a b) -> p a b", b=4)
        for i in range(6):
            wb_ = wada_b[i % 2]
            P.dma('sp', wb_.ap, w_ada[:, i * 1024:(i + 1) * 1024].rearrange("(kc p) n -> p kc n", p=128), writes=[wb_])
            for cl in range(8):
                ch = i * 8 + cl
                for kc in range(8):
                    P.op('pe', lambda E, wb_=wb_, cl=cl, kc=kc, ch=ch: E.matmul(modps[:, ch, :], lhsT=wb_.ap[:, kc, cl * 128:(cl + 1) * 128],
                                                                             rhs=scs.ap[:, kc, :], start=(kc == 0), stop=(kc == 7)),
                         reads=[wb_, scs], writes=[BK(0)])
        P.op('dve', lambda E: E.tensor_tensor(out=modT.ap, in0=modps, in1=badaT.ap.unsqueeze(2).to_broadcast([128, 48, 4]), op=ALU.add),
             reads=[BK(0), badaT], writes=[modT])

        def bcast_vec(srcT, dst):
            for half in range(2):
                for q in range(4):
                    kc = half * 4 + q
                    P.op('dve', lambda E, kc=kc: E.tensor_scalar(out=dgt.ap, in0=identf.ap, scalar1=srcT.ap[:, kc:kc + 1], scalar2=None, op0=ALU.mult),
                         reads=[identf, srcT], writes=[dgt])
                    P.op('pe', lambda E, q=q: E.matmul(banks[1][:, q * 128:(q + 1) * 128], lhsT=onesf.ap, rhs=dgt.ap, start=True, stop=True),
                         reads=[onesf, dgt], writes=[BK(1)])
                P.op('act', lambda E, half=half: E.activation(out=dst.ap[:, half * 512:(half + 1) * 512], in_=banks[1][:, :], func=AF.Copy),
                     reads=[BK(1)], writes=[dst])

        units = [dict(kind=0, j=0, sq=0), dict(kind=0, j=1, sq=1), dict(kind=1, j=2, sq=0), dict(kind=1, j=3, sq=1)]
        def do_unit(U):
            kind, j, sq = U['kind'], U['j'], U['sq']
            T = SEQ if kind == 0 else DSEQ
            TP = 128 if kind == 0 else 64
            NT = T // TP
            GT = 512 if kind == 0 else 64
            NG = T // GT
            TPG = GT // TP
            NK = SEQ if kind == 0 else NKS
            NKB = (NK + 127) // 128
            KOFF = 0 if kind == 0 else PAST
            x_d = xp[sq] if kind == 0 else xs[sq]
            o_y = o_yp[sq] if kind == 0 else o_ys[sq]
            o_k = o_kp[sq] if kind == 0 else o_ks[sq]
            o_v = o_vp[sq] if kind == 0 else o_vs[sq]
            o_ki = o_kip[sq] if kind == 0 else o_kis[sq]
            o_c = o_cp[sq] if kind == 0 else o_cs[sq]
            UE = 30 + T

            for (dst, gsrc, off) in [(G1T, n1gT, 8), (G2T, n2gT, 32)]:
                P.op('dve', lambda E, dst=dst, gsrc=gsrc, off=off: E.scalar_tensor_tensor(out=dst.ap, in0=modT.ap[:, off:off + 8, j], scalar=1.0, in1=gsrc.ap,
                                                                                      op0=ALU.add, op1=ALU.mult), reads=[modT, gsrc], writes=[dst])
            for (dst, off) in [(S1T, 0), (S2T, 24), (g1T, 16), (g2T, 40)]:
                P.op('dve', lambda E, dst=dst, off=off: E.tensor_copy(out=dst.ap, in_=modT.ap[:, off:off + 8, j]), reads=[modT], writes=[dst])

            RU.reset(); RM.reset(); RW.reset()
            QA = mk(RU, [8, T], BF16); KA = mk(RU, [4, NK], BF16); Vb = mk(RU, [NKB, 8, 65], BF16)
            KIz = [mk(RU, [NK], BF16) for _ in range(2)]; QIT = mk(RU, [4, T], BF16)
            Wt = mk(RU, [16, 8], F32)
            mixT = mk(RM, [8, T], BF16)
            uT = mk(RW, [4, UE], BF16)

            P.op('pool', lambda E: E.memset(QA.ap, 0.0), writes=[QA])
            P.op('pool', lambda E: E.memset(Vb.ap, 1.0), writes=[Vb])
            P.op('pool', lambda E: E.memset(KIz[0].ap[64:128, :], 0.0), writes=[KIz[0]])
            P.op('pool', lambda E: E.memset(KIz[1].ap[0:64, :], 0.0), writes=[KIz[1]])

            RM.reset()
            wtm = mk(RM, [8, 1096], BF16); hT0 = mk(RM, [8, GT], BF16)
            fmr = [mk(RM, [8, 128], BF16) for _ in range(3)]
            xt = [mk(RW, [D], F32) for _ in range(2)]
            kout = mk(RW, [512], F32); vout = mk(RW, [512], F32); kiw = mk(RW, [72], F32)
            sig = mk(RW, [GT], F32); junk = mk(RW, [D], BF16)
            ulast = mk(RW, [4, 32], F32); cst = mk(RW, [512], F32)
            hTs = [hT0, mk(RW, [8, GT], BF16)]

            P.dma('sp', wtm.ap[:, :, 0:1024], wsc_tm[:, 0:1024].rearrange("(kc p) n -> p kc n", p=128), reads=['wtm_a'], writes=[wtm])
            P.dma('sp', wtm.ap[:, :, 1024:1096], wsc_tm[:, 1024:1096].rearrange("(kc p) n -> p kc n", p=128), reads=['wtm_b'], writes=[wtm])
            if kind == 0:
                P.op('pool', lambda E: E.memset(uT.ap[:, :, 0:30], 0.0), writes=[uT])
            else:
                stg_off = [RW.base + RW.size - 2 * PAST * 4, RW.base + RW.size - PAST * 4]
                stg = [Buf(arena[:, o_ // 2:o_ // 2 + PAST * 2].bitcast(F32), o_, PAST * 4, 4) for o_ in stg_off]
                P.dma('sp', stg[0].ap[0:64, :], ckiT[sq], writes=[stg[0]])
                P.dma('sp', stg[0].ap[64:128, :], ckiT[sq], writes=[stg[0]])
                P.dma('sp', stg[1].ap, ckT[sq, 0:2].rearrange("a d n -> (a d) n"), writes=[stg[1]])

                def load_kidx():
                    P.op('act', lambda E: E.activation(out=KIz[0].ap[0:64, 0:PAST], in_=stg[0].ap[0:64, :], func=AF.Copy), reads=[stg[0]], writes=[(KIz[0], 0, PAST)])
                    P.op('act', lambda E: E.activation(out=KIz[1].ap[64:128, 0:PAST], in_=stg[0].ap[64:128, :], func=AF.Copy), reads=[stg[0]], writes=[(KIz[1], 0, PAST)])

                def gen_load():
                    items = [('k', 1), ('k', 2), ('k', 3), ('v', 0), ('v', 1), ('v', 2), ('v', 3)]
                    loaded = [('k', 0, stg[1])]
                    free = [stg[0]]
                    ci = 0
                    while loaded or items:
                        if items and free:
                            typ, idx = items.pop(0)
                            sb2 = free.pop(0)
                            if typ == 'k':
                                P.dma('sp', sb2.ap, ckT[sq, 2 * idx:2 * idx + 2].rearrange("a d n -> (a d) n"), writes=[sb2])
                            else:
                                P.dma('sp', sb2.ap.rearrange("p (a f) -> p a f", f=512), cv[sq][idx * 1024:(idx + 1) * 1024, :].rearrange("(kb p) f -> p kb f", p=128), writes=[sb2])
                            loaded.append((typ, idx, sb2))
                        typ, idx, sb2 = loaded.pop(0)
                        eng_ = 'dve' if ci % 2 == 0 else 'act'
                        ci += 1
                        if typ == 'k':
                            if eng_ == 'dve':
                                P.op('dve', lambda E, sb2=sb2, idx=idx: E.tensor_copy(out=KA.ap[:, idx, 0:PAST], in_=sb2.ap), reads=[sb2], writes=[(KA, idx * NK, idx * NK + PAST)])
                            else:
                                P.op('act', lambda E, sb2=sb2, idx=idx: E.activation(out=KA.ap[:, idx, 0:PAST], in_=sb2.ap, func=AF.Copy), reads=[sb2], writes=[(KA, idx * NK, idx * NK + PAST)])
                        else:
                            outv = Vb.ap[:, idx * 8:(idx + 1) * 8, :, 0:64].rearrange("p a h d -> p (a h) d")
                            inv = sb2.ap.rearrange("p (ah d) -> p ah d", d=64)
                            if eng_ == 'dve':
                                P.op('dve', lambda E, outv=outv, inv=inv: E.tensor_copy(out=outv, in_=inv), reads=[sb2], writes=[(Vb, idx * 8 * 520, (idx + 1) * 8 * 520)])
                            else:
                                P.op('act', lambda E, outv=outv, inv=inv: E.activation(out=outv, in_=inv, func=AF.Copy), reads=[sb2], writes=[(Vb, idx * 8 * 520, (idx + 1) * 8 * 520)])
                        free.append(sb2)
                        yield
                P.dma('pool', uT.ap[:, :, 0:30], scT_d[sq].rearrange("(c p) t -> p c t", p=128), writes=[uT])
            P.op('pool', lambda E: E.memset(ulast.ap, 0.0), writes=[ulast])

            fm_rr = 0
            fmst = dict(rr=0)
            def p1_pro(g):
                hT = hTs[g % 2]
                for tl in range(TPG):
                    tt = g * TPG + tl
                    xb = xt[tt % 2]
                    P.dma('sp', xb.ap[0:TP, :], x_d[tt * TP:(tt + 1) * TP, :], writes=[xb])
                    P.op('act', lambda E, xb=xb, tt=tt: E.activation(out=junk.ap[0:TP, :], in_=xb.ap[0:TP, :], func=AF.Square, accum_out=ss.ap[0:TP, tt:tt + 1]),
                         reads=[xb], writes=[junk, (ss, tt, tt + 1)])
                    P.op('dve', lambda E, tt=tt: E.tensor_scalar(out=rs.ap[0:TP, tt:tt + 1], in0=ss.ap[0:TP, tt:tt + 1], scalar1=1.0 / D, scalar2=EPS, op0=ALU.mult, op1=ALU.add),
                         reads=[(ss, tt, tt + 1)], writes=[(rs, tt, tt + 1)])
                    P.op('act', lambda E, tt=tt: E.activation(out=rs.ap[0:TP, tt:tt + 1], in_=rs.ap[0:TP, tt:tt + 1], func=AF.Sqrt),
                         reads=[(rs, tt, tt + 1)], writes=[(rs, tt, tt + 1)])
                    P.op('dve', lambda E, tt=tt: E.reciprocal(out=rs.ap[0:TP, tt:tt + 1], in_=rs.ap[0:TP, tt:tt + 1]),
                         reads=[(rs, tt, tt + 1)], writes=[(rs, tt, tt + 1)])
                    P.op('dve', lambda E, xb=xb, tt=tt: E.tensor_scalar(out=xb.ap[0:TP, :], in0=xb.ap[0:TP, :], scalar1=rs.ap[0:TP, tt:tt + 1], scalar2=None, op0=ALU.mult),
                         reads=[xb, (rs, tt, tt + 1)], writes=[xb])
                    for kc in range(8):
                        bk = kc // 4
                        P.op('pe', lambda E, xb=xb, kc=kc, bk=bk: E.transpose(banks[bk][:, (kc % 4) * 128:(kc % 4) * 128 + TP], xb.ap[0:TP, kc * 128:(kc + 1) * 128], identf.ap[0:TP, 0:TP]),
                             reads=[xb, identf], writes=[BK(bk)])
                    for kc in range(8):
                        bk = kc // 4
                        P.op('act', lambda E, kc=kc, bk=bk, tl=tl: E.activation(out=hT.ap[:, kc, tl * TP:(tl + 1) * TP], in_=banks[bk][:, (kc % 4) * 128:(kc % 4) * 128 + TP],
                                                                        func=AF.Identity, scale=G1T.ap[:, kc:kc + 1], bias=S1T.ap[:, kc:kc + 1]),
                             reads=[BK(bk), G1T, S1T], writes=[(hT, kc * GT + tl * TP, kc * GT + (tl + 1) * TP)])
            def p1_fm(g):
                hT = hTs[g % 2]
                order = [0, 1, 2, 3, 4, 5, 6, 7, 8, 9, 10, 11, 12, 17, 13, 18, 14, 19, 15, 20, 16]
                t0 = g * GT
                for ci in order:
                    wb_ = fmr[fmst['rr'] % 3]
                    pb = 2 + (fmst['rr'] % 3)
                    fmst['rr'] += 1
                    P.dma('sp', wb_.ap, wsc_fm[ci], reads=WFM, writes=[wb_])
                    for kc in range(8):
                        P.op('pe', lambda E, wb_=wb_, kc=kc, pb=pb: E.matmul(banks[pb][:, 0:GT], lhsT=wb_.ap[:, kc, :], rhs=hT.ap[:, kc, :], start=(kc == 0), stop=(kc == 7)),
                             reads=[wb_, hT], writes=[BK(pb)])
                    src = banks[pb][:, 0:GT]
                    if ci < 4:
                        c = ci
                        P.op('act', lambda E, src=src, c=c, t0=t0: E.activation(out=QA.ap[0:64, 2 * c, t0:t0 + GT], in_=src[0:64, :], func=AF.Copy, scale=0.125),
                             reads=[BK(pb)], writes=[(QA, 2 * c * T + t0, 2 * c * T + t0 + GT)])
                        P.op('act', lambda E, src=src, c=c, t0=t0: E.activation(out=QA.ap[64:128, 2 * c + 1, t0:t0 + GT], in_=src[64:128, :], func=AF.Copy, scale=0.125),
                             reads=[BK(pb)], writes=[((QA, (2 * c + 1) * T + t0, (2 * c + 1) * T + t0 + GT))])
                    elif ci < 8:
                        c = ci - 4
                        P.op('dve', lambda E, src=src, c=c, t0=t0: E.tensor_copy(out=KA.ap[:, c, KOFF + t0:KOFF + t0 + GT], in_=src),
                             reads=[BK(pb)], writes=[(KA, c * NK + KOFF + t0, c * NK + KOFF + t0 + GT)])
                    elif ci < 12:
                        c = ci - 8
                        P.op('act', lambda E, src=src, c=c, t0=t0: E.activation(out=QIT.ap[:, c, t0:t0 + GT], in_=src, func=AF.Copy),
                             reads=[BK(pb)], writes=[(QIT, c * T + t0, c * T + t0 + GT)])
                    elif ci == 12:
                        P.op('dve', lambda E, src=src, t0=t0: E.tensor_copy(out=KIz[0].ap[0:64, KOFF + t0:KOFF + t0 + GT], in_=src[0:64, :]),
                             reads=[BK(pb)], writes=[(KIz[0], KOFF + t0, KOFF + t0 + GT)])
                        P.op('dve', lambda E, src=src, t0=t0: E.tensor_copy(out=KIz[1].ap[64:128, KOFF + t0:KOFF + t0 + GT], in_=src[64:128, :]),
                             reads=[BK(pb)], writes=[(KIz[1], KOFF + t0, KOFF + t0 + GT)])
                    elif ci >= 17:
                        P.op('act', lambda E, src=src: E.activation(out=sig.ap, in_=src, func=AF.Sigmoid), reads=[BK(pb)], writes=[sig])
                    else:
                        c = ci - 13
                        P.op('dve', lambda E, src=src, c=c, t0=t0: E.tensor_tensor(out=uT.ap[:, c, 30 + t0:30 + t0 + GT], in0=src, in1=sig.ap, op=ALU.mult),
                             reads=[BK(pb), sig], writes=[(uT, c * UE + 30 + t0, c * UE + 30 + t0 + GT)])
                        if g == NG - 1:
                            P.op('dve', lambda E, src=src, c=c: E.tensor_tensor(out=ulast.ap[:, c, 0:30], in0=src[:, GT - 30:GT], in1=sig.ap[:, GT - 30:GT], op=ALU.mult),
                                 reads=[BK(pb), sig], writes=[ulast])
            def p1_tm(g):
                hT = hTs[g % 2]
                for tl in range(TPG):
                    tt = g * TPG + tl
                    for (pb, c0_, cw_) in [(5, 0, 512), (6, 512, 512), (7, 1024, 72)]:
                        for kc in range(8):
                            P.op('pe', lambda E, kc=kc, pb=pb, c0_=c0_, cw_=cw_, tl=tl: E.matmul(banks[pb][0:TP, 0:cw_], lhsT=hT.ap[:, kc, tl * TP:(tl + 1) * TP],
                                                                                           rhs=wtm.ap[:, kc, c0_:c0_ + cw_], start=(kc == 0), stop=(kc == 7)),
                                 reads=[hT, wtm], writes=[BK(pb)])
                    P.op('act', lambda E: E.activation(out=kout.ap[0:TP, :], in_=banks[5][0:TP, :], func=AF.Copy), reads=[BK(5)], writes=[kout])
                    P.dma('pool', o_k[tt * TP:(tt + 1) * TP, :], kout.ap[0:TP, :], reads=[kout], writes=[('o_k', j, tt)])
                    P.op('dve', lambda E: E.tensor_copy(out=vout.ap[0:TP, :], in_=banks[6][0:TP, :]), reads=[BK(6)], writes=[vout])
                    P.dma('pool', o_v[tt * TP:(tt + 1) * TP, :], vout.ap[0:TP, :], reads=[vout], writes=[('o_v', j, tt)])
                    kbv = tt if kind == 0 else 32
                    P.op('pool', lambda E, kbv=kbv: E.tensor_copy(out=Vb.ap[0:TP, kbv, :, 0:64], in_=vout.ap[0:TP, :].rearrange("p (h d) -> p h d", d=64)),
                         reads=[vout], writes=[(Vb, kbv * 520, kbv * 520 + 520)])
                    P.op('dve', lambda E: E.tensor_copy(out=kiw.ap[0:TP, :], in_=banks[7][0:TP, 0:72]), reads=[BK(7)], writes=[kiw])
                    P.dma('pool', o_ki[tt * TP:(tt + 1) * TP, :], kiw.ap[0:TP, 0:64], reads=[kiw], writes=[('o_ki', j, tt)])
                    P.op('dve', lambda E, tt=tt: E.tensor_scalar(out=Wt.ap[0:TP, tt, :], in0=kiw.ap[0:TP, 64:72], scalar1=float(1.0 / (8.0 * np.sqrt(8.0))), scalar2=None, op0=ALU.mult),
                         reads=[kiw], writes=[(Wt, tt * 8, tt * 8 + 8)])
            p1_pro(0)
            for g in range(NG):
                p1_fm(g)
                if g + 1 < NG:
                    p1_pro(g + 1)
                p1_tm(g)
            for c in range(4):
                P.op('pe', lambda E, c=c: E.transpose(banks[0][0:32, c * 128:(c + 1) * 128], ulast.ap[:, c, :], identf.ap), reads=[ulast, identf], writes=[BK(0)])
            P.op('act', lambda E: E.activation(out=cst.ap[0:32, :], in_=banks[0][0:32, :], func=AF.Copy), reads=[BK(0)], writes=[cst])
            P.dma('pool', o_c, cst.ap[0:30, :], reads=[cst], writes=[('o_c', j)])

            if 2 not in phases:
                return
            RM.reset()
            mixT = mk(RM, [8, T], BF16)
            RW.reset()
            uT = mk(RW, [4, UE], BF16)
            ysq = mk(RW, [GT], F32); mean_sb = mk(RW, [GT], F32); rstd_sb = mk(RW, [GT], F32); zt = mk(RW, [GT], F32)
            Dcv = mk(RW, [31, 128], BF16)
            cvb = 0
            for c in range(4):
                for jj in range(31):
                    P.op('dve', lambda E, c=c, jj=jj: E.tensor_scalar(out=Dcv.ap[:, jj, :], in0=identb.ap, scalar1=convwT.ap[:, c, jj:jj + 1], scalar2=None, op0=ALU.mult),
                         reads=[identb, convwT], writes=[(Dcv, jj * 128, jj * 128 + 128)])
                for g in range(NG):
                    pb = cvb % 3; cvb += 1
                    for jj in range(31):
                        P.op('pe', lambda E, c=c, jj=jj, pb=pb, g=g: E.matmul(banks[pb][:, 0:GT], lhsT=Dcv.ap[:, jj, :], rhs=uT.ap[:, c, g * GT + jj:g * GT + jj + GT],
                                                                          start=(jj == 0), stop=(jj == 30)),
                             reads=[Dcv, (uT, c * UE + g * GT, c * UE + g * GT + GT + 30)], writes=[BK(pb)])
                    P.op('act', lambda E, c=c, pb=pb, g=g: E.activation(out=mixT.ap[:, 4 + c, g * GT:(g + 1) * GT], in_=banks[pb][:, 0:GT], func=AF.Identity, bias=convbT.ap[:, c:c + 1]),
                         reads=[BK(pb), convbT], writes=[(mixT, (4 + c) * T + g * GT, (4 + c) * T + (g + 1) * GT)])
            for g in range(NG):
                for c in range(4):
                    mrange = (mixT, (4 + c) * T + g * GT, (4 + c) * T + (g + 1) * GT)
                    P.op('act', lambda E, c=c, g=g: E.activation(out=ysq.ap, in_=mixT.ap[:, 4 + c, g * GT:(g + 1) * GT], func=AF.Square), reads=[mrange], writes=[ysq])
                    P.op('pe', lambda E, c=c, g=g: E.matmul(banks[3][:, 0:GT], lhsT=onesb.ap, rhs=mixT.ap[:, 4 + c, g * GT:(g + 1) * GT], start=(c == 0), stop=(c == 3)),
                         reads=[onesb, mrange], writes=[BK(3)])
                    P.op('pe', lambda E, c=c: E.matmul(banks[4][:, 0:GT], lhsT=onesf.ap, rhs=ysq.ap, start=(c == 0), stop=(c == 3)),
                         reads=[onesf, ysq], writes=[BK(4)])
                P.op('act', lambda E: E.activation(out=mean_sb.ap, in_=banks[3][:, 0:GT], func=AF.Copy, scale=1.0 / 512), reads=[BK(3)], writes=[mean_sb])
                P.op('dve', lambda E: E.tensor_tensor(out=zt.ap, in0=mean_sb.ap, in1=mean_sb.ap, op=ALU.mult), reads=[mean_sb], writes=[zt])
                P.op('dve', lambda E: E.scalar_tensor_tensor(out=rstd_sb.ap, in0=banks[4][:, 0:GT], scalar=1.0 / 512, in1=zt.ap, op0=ALU.mult, op1=ALU.subtract),
                     reads=[BK(4), zt], writes=[rstd_sb])
                P.op('dve', lambda E: E.tensor_scalar(out=rstd_sb.ap, in0=rstd_sb.ap, scalar1=EPS, scalar2=None, op0=ALU.add), reads=[rstd_sb], writes=[rstd_sb])
                P.op('act', lambda E: E.activation(out=rstd_sb.ap, in_=rstd_sb.ap, func=AF.Sqrt), reads=[rstd_sb], writes=[rstd_sb])
                P.op('dve', lambda E: E.reciprocal(out=rstd_sb.ap, in_=rstd_sb.ap), reads=[rstd_sb], writes=[rstd_sb])
                for c in range(4):
                    mrange = (mixT, (4 + c) * T + g * GT, (4 + c) * T + (g + 1) * GT)
                    P.op('dve', lambda E, c=c, g=g: E.tensor_tensor(out=zt.ap, in0=mixT.ap[:, 4 + c, g * GT:(g + 1) * GT], in1=mean_sb.ap, op=ALU.subtract),
                         reads=[mrange, mean_sb], writes=[zt])
                    P.op('dve', lambda E: E.tensor_tensor(out=zt.ap, in0=zt.ap, in1=rstd_sb.ap, op=ALU.mult), reads=[zt, rstd_sb], writes=[zt])
                    P.op('act', lambda E, c=c, g=g: E.activation(out=mixT.ap[:, 4 + c, g * GT:(g + 1) * GT], in_=zt.ap, func=AF.Silu, scale=lngT.ap[:, c:c + 1], bias=lnbT.ap[:, c:c + 1]),
                         reads=[zt, lngT, lnbT], writes=[mrange])

            RW.reset()
            LMAX = NK
            NLS = 2 if kind == 0 else 1
            Ibs = [mk(RW, [LMAX], F32) for _ in range(NLS)]; Mbs = [mk(RW, [LMAX], BF16) for _ in range(NLS)]
            NMT = 2 if (kind == 0 and PIPE) else 1
            MTs = [mk(RW, [NKB, GT], BF16) for _ in range(NMT)]
            Rr = [mk(RW, [512], BF16) for _ in range(4)]
            SLOTS3 = [0, 1, 2]; SLOTS6 = [0, 1, 2, 3, 7, 4]
            NEB = 6
            Eb = [mk(RW, [GT], BF16) for _ in range(NEB)]; Pm = Eb
            bcs = mk(RW, [GT], F32); Dg = mk(RW, [8, 128], BF16)
            NPIECE = NG
            wN = BIS_B / (2 ** NITER)
            DB = [3, 7]
            st_ = dict(att_rr=0, d_rr=0, e_rr=0)

            def gen_idx(qp, pr):
                MT = MTs[qp % NMT]
                tiles = list(range(pr, min(pr + NLS, TPG)))
                info = []
                for r in tiles:
                    i = qp * TPG + r
                    L = 128 * (i + 1) if kind == 0 else NKS
                    qs = i * TP
                    Ib = Ibs[r % NLS]; Mb = Mbs[r % NLS]
                    info.append((r, i, L, Ib, Mb))
                    for h in range(8):
                        P.op('dve', lambda E, h=h, i=i: E.tensor_scalar(out=Dg.ap[0:TP, h, 0:TP], in0=identb.ap[0:TP, 0:TP], scalar1=Wt.ap[0:TP, i, h:h + 1], scalar2=None, op0=ALU.mult),
                             reads=[identb, (Wt, i * 8, i * 8 + 8)], writes=[(Dg, h * 128, h * 128 + 128)])
                    nkp = (L + 511) // 512
                    for kp in range(nkp):
                        k0 = kp * 512
                        kw = min(512, L - k0)

                        def dmm(h):
                            pb = DB[st_['d_rr'] % 2]; st_['d_rr'] += 1
                            P.op('pe', lambda E, h=h, pb=pb, kw=kw, k0=k0, qs=qs: E.matmul(banks[pb][0:TP, 0:kw], lhsT=QIT.ap[:, h // 2, qs:qs + TP], rhs=KIz[h % 2].ap[:, k0:k0 + kw],
                                                                                 start=True, stop=True),
                                 reads=[(QIT, (h // 2) * T + qs, (h // 2) * T + qs + TP), (KIz[h % 2], k0, k0 + kw)], writes=[BK(pb)])
                            rb = Rr[h % 4]
                            if h % 2 == 0 or h == 7:
                                P.op('act', lambda E, pb=pb, rb=rb, kw=kw: E.activation(out=rb.ap[0:TP, 0:kw], in_=banks[pb][0:TP, 0:kw], func=AF.Relu), reads=[BK(pb)], writes=[rb])
                            else:
                                P.op('dve', lambda E, pb=pb, rb=rb, kw=kw: E.tensor_scalar(out=rb.ap[0:TP, 0:kw], in0=banks[pb][0:TP, 0:kw], scalar1=0.0, scalar2=None, op0=ALU.max), reads=[BK(pb)], writes=[rb])

                        def amm(h):
                            rb = Rr[h % 4]
                            P.op('pe', lambda E, h=h, rb=rb, kw=kw: E.matmul(banks[4][0:TP, 0:kw], lhsT=Dg.ap[0:TP, h, 0:TP], rhs=rb.ap[0:TP, 0:kw], start=(h == 0), stop=(h == 7)),
                                 reads=[(Dg, h * 128, h * 128 + 128), rb], writes=[BK(4)])
                        dmm(0)
                        for h in range(8):
                            if h + 1 < 8:
                                dmm(h + 1)
                            amm(h)
                            if h % 4 == 3:
                                yield
                        P.op('dve', lambda E, k0=k0, kw=kw, Ib=Ib: E.tensor_copy(out=Ib.ap[0:TP, k0:k0 + kw], in_=banks[4][0:TP, 0:kw]), reads=[BK(4)], writes=[(Ib, k0, k0 + kw)])
                    if kind == 0:
                        P.op('dve', lambda E, L=L, Ib=Ib: E.memset(Ib.ap[0:64, L - 64:L], -1e30), writes=[(Ib, L - 64, L)])
                    yield
                bis = [t_ for t_ in info if t_[2] > 256]
                for (r, i, L, Ib, Mb) in bis:
                    col = r % NLS
                    P.op('dve', lambda E, col=col: E.memset(negmid.ap[0:TP, col:col + 1], 0.0), writes=[('negmid', col)])
                if kind != 0 and bis:
                    (r, i, L, Ib, Mb) = bis[0]
                    LA = (L // 2) // 64 * 64
                    P.op('dve', lambda E: E.memset(negmid.ap[0:TP, 1:2], 0.0), writes=[('negmid', 1)])
                    for it in range(NITER):
                        wk = BIS_B / (2 ** it)
                        P.op('act', lambda E: E.activation(out=Mb.ap[0:TP, 0:LA], in_=Ib.ap[0:TP, 0:LA], func=AF.Sign, bias=negmid.ap[0:TP, 0:1], scale=1.0, accum_out=sacc.ap[0:TP, 0:1]),
                             reads=[(Ib, 0, LA), ('negmid', 0)], writes=[(Mb, 0, LA), ('sacc', 0)])
                        P.op('dve', lambda E: E.tensor_scalar(out=Mb.ap[0:TP, LA:L], in0=Ib.ap[0:TP, LA:L], scalar1=negmid.ap[0:TP, 1:2], scalar2=0.0, op0=ALU.is_ge, op1=ALU.add,
                                                              accum_out=sacc.ap[0:TP, 1:2]),
                             reads=[(Ib, LA, L), ('negmid', 1)], writes=[(Mb, LA, L), ('sacc', 1)])
                        P.op('dve', lambda E: E.scalar_tensor_tensor(out=dd.ap[0:TP, 1:2], in0=sacc.ap[0:TP, 1:2], scalar=2.0, in1=sacc.ap[0:TP, 0:1], op0=ALU.mult, op1=ALU.add),
                             reads=[('sacc', 0), ('sacc', 1)], writes=[('dd', 1)])
                        P.op('dve', lambda E: E.tensor_scalar(out=dd.ap[0:TP, 0:1], in0=dd.ap[0:TP, 1:2], scalar1=float(512 - LA), scalar2=0.5, op0=ALU.is_ge, op1=ALU.subtract),
                             reads=[('dd', 1)], writes=[('dd', 0)])
                        P.op('dve', lambda E, wk=wk: E.scalar_tensor_tensor(out=negmid.ap[0:TP, 0:1], in0=dd.ap[0:TP, 0:1], scalar=-wk, in1=negmid.ap[0:TP, 0:1], op0=ALU.mult, op1=ALU.add),
                             reads=[('dd', 0), ('negmid', 0)], writes=[('negmid', 0)])
                        P.op('dve', lambda E, wk=wk: E.scalar_tensor_tensor(out=negmid.ap[0:TP, 1:2], in0=dd.ap[0:TP, 0:1], scalar=wk, in1=negmid.ap[0:TP, 1:2], op0=ALU.mult, op1=ALU.add),
                             reads=[('dd', 0), ('negmid', 1)], writes=[('negmid', 1)])
                        yield
                    bis = []
                for it in range(NITER if bis else 0):
                    wk = BIS_B / (2 ** it)
                    for (r, i, L, Ib, Mb) in bis:
                        col = r % NLS
                        if col == 0:
                            P.op('act', lambda E, L=L, Ib=Ib, Mb=Mb, col=col: E.activation(out=Mb.ap[0:TP, 0:L], in_=Ib.ap[0:TP, 0:L], func=AF.Sign, bias=negmid.ap[0:TP, col:col + 1], scale=1.0,
                                                                                       accum_out=sacc.ap[0:TP, col:col + 1]),
                                 reads=[(Ib, 0, L), ('negmid', col)], writes=[(Mb, 0, L), ('sacc', col)])
                        else:
                            P.op('dve', lambda E, L=L, Ib=Ib, Mb=Mb, col=col: E.tensor_scalar(out=Mb.ap[0:TP, 0:L], in0=Ib.ap[0:TP, 0:L], scalar1=negmid.ap[0:TP, col:col + 1], scalar2=0.0,
                                                                                          op0=ALU.is_ge, op1=ALU.add, accum_out=sacc.ap[0:TP, col:col + 1]),
                                 reads=[(Ib, 0, L), ('negmid', col)], writes=[(Mb, 0, L), ('sacc', col)])
                    for (r, i, L, Ib, Mb) in bis:
                        col = r % NLS
                        Cc = float(512 - L) if col == 0 else 256.0
                        sg = -wk if col == 0 else wk
                        P.op('dve', lambda E, Cc=Cc, col=col: E.tensor_scalar(out=dd.ap[0:TP, col:col + 1], in0=sacc.ap[0:TP, col:col + 1], scalar1=Cc, scalar2=0.5, op0=ALU.is_ge, op1=ALU.subtract),
                             reads=[('sacc', col)], writes=[('dd', col)])
                        P.op('dve', lambda E, sg=sg, col=col: E.scalar_tensor_tensor(out=negmid.ap[0:TP, col:col + 1], in0=dd.ap[0:TP, col:col + 1], scalar=sg, in1=negmid.ap[0:TP, col:col + 1],
                                                                                  op0=ALU.mult, op1=ALU.add),
                             reads=[('dd', col), ('negmid', col)], writes=[('negmid', col)])
                    for _y in range(BIS_YIELDS):
                        yield
                for (r, i, L, Ib, Mb) in info:
                    col = r % NLS
                    if L > 256:
                        sgn = -1.0 if col == 0 else 1.0
                        P.op('dve', lambda E, col=col, sgn=sgn: E.tensor_scalar(out=thr.ap[0:TP, col:col + 1], in0=negmid.ap[0:TP, col:col + 1], scalar1=sgn, scalar2=-wN, op0=ALU.mult, op1=ALU.add),
                             reads=[('negmid', col)], writes=[('thr', col)])
                    else:
                        P.op('dve', lambda E, col=col: E.memset(thr.ap[0:TP, col:col + 1], -1e29), writes=[('thr', col)])
                    P.op('dve', lambda E, L=L, Ib=Ib, Mb=Mb, col=col: E.tensor_scalar(out=Mb.ap[0:TP, 0:L], in0=Ib.ap[0:TP, 0:L], scalar1=thr.ap[0:TP, col:col + 1], scalar2=-30000.0, op0=ALU.is_lt, op1=ALU.mult),
                         reads=[(Ib, 0, L), ('thr', col)], writes=[(Mb, 0, L)])
                    nkb = (L + 127) // 128
                    kb = 0
                    half = 0
                    while kb < nkb:
                        nb = min(4, nkb - kb)
                        full = all(min(128, L - 128 * (kb + s_)) == 128 for s_ in range(nb))
                        if not full and nb > 1:
                            nb -= 1
                        base = half * 512
                        for s_ in range(nb):
                            kw_ = min(128, L - 128 * (kb + s_))
                            P.op('pe', lambda E, s_=s_, kw_=kw_, kb=kb, base=base, Mb=Mb: E.transpose(bkb(4)[0:kw_, base + s_ * 128: base + s_ * 128 + TP], Mb.ap[0:TP, (kb + s_) * 128:(kb + s_) * 128 + kw_], identb.ap[0:TP, 0:TP]),
                                 reads=[(Mb, (kb + s_) * 128, (kb + s_) * 128 + kw_), identb], writes=[BK(4)])
                        kw_ = min(128, L - 128 * kb) if nb == 1 else 128
                        srcv = bkb(4)[0:kw_, base:base + nb * 128].rearrange("p (a b) -> p a b", b=128)[:, :, 0:TP]
                        P.op('act', lambda E, kb=kb, nb=nb, kw_=kw_, srcv=srcv, r=r, MT=MT: E.activation(out=MT.ap[0:kw_, kb:kb + nb, r * TP:(r + 1) * TP], in_=srcv, func=AF.Copy),
                             reads=[BK(4)], writes=[(MT, kb * GT, (kb + nb) * GT)])
                        kb += nb
                        half ^= 1
                        yield

            def gen_att(qp):
                MT = MTs[qp % NMT]
                SL = SLOTS6 if (kind != 0 or qp == NPIECE - 1) else SLOTS3
                NSL_ = len(SL)
                SK_ = NSL_ - 1
                skey = lambda sl: BK(SL[sl])
                sview = lambda sl, rows, n: banks[SL[sl]][0:rows, 0:n]
                st_['att_rr'] = 0
                q0 = qp * GT
                kb_last = (qp * 4 + 3) if kind == 0 else (NKB - 1)
                tb0 = qp * 4 if kind == 0 else 32
                blocks = [(h, kb) for h in range(8) for kb in range(kb_last + 1)]

                def geom(kb):
                    kw_ = min(128, NK - 128 * kb)
                    if kind == 0:
                        jd = kb - 4 * qp
                        r0 = max(jd, 0)
                        return kw_, r0, r0 * 128, jd >= 0, 128
                    return kw_, 0, 0, (kb == NKB - 1), 64

                def front(h, kb):
                    kw_, r0, c0, diag, TPd = geom(kb)
                    hb = (h % 2) * 64; hc = h // 2
                    N = GT - c0
                    sb_ = st_['att_rr'] % NSL_; st_['att_rr'] += 1
                    P.op('pe', lambda E: E.matmul(sview(sb_, kw_, N), lhsT=KA.ap[:, hc, kb * 128:kb * 128 + kw_], rhs=QA.ap[:, h, q0 + c0:q0 + GT], start=True, stop=False),
                         reads=[(KA, hc * NK + kb * 128, hc * NK + kb * 128 + kw_), (QA, h * T + q0, h * T + q0 + GT)], writes=[skey(sb_)])
                    if diag:
                        P.op('pe', lambda E: E.matmul(sview(sb_, kw_, TPd), lhsT=identb.ap[0:kw_, 0:kw_], rhs=cdiag.ap[0:kw_, h, 0:TPd], start=False, stop=False),
                             reads=[identb, cdiag], writes=[skey(sb_)])
                    P.op('pe', lambda E: E.matmul(sview(sb_, kw_, N), lhsT=identb.ap[0:kw_, 0:kw_], rhs=MT.ap[0:kw_, kb, c0:GT], start=False, stop=True),
                         reads=[identb, (MT, kb * GT, (kb + 1) * GT)], writes=[skey(sb_)])
                    return sb_

                def back(h, kb, sb_):
                    kw_, r0, c0, diag, TPd = geom(kb)
                    hb = (h % 2) * 64; hc = h // 2
                    ob = 5 + (h % 2)
                    N = GT - c0
                    eb = Eb[st_['e_rr'] % NEB]; pm = Pm[st_['e_rr'] % NEB]; st_['e_rr'] += 1
                    if kind == 0 and h < 2:
                        for r in range(r0, 4):
                            dl = kb - (tb0 + r) + 32
                            a0 = r * 128 - c0
                            P.op('act', lambda E, a0=a0, dl=dl: E.activation(out=eb.ap[0:kw_, a0:a0 + 128], in_=banks[SL[sb_]][0:kw_, a0:a0 + 128], func=AF.Exp,
                                                                             bias=btab.ap[0:kw_, h, dl:dl + 1], scale=1.0),
                                 reads=[skey(sb_), btab], writes=[eb])
                    else:
                        dl = kb - tb0 + 32
                        P.op('act', lambda E: E.activation(out=eb.ap[0:kw_, 0:N], in_=sview(sb_, kw_, N), func=AF.Exp, bias=btab.ap[0:kw_, h, dl:dl + 1], scale=1.0),
                             reads=[skey(sb_), btab], writes=[eb])
                    P.op('pe', lambda E: E.matmul(banks[ob][0:65, c0:GT], lhsT=Vb.ap[0:kw_, kb, h, :], rhs=eb.ap[0:kw_, 0:N], start=(kb == 0), stop=(kb == kb_last)),
                         reads=[(Vb, kb * 520, kb * 520 + 520), eb], writes=[BK(ob)])
                    if kb == kb_last:
                        P.op('dve', lambda E: E.tensor_scalar(out=bcs.ap[64:65, 0:GT], in0=banks[ob][64:65, 0:GT], scalar1=1e-30, scalar2=None, op0=ALU.max), reads=[BK(ob)], writes=[bcs])
                        P.op('dve', lambda E: E.reciprocal(out=bcs.ap[64:65, 0:GT], in_=bcs.ap[64:65, 0:GT]), reads=[bcs], writes=[bcs])

                        def stage2():
                            nb_ = st_['att_rr'] % NSL_; st_['att_rr'] += 1
                            P.op('pe', lambda E: E.matmul(sview(nb_, 64, GT), lhsT=onesf.ap[64:65, 0:64], rhs=bcs.ap[64:65, 0:GT], start=True, stop=True), reads=[onesf, bcs], writes=[skey(nb_)])
                            P.op('act', lambda E: E.activation(out=bcs.ap[0:64, :], in_=sview(nb_, 64, GT), func=AF.Copy), reads=[skey(nb_)], writes=[bcs])
                            P.op('dve', lambda E: E.tensor_tensor(out=mixT.ap[hb:hb + 64, hc, q0:q0 + GT], in0=banks[ob][0:64, 0:GT], in1=bcs.ap[0:64, :], op=ALU.mult),
                                 reads=[BK(ob), bcs], writes=[(mixT, hc * T + q0, hc * T + q0 + GT)])
                        norm_q.append([NORM_DELAY, stage2])

                norm_q = []
                NORM_DELAY = 3
                if False:
                    P.op('dve', lambda E: E.memset(dd.ap[0:1, 0:1], 0.0), reads=[BK(0), BK(1), BK(2)], writes=[('pss', sl) for sl in range(NSL)] + [BK(0), BK(1), BK(2)])
                pend = []
                nf = 0
                for n in range(len(blocks)):
                    while nf < len(blocks) and nf <= n + SK_ - 1:
                        h_, kb_ = blocks[nf]
                        pend.append((h_, kb_, front(h_, kb_)))
                        nf += 1
                    back(*pend.pop(0))
                    for it_ in norm_q:
                        it_[0] -= 1
                    if norm_q and norm_q[0][0] <= 0:
                        norm_q.pop(0)[1]()
                    yield
                for it_ in list(norm_q):
                    it_[1]()
                norm_q.clear()
                if False:
                    P.op('dve', lambda E: E.memset(dd.ap[0:1, 0:1], 0.0), reads=[('pss', sl) for sl in range(NSL)], writes=[('pss', sl) for sl in range(NSL)] + [BK(0), BK(1), BK(2)])
                yield

            def run_streams(A, B, na, nb):
                ia = ib = 0
                a_done = A is None
                b_done = B is None
                while not (a_done and b_done):
                    pick_a = (not a_done) and (b_done or ia * max(nb, 1) <= ib * max(na, 1))
                    if pick_a:
                        try:
                            next(A); ia += 1
                        except StopIteration:
                            a_done = True
                    else:
                        try:
                            next(B); ib += 1
                        except StopIteration:
                            b_done = True

            def chain(*gens):
                for g_ in gens:
                    yield from g_

            def count_steps(make):
                return None

            pairs = list(range(0, TPG, NLS))
            if not PIPE:
                for qp in range(NPIECE):
                    run_streams(chain(*[gen_idx(qp, pr) for pr in pairs]), None, 1, 1)
                    run_streams(gen_att(qp), None, 1, 1)
            else:
                if kind == 0:
                    run_streams(chain(*[gen_idx(0, pr) for pr in pairs]), None, 1, 1)
                else:
                    load_kidx()
                    run_streams(chain(*[gen_idx(0, pr) for pr in pairs]), gen_load(), 60, 9)
                for qp in range(NPIECE):
                    kbl = (qp * 4 + 4) if kind == 0 else NKB
                    na = 8 * kbl + 1
                    if qp + 1 < NPIECE:
                        nb = 0
                        for r in range(TPG):
                            L_ = 128 * ((qp + 1) * TPG + r + 1)
                            nb += ((L_ + 511) // 512) * 2 + 1 + ((L_ + 127) // 128 + 3) // 4
                        nb += NITER * BIS_YIELDS * len(pairs)
                        run_streams(gen_att(qp), chain(*[gen_idx(qp + 1, pr) for pr in pairs]), na, nb)
                    else:
                        run_streams(gen_att(qp), None, na, 1)

            if 3 not in phases:
                return
            RU.reset(); RW.reset()
            fgbc = mk(RU, [D], F32); g1bc = mk(RU, [D], F32); g2bc = mk(RU, [D], F32)
            P.dma('sp', fgbc.ap, fgbc_d, writes=[fgbc])
            bcast_vec(g1T, g1bc)
            bcast_vec(g2T, g2bc)
            wo = mk(RU, [8, D], BF16)
            xg = [mk(RU, [D], F32) for _ in range(TPG)]
            h2T = mk(RU, [8, GT], BF16)
            hid = mk(RU, [32, GT], BF16)
            f1r = [mk(RW, [4, 8, 128], BF16) for _ in range(2)]
            f2r = [mk(RW, [8, 512], BF16) for _ in range(2)]
            xn2 = mk(RW, [D], F32); tmp5 = mk(RW, [512], F32); rl = mk(RW, [GT], F32); junk3 = mk(RW, [D], BF16)
            P.dma('sp', wo.ap, wsc_out.rearrange("(kc p) n -> p kc n", p=128), reads=WOUT, writes=[wo])
            f1_rr = 0; f2_rr = 0; f1b_rr = 0
            for g in range(NG):
                for tl in range(TPG):
                    tt = g * TPG + tl
                    xb = xg[tl]
                    P.dma('sp', xb.ap[0:TP, :], x_d[tt * TP:(tt + 1) * TP, :], writes=[xb])
                    for half in range(2):
                        for kc in range(8):
                            P.op('pe', lambda E, kc=kc, half=half, tt=tt: E.matmul(banks[half][0:TP, :], lhsT=mixT.ap[:, kc, tt * TP:(tt + 1) * TP], rhs=wo.ap[:, kc, half * 512:(half + 1) * 512],
                                                                             start=(kc == 0), stop=(kc == 7)),
                                 reads=[(mixT, kc * T + tt * TP, kc * T + (tt + 1) * TP), wo], writes=[BK(half)])
                        P.op('dve', lambda E, half=half: E.tensor_tensor(out=tmp5.ap[0:TP, :], in0=banks[half][0:TP, :], in1=g1bc.ap[0:TP, half * 512:(half + 1) * 512], op=ALU.mult),
                             reads=[BK(half), g1bc], writes=[tmp5])
                        P.op('dve', lambda E, half=half, xb=xb: E.tensor_tensor(out=xb.ap[0:TP, half * 512:(half + 1) * 512], in0=xb.ap[0:TP, half * 512:(half + 1) * 512], in1=tmp5.ap[0:TP, :], op=ALU.add),
                             reads=[tmp5, xb], writes=[xb])
                    P.op('act', lambda E, xb=xb, tl=tl: E.activation(out=junk3.ap[0:TP, :], in_=xb.ap[0:TP, :], func=AF.Square, accum_out=ss.ap[0:TP, tl:tl + 1]),
                         reads=[xb], writes=[junk3, (ss, tl, tl + 1)])
                    P.op('dve', lambda E, tl=tl: E.tensor_scalar(out=rs.ap[0:TP, tl:tl + 1], in0=ss.ap[0:TP, tl:tl + 1], scalar1=1.0 / D, scalar2=EPS, op0=ALU.mult, op1=ALU.add),
                         reads=[(ss, tl, tl + 1)], writes=[(rs, tl, tl + 1)])
                    P.op('act', lambda E, tl=tl: E.activation(out=rs.ap[0:TP, tl:tl + 1], in_=rs.ap[0:TP, tl:tl + 1], func=AF.Sqrt), reads=[(rs, tl, tl + 1)], writes=[(rs, tl, tl + 1)])
                    P.op('dve', lambda E, tl=tl: E.reciprocal(out=rs.ap[0:TP, tl:tl + 1], in_=rs.ap[0:TP, tl:tl + 1]), reads=[(rs, tl, tl + 1)], writes=[(rs, tl, tl + 1)])
                    P.op('dve', lambda E, xb=xb, tl=tl: E.tensor_scalar(out=xn2.ap[0:TP, :], in0=xb.ap[0:TP, :], scalar1=rs.ap[0:TP, tl:tl + 1], scalar2=None, op0=ALU.mult),
                         reads=[xb, (rs, tl, tl + 1)], writes=[xn2])
                    for kc in range(8):
                        bk = 2 + kc // 4
                        P.op('pe', lambda E, kc=kc, bk=bk: E.transpose(banks[bk][:, (kc % 4) * 128:(kc % 4) * 128 + TP], xn2.ap[0:TP, kc * 128:(kc + 1) * 128], identf.ap[0:TP, 0:TP]),
                             reads=[xn2, identf], writes=[BK(bk)])
                    for kc in range(8):
                        bk = 2 + kc // 4
                        P.op('act', lambda E, kc=kc, bk=bk, tl=tl: E.activation(out=h2T.ap[:, kc, tl * TP:(tl + 1) * TP], in_=banks[bk][:, (kc % 4) * 128:(kc % 4) * 128 + TP],
                                                                        func=AF.Identity, scale=G2T.ap[:, kc:kc + 1], bias=S2T.ap[:, kc:kc + 1]),
                             reads=[BK(bk), G2T, S2T], writes=[(h2T, kc * GT + tl * TP, kc * GT + (tl + 1) * TP)])
                for j4 in range(8):
                    fb = f1r[f1_rr % 2]; f1_rr += 1
                    P.dma('sp', fb.ap, wsc_ff1[j4 * 4:(j4 + 1) * 4].rearrange("j p kc n -> p j kc n"), reads=WFF1, writes=[fb])
                    for jj in range(4):
                        jh = j4 * 4 + jj
                        pb = f1b_rr % 4; f1b_rr += 1
                        for kc in range(8):
                            P.op('pe', lambda E, fb=fb, jj=jj, kc=kc, pb=pb: E.matmul(banks[pb][:, 0:GT], lhsT=fb.ap[:, jj, kc, :], rhs=h2T.ap[:, kc, :], start=(kc == 0), stop=(kc == 7)),
                                 reads=[fb, h2T], writes=[BK(pb)])
                        P.op('act', lambda E, pb=pb: E.activation(out=rl.ap, in_=banks[pb][:, 0:GT], func=AF.Relu), reads=[BK(pb)], writes=[rl])
                        P.op('dve', lambda E, jh=jh: E.tensor_tensor(out=hid.ap[:, jh, :], in0=rl.ap, in1=rl.ap, op=ALU.mult), reads=[rl], writes=[(hid, jh * GT, (jh + 1) * GT)])
                for half in range(2):
                    for j8 in range(4):
                        fb = f2r[f2_rr % 2]; f2_rr += 1
                        P.dma('sp', fb.ap, wsc_ff2[j8 * 1024:(j8 + 1) * 1024, half * 512:(half + 1) * 512].rearrange("(jj p) n -> p jj n", p=128), reads=WFF2, writes=[fb])
                        for jj in range(8):
                            jh = j8 * 8 + jj
                            for tl in range(TPG):
                                P.op('pe', lambda E, fb=fb, jj=jj, jh=jh, tl=tl: E.matmul(banks[4 + tl][0:TP, :], lhsT=hid.ap[:, jh, tl * TP:(tl + 1) * TP], rhs=fb.ap[:, jj, :],
                                                                                     start=(jh == 0), stop=(jh == 31)),
                                     reads=[(hid, jh * GT, (jh + 1) * GT), fb], writes=[BK(4 + tl)])
                    for tl in range(TPG):
                        xb = xg[tl]
                        P.op('dve', lambda E, half=half, tl=tl: E.tensor_tensor(out=tmp5.ap[0:TP, :], in0=banks[4 + tl][0:TP, :], in1=g2bc.ap[0:TP, half * 512:(half + 1) * 512], op=ALU.mult),
                             reads=[BK(4 + tl), g2bc], writes=[tmp5])
                        P.op('dve', lambda E, half=half, xb=xb: E.tensor_tensor(out=xb.ap[0:TP, half * 512:(half + 1) * 512], in0=xb.ap[0:TP, half * 512:(half + 1) * 512], in1=tmp5.ap[0:TP, :], op=ALU.add),
                             reads=[tmp5, xb], writes=[xb])
                for tl in range(TPG):
                    tt = g * TPG + tl
                    xb = xg[tl]
                    P.op('act', lambda E, xb=xb, tl=tl: E.activation(out=junk3.ap[0:TP, :], in_=xb.ap[0:TP, :], func=AF.Square, accum_out=ss.ap[0:TP, 8 + tl:9 + tl]),
                         reads=[xb], writes=[junk3, (ss, 8 + tl, 9 + tl)])
                    P.op('dve', lambda E, tl=tl: E.tensor_scalar(out=rs.ap[0:TP, 8 + tl:9 + tl], in0=ss.ap[0:TP, 8 + tl:9 + tl], scalar1=1.0 / D, scalar2=EPS, op0=ALU.mult, op1=ALU.add),
                         reads=[(ss, 8 + tl, 9 + tl)], writes=[(rs, 8 + tl, 9 + tl)])
                    P.op('act', lambda E, tl=tl: E.activation(out=rs.ap[0:TP, 8 + tl:9 + tl], in_=rs.ap[0:TP, 8 + tl:9 + tl], func=AF.Sqrt), reads=[(rs, 8 + tl, 9 + tl)], writes=[(rs, 8 + tl, 9 + tl)])
                    P.op('dve', lambda E, tl=tl: E.reciprocal(out=rs.ap[0:TP, 8 + tl:9 + tl], in_=rs.ap[0:TP, 8 + tl:9 + tl]), reads=[(rs, 8 + tl, 9 + tl)], writes=[(rs, 8 + tl, 9 + tl)])
                    P.op('dve', lambda E, xb=xb, tl=tl: E.scalar_tensor_tensor(out=xb.ap[0:TP, :], in0=xb.ap[0:TP, :], scalar=rs.ap[0:TP, 8 + tl:9 + tl], in1=fgbc.ap[0:TP, :], op0=ALU.mult, op1=ALU.mult),
                         reads=[xb, (rs, 8 + tl, 9 + tl), fgbc], writes=[xb])
                    P.dma('pool', o_y[tt * TP:(tt + 1) * TP, :], xb.ap[0:TP, :], reads=[xb], writes=[('o_y', j, tt)])

        for U in units:
            do_unit(U)
        P.finish()
        print("instructions:", P.nins, {e: len(v) for e, v in P.ops.items()})
        P.run_block()
    return nc


def _consts():
    bf = ml_dtypes.bfloat16
    sl = np.array([2.0 ** (-(h + 1)) for h in range(8)], np.float64)
    s_loc = np.arange(128)[:, None, None]
    t_loc = np.arange(128)[None, None, :]
    cdiag = (-2.0 * sl[None, :, None] * np.maximum(s_loc - t_loc, 0)).astype(np.float32)
    out = {"c_cdiag": cdiag.astype(bf)}
    s_loc1 = np.arange(128)[:, None, None].astype(np.float64)
    dl = (np.arange(36) - 32)[None, None, :].astype(np.float64)
    out["c_btab"] = (sl[None, :, None] * (s_loc1 + 128.0 * dl)).astype(np.float32)
    return out


_NC_CACHE = {}


def kernel(x_prompt, x_sample, c_prompt, c_sample, cache_k, cache_v, cache_kidx, state_conv,
           w_ada, b_ada, norm1_g, w_in, conv_w, conv_b, conv_ln_g, conv_ln_b, w_out, norm2_g,
           w_ff1, w_ff2, final_g):
    f = lambda a: np.ascontiguousarray(np.asarray(a, dtype=np.float32))
    x_prompt, x_sample, c_prompt, c_sample = f(x_prompt), f(x_sample), f(c_prompt), f(c_sample)
    cache_k, cache_v, cache_kidx, state_conv = f(cache_k)[0], f(cache_v)[0], f(cache_kidx)[0], f(state_conv)[0]
    if 'nc' not in _NC_CACHE:
        _NC_CACHE['nc'] = build()
    nc = _NC_CACHE['nc']
    consts = _consts()
    vecT = lambda v, n: np.ascontiguousarray(f(v).reshape(n, 128).T)
    shared = {
        "w_ada": f(w_ada)[0], "b_adaT": vecT(f(b_ada)[0], 48), "n1gT": vecT(f(norm1_g)[0], 8), "n2gT": vecT(f(norm2_g)[0], 8),
        "fgbc": np.ascontiguousarray(np.broadcast_to(f(final_g)[None, :], (128, D))),
        "w_in": f(w_in)[0], "w_out": f(w_out)[0], "w_ff1": f(w_ff1)[0], "w_ff2": f(w_ff2)[0],
        "convwT": np.ascontiguousarray(f(conv_w)[0].reshape(31, 4, 128).transpose(2, 1, 0)),
        "convbT": vecT(f(conv_b)[0], 4), "lngT": vecT(f(conv_ln_g)[0], 4), "lnbT": vecT(f(conv_ln_b)[0], 4),
    }
    shared.update(consts)
    in_maps = []
    for c in range(8):
        sl_ = slice(2 * c, 2 * c + 2)
        cc = np.concatenate([c_prompt[sl_], c_sample[sl_]], axis=0)
        m = dict(shared)
        m["xp"] = np.ascontiguousarray(x_prompt[sl_])
        m["xs"] = np.ascontiguousarray(x_sample[sl_])
        m["cT"] = np.ascontiguousarray(cc.reshape(4, 8, 128).transpose(2, 1, 0))
        m["ckT"] = np.ascontiguousarray(cache_k[sl_].transpose(0, 2, 3, 1))
        m["cv"] = np.ascontiguousarray(cache_v[sl_].reshape(2, PAST, 512))
        m["ckiT"] = np.ascontiguousarray(cache_kidx[sl_].transpose(0, 2, 1))
        m["scT"] = np.ascontiguousarray(state_conv[sl_].transpose(0, 2, 1))
        in_maps.append(m)
    res = run_bass_kernel_spmd(nc, in_maps, core_ids=list(range(8)))
    R = res.results
    cat = lambda k: np.concatenate([r[k] for r in R], axis=0)
    y_prompt = cat("yp"); y_sample = cat("ys")
    k_prompt = cat("kp").reshape(1, 16, SEQ, 8, 64); v_prompt = cat("vp").reshape(1, 16, SEQ, 8, 64)
    kidx_prompt = cat("kip").reshape(1, 16, SEQ, 64); conv_prompt = cat("cp").reshape(1, 16, 30, 512)
    k_sample = cat("ks").reshape(1, 16, DSEQ, 8, 64); v_sample = cat("vs").reshape(1, 16, DSEQ, 8, 64)
    kidx_sample = cat("kis").reshape(1, 16, DSEQ, 64); conv_sample = cat("cs").reshape(1, 16, 30, 512)
    return (y_prompt, y_sample, k_prompt, v_prompt, kidx_prompt, conv_prompt, k_sample, v_sample, kidx_sample, conv_sample)
```
